# Optimizing a Trainium2 kernel written in Bass

```python
import math
import jax, jax.numpy as jnp
from jax import lax
import numpy as np


D_MODEL = 4096
BATCH = 8
SEQ = 2048
DEPTH = 1
DEC_BATCH = 4
DEC_SEQ = 2048
PAST_LEN = 128

HEAD_DIM = 128
A_HEADS = 16
A_WIDTH = A_HEADS * HEAD_DIM
DILATED_PATTERNS = ((128, 1), (512, 4), (2048, 16))
B_HEADS = 8
B_WIDTH = B_HEADS * 2 * HEAD_DIM
IN_WIDTH = 3 * A_WIDTH + 3 * B_WIDTH + 2 * D_MODEL
Q_BLOCK = 128
PK_HEADS = 8
PK_DIM = 256
N_KEYS = 128
PK_TOPK = 16
N_EXPERTS = N_KEYS * N_KEYS
TOKEN_CHUNK = 128
NORM_EPS = 1e-6
NEG_INF = -1e30

kernel_name = "hybrid_dilated_diffattn_peer_encoder"


def rms_norm(x, g):
    xf = x.astype(jnp.float32)
    y = xf * lax.rsqrt(jnp.mean(xf * xf, axis=-1, keepdims=True) + NORM_EPS)
    return (y * g.astype(jnp.float32)).astype(x.dtype)


def alibi_slopes(n):
    return jnp.asarray(2.0 ** (-8.0 * np.arange(1, n + 1) / n), dtype=jnp.float32)


def dilated_window_attention(q, k, v, slopes, window, dilation):
    b, s, h, hd = q.shape
    radius = window // (2 * dilation)
    blk = radius
    unit = dilation * blk
    s_pad = -(-s // unit) * unit
    l_pad = s_pad // dilation
    nb = l_pad // blk

    def to_sub(a):
        a = jnp.pad(a, ((0, 0), (0, s_pad - s), (0, 0), (0, 0)))
        a = a.reshape(b, l_pad, dilation, h, hd).transpose(0, 2, 1, 3, 4)
        return a.reshape(b, dilation, nb, blk, h, hd)

    def band(a):
        z = jnp.zeros_like(a[:, :, :1])
        prev = jnp.concatenate([z, a[:, :, :-1]], axis=2)
        nxt = jnp.concatenate([a[:, :, 1:], z], axis=2)
        return jnp.concatenate([prev, a, nxt], axis=3)

    qs = to_sub(q)
    kb = band(to_sub(k))
    vb = band(to_sub(v))
    scores = jnp.einsum('bgnqhd,bgnkhd->bgnhqk', qs, kb).astype(jnp.float32)
    qi = jnp.arange(nb)[:, None] * blk + jnp.arange(blk)[None, :]
    ki = jnp.arange(nb)[:, None] * blk + jnp.arange(-blk, 2 * blk)[None, :]
    rel = jnp.abs(ki[:, None, :] - qi[:, :, None])
    kpos = ki[None] * dilation + jnp.arange(dilation)[:, None, None]
    key_ok = (ki >= 0)[None] & (kpos < s)
    valid = (rel <= radius)[None, :, None] & key_ok[:, :, None, None, :]
    bias = -slopes[None, :, None, None] * (dilation * rel).astype(jnp.float32)[:, None]
    scores = jnp.where(valid, scores + bias, NEG_INF)
    m = jnp.max(scores, axis=-1, keepdims=True)
    e = jnp.exp(scores - m)
    den = jnp.sum(e, axis=-1, keepdims=True)
    out = jnp.einsum('bgnhqk,bgnkhd->bgnqhd', (e / den).astype(v.dtype), vb)
    lse = (m + jnp.log(den))[..., 0]
    out = out.reshape(b, dilation, l_pad, h, hd).transpose(0, 2, 1, 3, 4).reshape(b, s_pad, h, hd)[:, :s]
    lse = lse.transpose(0, 1, 2, 4, 3).reshape(b, dilation, l_pad, h).transpose(0, 2, 1, 3).reshape(b, s_pad, h)[:, :s]
    return out, lse


def mixture_dilated_attention(q, k, v, slopes):
    outs, lses = [], []
    for window, dilation in DILATED_PATTERNS:
        o, l = dilated_window_attention(q, k, v, slopes, window, dilation)
        outs.append(o)
        lses.append(l)
    w = jax.nn.softmax(jnp.stack(lses), axis=0)
    out = jnp.sum(w[..., None] * jnp.stack(outs).astype(jnp.float32), axis=0)
    return out.astype(q.dtype)


def differential_attention(q1, q2, k1, k2, v, slopes, lam):
    b, s, h, hd = q1.shape
    nq = s // Q_BLOCK

    def blocks(a):
        return a.reshape(b, nq, Q_BLOCK, h, hd).transpose(1, 0, 2, 3, 4)

    kpos = jnp.arange(s)

    def step(args):
        i, q1b, q2b = args
        t = i * Q_BLOCK + jnp.arange(Q_BLOCK)
        dist = jnp.abs(t[:, None] - kpos[None, :]).astype(jnp.float32)
        bias = -slopes[:, None, None] * dist
        p1 = jax.nn.softmax(jnp.einsum('bqhd,bkhd->bhqk', q1b, k1).astype(jnp.float32) + bias, axis=-1)
        p2 = jax.nn.softmax(jnp.einsum('bqhd,bkhd->bhqk', q2b, k2).astype(jnp.float32) + bias, axis=-1)
        a = (p1 - lam * p2).astype(v.dtype)
        return jnp.einsum('bhqk,bkhe->bqhe', a, v)

    out = lax.map(step, (jnp.arange(nq), blocks(q1), blocks(q2)))
    return out.transpose(1, 0, 2, 3, 4).reshape(b, s, h, v.shape[-1])


def peer_ffn(x, w_query, sub_keys_1, sub_keys_2, expert_u, expert_v):
    b, s, d = x.shape
    t = b * s
    xf = x.reshape(t, d)
    q = (xf @ w_query).reshape(t, PK_HEADS, 2, PK_DIM // 2)
    s1 = jnp.einsum('thc,hnc->thn', q[:, :, 0], sub_keys_1).astype(jnp.float32)
    s2 = jnp.einsum('thc,hnc->thn', q[:, :, 1], sub_keys_2).astype(jnp.float32)
    v1, i1 = lax.top_k(s1, PK_TOPK)
    v2, i2 = lax.top_k(s2, PK_TOPK)
    cand_s = (v1[..., :, None] + v2[..., None, :]).reshape(t, PK_HEADS, PK_TOPK * PK_TOPK)
    cand_i = (i1[..., :, None] * N_KEYS + i2[..., None, :]).reshape(t, PK_HEADS, PK_TOPK * PK_TOPK)
    top_s, pos = lax.top_k(cand_s, PK_TOPK)
    idx = jnp.take_along_axis(cand_i, pos, axis=-1)
    gate = jax.nn.softmax(top_s, axis=-1)
    nc = t // TOKEN_CHUNK
    e_tok = PK_HEADS * PK_TOPK

    def step(args):
        xc, ic, gc = args
        u = expert_u[ic]
        hdn = jnp.einsum('cd,ced->ce', xc, u)
        a = (jax.nn.gelu(hdn.astype(jnp.float32)) * gc).astype(expert_v.dtype)
        return jnp.einsum('ce,ced->cd', a, expert_v[ic])

    out = lax.map(step, (xf.reshape(nc, TOKEN_CHUNK, d),
                         idx.reshape(nc, TOKEN_CHUNK, e_tok),
                         gate.reshape(nc, TOKEN_CHUNK, e_tok)))
    return out.reshape(b, s, d).astype(x.dtype)


def encoder_trunk(x, w_in, w_proj_a, w_proj_b, w_out, g_mix_norm, lam_q1, lam_k1, lam_q2, lam_k2,
                  g_subln, g_ffn_norm, w_query, sub_keys_1, sub_keys_2, expert_u, expert_v, g_final):
    b, s, _ = x.shape
    slopes_a = alibi_slopes(A_HEADS)
    slopes_b = alibi_slopes(B_HEADS)
    scale = HEAD_DIM ** -0.5
    splits = [int(c) for c in np.cumsum([A_WIDTH] * 3 + [B_WIDTH] * 3 + [D_MODEL])]
    for l in range(DEPTH):
        lam_init = 0.8 - 0.6 * math.exp(-0.3 * l)
        xn = rms_norm(x, g_mix_norm[l])
        z = jnp.einsum('bsd,de->bse', xn, w_in[l])
        qa, ka, va, qb, kb, vb, gate_a, gate_b = jnp.split(z, splits, axis=-1)
        shp_a = (b, s, A_HEADS, HEAD_DIM)
        o_a = mixture_dilated_attention(qa.reshape(shp_a) * scale, ka.reshape(shp_a), va.reshape(shp_a),
                                        slopes_a).reshape(b, s, A_WIDTH)
        qb = qb.reshape(b, s, B_HEADS, 2, HEAD_DIM) * scale
        kb = kb.reshape(b, s, B_HEADS, 2, HEAD_DIM)
        lam = (jnp.exp(jnp.sum(lam_q1[l].astype(jnp.float32) * lam_k1[l].astype(jnp.float32)))
               - jnp.exp(jnp.sum(lam_q2[l].astype(jnp.float32) * lam_k2[l].astype(jnp.float32)))
               + lam_init)
        o_b = differential_attention(qb[:, :, :, 0], qb[:, :, :, 1], kb[:, :, :, 0], kb[:, :, :, 1],
                                     vb.reshape(b, s, B_HEADS, 2 * HEAD_DIM), slopes_b, lam)
        o_b = (rms_norm(o_b, g_subln[l]) * (1.0 - lam_init)).reshape(b, s, B_WIDTH)
        merged = (jax.nn.sigmoid(gate_a) * (o_a @ w_proj_a[l])
                  + jax.nn.sigmoid(gate_b) * (o_b @ w_proj_b[l]))
        x = x + merged @ w_out[l]
        x = x + peer_ffn(rms_norm(x, g_ffn_norm[l]), w_query[l], sub_keys_1[l], sub_keys_2[l],
                         expert_u[l], expert_v[l])
    return rms_norm(x, g_final)


def setup_inputs(seed: int = 0) -> dict:
    key = jax.random.key(seed)
    ks = jax.random.split(key, 20)
    nrm = jax.random.normal
    f32 = jnp.float32
    return {
        'x_prompt': nrm(ks[0], (BATCH, SEQ, D_MODEL), f32),
        'x_sample': nrm(ks[1], (DEC_BATCH, DEC_SEQ, D_MODEL), f32),
        'w_in': nrm(ks[2], (DEPTH, D_MODEL, IN_WIDTH), f32) * D_MODEL ** -0.5,
        'w_proj_a': nrm(ks[3], (DEPTH, A_WIDTH, D_MODEL), f32) * A_WIDTH ** -0.5,
        'w_proj_b': nrm(ks[4], (DEPTH, B_WIDTH, D_MODEL), f32) * B_WIDTH ** -0.5,
        'w_out': nrm(ks[5], (DEPTH, D_MODEL, D_MODEL), f32) * D_MODEL ** -0.5,
        'g_mix_norm': 1.0 + 0.01 * nrm(ks[6], (DEPTH, D_MODEL), f32),
        'lam_q1': 0.1 * nrm(ks[7], (DEPTH, HEAD_DIM), f32),
        'lam_k1': 0.1 * nrm(ks[8], (DEPTH, HEAD_DIM), f32),
        'lam_q2': 0.1 * nrm(ks[9], (DEPTH, HEAD_DIM), f32),
        'lam_k2': 0.1 * nrm(ks[10], (DEPTH, HEAD_DIM), f32),
        'g_subln': 1.0 + 0.01 * nrm(ks[11], (DEPTH, 2 * HEAD_DIM), f32),
        'g_ffn_norm': 1.0 + 0.01 * nrm(ks[12], (DEPTH, D_MODEL), f32),
        'w_query': nrm(ks[13], (DEPTH, D_MODEL, PK_HEADS * PK_DIM), f32) * D_MODEL ** -0.5,
        'sub_keys_1': nrm(ks[14], (DEPTH, PK_HEADS, N_KEYS, PK_DIM // 2), f32) * (PK_DIM // 2) ** -0.5,
        'sub_keys_2': nrm(ks[15], (DEPTH, PK_HEADS, N_KEYS, PK_DIM // 2), f32) * (PK_DIM // 2) ** -0.5,
        'expert_u': nrm(ks[16], (DEPTH, N_EXPERTS, D_MODEL), f32) * D_MODEL ** -0.5,
        'expert_v': nrm(ks[17], (DEPTH, N_EXPERTS, D_MODEL), f32) * PK_HEADS ** -0.5,
        'g_final': 1.0 + 0.01 * nrm(ks[18], (D_MODEL,), f32),
    }


def reference(x_prompt, x_sample, w_in, w_proj_a, w_proj_b, w_out, g_mix_norm, lam_q1, lam_k1, lam_q2,
              lam_k2, g_subln, g_ffn_norm, w_query, sub_keys_1, sub_keys_2, expert_u, expert_v, g_final):
    y_prompt = encoder_trunk(x_prompt, w_in, w_proj_a, w_proj_b, w_out, g_mix_norm, lam_q1, lam_k1,
                             lam_q2, lam_k2, g_subln, g_ffn_norm, w_query, sub_keys_1, sub_keys_2,
                             expert_u, expert_v, g_final)
    y_sample = encoder_trunk(x_sample, w_in, w_proj_a, w_proj_b, w_out, g_mix_norm, lam_q1, lam_k1,
                             lam_q2, lam_k2, g_subln, g_ffn_norm, w_query, sub_keys_1, sub_keys_2,
                             expert_u, expert_v, g_final)
    return (y_prompt, y_sample)
```

```python
import math
from contextlib import ExitStack

import numpy as np
import ml_dtypes

import concourse.bass as bass
import concourse.mybir as mybir
from concourse.alu_op_type import AluOpType as ALU
from concourse.bass_utils import run_bass_kernel_spmd

F32 = mybir.dt.float32
BF16 = mybir.dt.bfloat16
AF = mybir.ActivationFunctionType
AX = mybir.AxisListType

D = 4096
SEQ = 2048
TQ = 3072
TK = 4096
AW = 2048
BW = 2048
INW = 20480
NE = 16384
EPS = 1e-6
SCALE = 128 ** -0.5
LAM_INIT = 0.8 - 0.6 * math.exp(-0.0)
NEGBIG = -30000.0
ABW = 3968

ALL_STAGES = ("s1", "s2", "s3", "s4", "s5a", "s5b", "s6")
ENGS = ("pe", "dve", "act", "pool", "sp")
NDMA = 8


class Sched:
    def __init__(self, nc, stack):
        self.nc = nc
        self.sem = {e: stack.enter_context(nc.semaphore("s_" + e)) for e in ENGS}
        self.dsem = {q: [stack.enter_context(nc.semaphore("d_%s%d" % (q, i))) for i in range(NDMA)]
                     for q in ("sp", "act", "pool")}
        self.cnt = {e: 0 for e in ENGS}
        self.dcnt = {q: 0 for q in self.dsem}
        self.lists = {e: [] for e in ENGS}
        self.waited = {e: {} for e in ENGS}
        self.lastw = {}
        self.readers = {}
        self.n_inst = 0

    def _semobj(self, key):
        if isinstance(key, tuple):
            return self.dsem[key[0]][key[1]]
        return self.sem[key]

    def _need(self, eng, ev, waits):
        if ev is None:
            return
        key, val = ev
        if self.waited[eng].get(key, 0) >= val:
            return
        self.waited[eng][key] = val
        waits.append((key, val))

    def _deps(self, eng, reads, writes, waits):
        for r in reads:
            self._need(eng, self.lastw.get(r), waits)
        for w in writes:
            self._need(eng, self.lastw.get(w), waits)
            for ev in self.readers.get(w, ()):
                self._need(eng, ev, waits)

    def _commit(self, ev, reads, writes):
        for r in reads:
            self.readers.setdefault(r, []).append(ev)
        for w in writes:
            self.lastw[w] = ev
            self.readers[w] = []

    def op(self, eng, fn, reads=(), writes=()):
        waits = []
        self._deps(eng, reads, writes, waits)
        if eng == "pe":
            waits = [w for w in waits if w[0] != "pe"]
        self.cnt[eng] += 1
        ev = (eng, self.cnt[eng])
        self.lists[eng].append((fn, waits, (eng, 1)))
        self._commit(ev, reads, writes)
        self.n_inst += 1 + len(waits)
        return ev

    def dma(self, q, fn, reads=(), writes=()):
        waits = []
        j = self.dcnt[q]
        slot = j % NDMA
        prev = 16 * (j // NDMA)
        key = (q, slot)
        if prev > 0:
            self._need(q, (key, prev), waits)
        self._deps(q, reads, writes, waits)
        self.dcnt[q] += 1
        ev = (key, prev + 16)
        self.lists[q].append((fn, waits, (key, 16)))
        self._commit(ev, reads, writes)
        self.n_inst += 1 + len(waits)
        return ev

    def barrier(self):
        evs = [(e, self.cnt[e]) for e in ENGS if self.cnt[e] > 0]
        for q in self.dsem:
            j = self.dcnt[q]
            for slot in range(NDMA):
                if j > slot:
                    n = (j - 1 - slot) // NDMA + 1
                    evs.append(((q, slot), 16 * n))
        for e in ENGS:
            waits = []
            for ev in evs:
                self._need(e, ev, waits)
            if waits:
                self.lists[e].append((None, waits, None))
        self.lastw = {}
        self.readers = {}

    def flush(self):
        nc = self.nc
        lists = self.lists
        self.lists = {e: [] for e in ENGS}
        sched = self

        def run(engobj, lst):
            for fn, waits, inc in lst:
                for key, val in waits:
                    engobj.wait_ge(sched._semobj(key), val)
                if fn is not None:
                    ins = fn(engobj)
                    ins.then_inc(sched._semobj(inc[0]), inc[1])

        with nc.Block() as block:
            @block.tensor
            def _(e):
                run(e, lists["pe"])

            @block.vector
            def _(e):
                run(e, lists["dve"])

            @block.scalar
            def _(e):
                run(e, lists["act"])

            @block.gpsimd
            def _(e):
                run(e, lists["pool"])

            @block.sync
            def _(e):
                run(e, lists["sp"])


class Ctx:
    pass


_uid = [0]


def _sb(nc, st, name, shape, dt):
    _uid[0] += 1
    return st.enter_context(nc.sbuf_tensor("sb%d_%s" % (_uid[0], name), shape, dt))


def _ps(nc, st, name, shape, dt):
    _uid[0] += 1
    return st.enter_context(nc.psum_tensor("ps%d_%s" % (_uid[0], name), shape, dt))


def norm_transpose(S, T, src_rows, col0, uid):
    xs, gb, xnb, ss, idb, pt, xnT = T["xs"], T["gb"], T["xnb"], T["ss"], T["idb"], T["pt"], T["xnT"]
    S.dma("sp", lambda e: e.dma_start(out=xs[:], in_=src_rows), writes=["xs"])
    S.op("act", lambda e: e.activation(out=xnb[:], in_=xs[:], func=AF.Square, accum_out=ss[:, 0:1]),
         reads=["xs"], writes=["xnb", "ss0"])
    S.op("act", lambda e: e.activation(out=ss[:, 1:2], in_=ss[:, 0:1], func=AF.Sqrt, scale=1.0 / D, bias=T["epsb"][:, 0:1]),
         reads=["ss0", "epsb"], writes=["ss1"])
    S.op("dve", lambda e: e.reciprocal(out=ss[:, 2:3], in_=ss[:, 1:2]), reads=["ss1"], writes=["ss2"])
    S.op("dve", lambda e: e.scalar_tensor_tensor(out=xnb[:], in0=xs[:], scalar=ss[:, 2:3], in1=gb[:],
                                                 op0=ALU.mult, op1=ALU.mult),
         reads=["xs", "ss2", "gb"], writes=["xnb"])
    for g in range(4):
        p = pt[g % 2]
        pk = "pt%d" % (g % 2)
        for k in range(8):
            kc = g * 8 + k
            S.op("pe", lambda e, p=p, k=k, kc=kc: e.transpose(out=p[:, k * 128:(k + 1) * 128],
                                                               in_=xnb[:, kc * 128:(kc + 1) * 128],
                                                               identity=idb[:]),
                 reads=["xnb", "idb"], writes=[pk])
        eng = "act" if g % 2 == 0 else "dve"
        if eng == "act":
            S.op("act", lambda e, p=p, g=g: e.activation(
                out=xnT[:, g * 8:(g + 1) * 8, col0:col0 + 128],
                in_=p[:].rearrange("p (k t) -> p k t", k=8), func=AF.Copy),
                reads=[pk], writes=[("xnT", uid, g)])
        else:
            S.op("dve", lambda e, p=p, g=g: e.tensor_copy(
                out=xnT[:, g * 8:(g + 1) * 8, col0:col0 + 128],
                in_=p[:].rearrange("p (k t) -> p k t", k=8)),
                reads=[pk], writes=[("xnT", uid, g)])


def xnT_keys(uids):
    return [("xnT", u, g) for u in uids for g in range(4)]


def stage1(nc, S, C, tiles=(0, 1, 2, 3)):
    with ExitStack() as st:
        T = {}
        T["xnT"] = _sb(nc, st, "xnT", [128, 32, 1024], BF16)
        T["xs"] = _sb(nc, st, "xs", [128, D], F32)
        T["gb"] = _sb(nc, st, "gb", [128, D], F32)
        T["xnb"] = _sb(nc, st, "xnb", [128, D], BF16)
        T["ss"] = _sb(nc, st, "ss", [128, 4], F32)
        T["idb"] = _sb(nc, st, "idb", [128, 128], BF16)
        T["epsb"] = _sb(nc, st, "epsb", [128, 1], F32)
        S.op("dve", lambda e: e.memset(T["epsb"][:], EPS), writes=["epsb"])
        wst = [_sb(nc, st, "wst%d" % i, [128, 32, 128], F32) for i in range(2)]
        wb = [_sb(nc, st, "wb%d" % i, [128, 32, 128], BF16) for i in range(2)]
        ob = [_sb(nc, st, "ob%d" % i, [128, 512], BF16) for i in range(2)]
        of = [_sb(nc, st, "of%d" % i, [128, 512], F32) for i in range(2)]
        vt = [_sb(nc, st, "vt%d" % i, [128, 4, 128], BF16) for i in range(2)]
        pm = [_ps(nc, st, "pm%d" % i, [128, 512], F32) for i in range(4)]
        T["pt"] = [_ps(nc, st, "pt%d" % i, [128, 1024], BF16) for i in range(2)]
        pv = [_ps(nc, st, "pv%d" % i, [128, 1024], BF16) for i in range(2)]
        xnT, idb = T["xnT"], T["idb"]

        S.dma("sp", lambda e: e.dma_start(out=T["gb"][:], in_=C.g_mix.partition_broadcast(128)), writes=["gb"])
        S.dma("sp", lambda e: e.dma_start(out=idb[:], in_=C.ident_bf), writes=["idb"])

        cnt = {"w": 0, "pm": 0, "ob": 0, "of": 0, "vt": 0}
        w_in = C.w_in
        for tt in tiles:
            tok0 = tt * 1024
            for tb in range(8):
                norm_transpose(S, T, C.x[tok0 + tb * 128: tok0 + (tb + 1) * 128, :], tb * 128, tb)
            xkeys = xnT_keys(range(8))
            plan = []
            kv_only = (tt == 3)
            for fb in range(160):
                if fb < 16:
                    if not kv_only:
                        plan.append((fb, "qk", C.qaT, fb))
                elif fb < 32:
                    plan.append((fb, "qk", C.kaT, fb - 16))
                elif fb < 48:
                    plan.append((fb, "v", C.va, fb - 32))
                elif fb < 64:
                    if not kv_only:
                        plan.append((fb, "qk", C.qbT, fb - 48))
                elif fb < 80:
                    plan.append((fb, "qk", C.kbT, fb - 64))
                elif fb < 96:
                    plan.append((fb, "v", C.vb, fb - 80))
                elif fb < 128:
                    if not kv_only:
                        plan.append((fb, "gate", C.sga, fb - 96))
                else:
                    if not kv_only:
                        plan.append((fb, "gate", C.sgb, fb - 128))
            for (fb, kind, dst, db) in plan:
                wi = cnt["w"] % 2
                cnt["w"] += 1
                S.dma("sp", lambda e, wi=wi, fb=fb: e.dma_start(
                    out=wst[wi][:], in_=w_in[:, fb * 128:(fb + 1) * 128].rearrange("(kc p) f -> p kc f", p=128)),
                    writes=["wst%d" % wi])
                S.op("pool", lambda e, wi=wi: e.tensor_copy(out=wb[wi][:], in_=wst[wi][:]),
                     reads=["wst%d" % wi], writes=["wb%d" % wi])
                for half in range(2):
                    pi = cnt["pm"] % 4
                    cnt["pm"] += 1
                    for kc in range(32):
                        S.op("pe", lambda e, pi=pi, wi=wi, kc=kc, half=half: e.matmul(
                            out=pm[pi][:], lhsT=wb[wi][:, kc, :], rhs=xnT[:, kc, half * 512:(half + 1) * 512],
                            start=(kc == 0), stop=(kc == 31)),
                            reads=["wb%d" % wi] + (xkeys if kc in (0, 31) else []), writes=["pm%d" % pi])
                    c0 = tok0 + half * 512
                    if kind == "qk":
                        oi = cnt["ob"] % 2
                        cnt["ob"] += 1
                        S.op("act", lambda e, oi=oi, pi=pi: e.activation(out=ob[oi][:], in_=pm[pi][:], func=AF.Copy),
                             reads=["pm%d" % pi], writes=["ob%d" % oi])
                        S.dma("act", lambda e, oi=oi, dst=dst, db=db, c0=c0: e.dma_start(
                            out=dst[db * 128:(db + 1) * 128, c0:c0 + 512], in_=ob[oi][:]),
                            reads=["ob%d" % oi])
                    elif kind == "gate":
                        oi = cnt["of"] % 2
                        cnt["of"] += 1
                        S.op("act", lambda e, oi=oi, pi=pi: e.activation(out=of[oi][:], in_=pm[pi][:], func=AF.Sigmoid),
                             reads=["pm%d" % pi], writes=["of%d" % oi])
                        S.dma("act", lambda e, oi=oi, dst=dst, db=db, c0=c0: e.dma_start(
                            out=dst[db * 128:(db + 1) * 128, c0:c0 + 512], in_=of[oi][:]),
                            reads=["of%d" % oi])
                    else:
                        oi = cnt["ob"] % 2
                        cnt["ob"] += 1
                        S.op("act", lambda e, oi=oi, pi=pi: e.activation(out=ob[oi][:], in_=pm[pi][:], func=AF.Copy),
                             reads=["pm%d" % pi], writes=["ob%d" % oi])
                        vi = cnt["vt"] % 2
                        cnt["vt"] += 1
                        for t in range(4):
                            S.op("pe", lambda e, oi=oi, vi=vi, t=t: e.transpose(
                                out=pv[vi][:, t * 128:(t + 1) * 128], in_=ob[oi][:, t * 128:(t + 1) * 128],
                                identity=idb[:]), reads=["ob%d" % oi, "idb"], writes=["pv%d" % vi])
                        S.op("dve", lambda e, vi=vi: e.tensor_copy(
                            out=vt[vi][:], in_=pv[vi][:, 0:512].rearrange("p (t c) -> p t c", t=4)),
                            reads=["pv%d" % vi], writes=["vt%d" % vi])
                        S.dma("act", lambda e, vi=vi, dst=dst, db=db, c0=c0: e.dma_start(
                            out=dst[c0:c0 + 512, db * 128:(db + 1) * 128].rearrange("(t p) c -> p t c", p=128),
                            in_=vt[vi][:]), reads=["vt%d" % vi])
        S.barrier()
        S.flush()


def stage2(nc, S, C, seqs=((0, 2048, 0), (2048, 1024, 2048)), a_heads=range(16), b_heads=range(8)):
    with ExitStack() as st:
        idb = _sb(nc, st, "idb2", [128, 128], BF16)
        abt = _sb(nc, st, "abt", [128, ABW], F32)
        lct = _sb(nc, st, "lct", [128, ABW], F32)
        tb = [_sb(nc, st, "tb%d" % i, [128, ABW], F32) for i in range(2)]
        qT = [_sb(nc, st, "qT%d" % i, [128, 2, 2048], BF16) for i in range(2)]
        kT = [_sb(nc, st, "kT%d" % i, [128, 2, 2048], BF16) for i in range(2)]
        vS = [_sb(nc, st, "vS%d" % i, [128, 16, 256], BF16) for i in range(2)]
        St = [_sb(nc, st, "St%d" % i, [128, 2048], F32) for i in range(2)]
        Pf = [_sb(nc, st, "Pf%d" % i, [128, 2048], F32) for i in range(2)]
        Pb = _sb(nc, st, "Pb", [128, 2048], BF16)
        aT = [_sb(nc, st, "aT%d" % i, [128, 16, 128], BF16) for i in range(2)]
        sm = _sb(nc, st, "sm", [128, 16], F32)
        oS = _sb(nc, st, "oS", [128, 256], F32)
        junk = _sb(nc, st, "junk2", [128, 256], F32)
        ob16 = _sb(nc, st, "ob16", [128, 256], BF16)
        oT = [_sb(nc, st, "oT%d" % i, [128, 2, 128], BF16) for i in range(2)]
        gsub = _sb(nc, st, "gsub", [128, 256], F32)
        lamt = _sb(nc, st, "lamt", [128, 512], F32)
        lamv = _sb(nc, st, "lamv", [128, 8], F32)
        epsb = _sb(nc, st, "epsb2", [128, 1], F32)
        ps = [_ps(nc, st, "ps%d" % i, [128, 512], F32) for i in range(4)]
        ptr = [_ps(nc, st, "ptr%d" % i, [128, 1024], BF16) for i in range(2)]
        po = _ps(nc, st, "po", [128, 512], F32)
        pot = _ps(nc, st, "pot", [128, 1024], BF16)

        S.dma("sp", lambda e: e.dma_start(out=idb[:], in_=C.ident_bf), writes=["idb"])
        S.dma("sp", lambda e: e.dma_start(out=abt[:], in_=C.abt), writes=["abt"])
        S.dma("sp", lambda e: e.dma_start(out=lct[:], in_=C.lct), writes=["lct"])
        S.dma("sp", lambda e: e.dma_start(out=gsub[:], in_=C.g_subln.partition_broadcast(128)), writes=["gsub"])
        S.dma("sp", lambda e: e.dma_start(out=lamt[:], in_=C.lam.partition_broadcast(128)), writes=["lamt"])
        S.op("dve", lambda e: e.memset(epsb[:], EPS), writes=["epsb"])
        S.op("dve", lambda e: e.tensor_scalar_mul(out=gsub[:], in0=gsub[:], scalar1=1.0 - LAM_INIT),
             reads=["gsub"], writes=["gsub"])
        for j in range(2):
            S.op("dve", lambda e, j=j: e.tensor_tensor(out=junk[:, 0:128], in0=lamt[:, j * 256:j * 256 + 128],
                                                       in1=lamt[:, j * 256 + 128:j * 256 + 256], op=ALU.mult),
                 reads=["lamt"], writes=["junk"])
            S.op("dve", lambda e, j=j: e.tensor_reduce(out=lamv[:, j:j + 1], in_=junk[:, 0:128], axis=AX.X, op=ALU.add),
                 reads=["junk"], writes=["lamv%d" % j])
            S.op("act", lambda e, j=j: e.activation(out=lamv[:, 2 + j:3 + j], in_=lamv[:, j:j + 1], func=AF.Exp),
                 reads=["lamv%d" % j], writes=["lame%d" % j])
        S.op("dve", lambda e: e.tensor_tensor(out=lamv[:, 4:5], in0=lamv[:, 2:3], in1=lamv[:, 3:4], op=ALU.subtract),
             reads=["lame0", "lame1"], writes=["lamd"])
        S.op("dve", lambda e: e.tensor_scalar(out=lamv[:, 5:6], in0=lamv[:, 4:5], scalar1=LAM_INIT, scalar2=-1.0,
                                              op0=ALU.add, op1=ALU.mult), reads=["lamd"], writes=["nlam"])

        cnt = {"h": 0, "ps": 0, "aT": 0, "oT": 0}

        def head(kind, h, qbase, nq, kbase):
            b = cnt["h"] % 2
            cnt["h"] += 1
            nj = 1 if kind == "a" else 2
            ve = 128 if kind == "a" else 256
            if kind == "a":
                slope = 2.0 ** (-8.0 * (h + 1) / 16)
                qsrc, ksrc, vsrc, odst = C.qaT, C.kaT, C.va, C.oaT
            else:
                slope = 2.0 ** (-8.0 * (h + 1) / 8)
                qsrc, ksrc, vsrc, odst = C.qbT, C.kbT, C.vb, C.obT
            f0 = h * 128 * nj
            for j in range(nj):
                S.dma("sp", lambda e, j=j: e.dma_start(out=qT[b][:, j, 0:nq],
                                                        in_=qsrc[f0 + j * 128:f0 + (j + 1) * 128, qbase:qbase + nq]),
                      writes=["qT%d" % b])
                S.dma("sp", lambda e, j=j: e.dma_start(out=kT[b][:, j, :],
                                                        in_=ksrc[f0 + j * 128:f0 + (j + 1) * 128, kbase:kbase + 2048]),
                      writes=["kT%d" % b])
            S.dma("sp", lambda e: e.dma_start(
                out=vS[b][:, :, 0:ve],
                in_=vsrc[kbase:kbase + 2048, h * ve:(h + 1) * ve].rearrange("(kb p) c -> p kb c", p=128)),
                writes=["vS%d" % b])
            if kind == "a":
                S.op("dve", lambda e: e.scalar_tensor_tensor(out=tb[b][:], in0=abt[:], scalar=-slope, in1=lct[:],
                                                             op0=ALU.mult, op1=ALU.add),
                     reads=["abt", "lct"], writes=["tb%d" % b])
            else:
                S.op("dve", lambda e: e.tensor_scalar_mul(out=tb[b][:], in0=abt[:], scalar1=-slope),
                     reads=["abt"], writes=["tb%d" % b])
            def qblock(i):
                off = 1920 - 128 * i
                for j in range(nj):
                    for c in range(4):
                        r = cnt["ps"] % 4
                        cnt["ps"] += 1
                        S.op("pe", lambda e, r=r, j=j, c=c: e.matmul(
                            out=ps[r][:], lhsT=qT[b][:, j, i * 128:(i + 1) * 128],
                            rhs=kT[b][:, j, c * 512:(c + 1) * 512], start=True, stop=True),
                            reads=["qT%d" % b, "kT%d" % b], writes=["ps%d" % r])
                        S.op("dve", lambda e, r=r, j=j, c=c: e.scalar_tensor_tensor(
                            out=St[j][:, c * 512:(c + 1) * 512], in0=ps[r][:], scalar=SCALE,
                            in1=tb[b][:, off + c * 512:off + (c + 1) * 512], op0=ALU.mult, op1=ALU.add),
                            reads=["ps%d" % r, "tb%d" % b], writes=["St%d" % j])
                    S.op("dve", lambda e, j=j: e.tensor_reduce(out=sm[:, j:j + 1], in_=St[j][:], axis=AX.X, op=ALU.max),
                         reads=["St%d" % j], writes=["mx%d" % j])
                    S.op("dve", lambda e, j=j: e.tensor_scalar_mul(out=sm[:, 2 + j:3 + j], in0=sm[:, j:j + 1], scalar1=-1.0),
                         reads=["mx%d" % j], writes=["nm%d" % j])
                    if kind == "a":
                        S.op("act", lambda e: e.activation(out=Pb[:], in_=St[0][:], func=AF.Exp, bias=sm[:, 2:3],
                                                           accum_out=sm[:, 4:5]),
                             reads=["St0", "nm0"], writes=["Pb", "rs0"])
                    else:
                        S.op("act", lambda e, j=j: e.activation(out=Pf[j][:], in_=St[j][:], func=AF.Exp,
                                                                bias=sm[:, 2 + j:3 + j], accum_out=sm[:, 4 + j:5 + j]),
                             reads=["St%d" % j, "nm%d" % j], writes=["Pf%d" % j, "rs%d" % j])
                if kind == "b":
                    S.op("dve", lambda e: e.reciprocal(out=sm[:, 6:8], in_=sm[:, 4:6]), reads=["rs0", "rs1"], writes=["rinv"])
                    S.op("dve", lambda e: e.scalar_tensor_tensor(out=sm[:, 8:9], in0=sm[:, 4:5], scalar=sm[:, 7:8],
                                                                 in1=lamv[:, 5:6], op0=ALU.mult, op1=ALU.mult),
                         reads=["rs0", "rinv", "nlam"], writes=["ratio"])
                    S.op("dve", lambda e: e.scalar_tensor_tensor(out=Pb[:], in0=Pf[1][:], scalar=sm[:, 8:9], in1=Pf[0][:],
                                                                 op0=ALU.mult, op1=ALU.add),
                         reads=["Pf0", "Pf1", "ratio"], writes=["Pb"])
                else:
                    S.op("dve", lambda e: e.reciprocal(out=sm[:, 6:7], in_=sm[:, 4:5]), reads=["rs0"], writes=["rinv"])
                ai = cnt["aT"] % 2
                cnt["aT"] += 1
                for g in range(2):
                    for k in range(8):
                        kb = g * 8 + k
                        S.op("pe", lambda e, g=g, k=k, kb=kb: e.transpose(
                            out=ptr[g][:, k * 128:(k + 1) * 128], in_=Pb[:, kb * 128:(kb + 1) * 128], identity=idb[:]),
                            reads=["Pb", "idb"], writes=["ptr%d" % g])
                    S.op("act", lambda e, g=g: e.activation(out=aT[ai][:, g * 8:(g + 1) * 8, :],
                                                            in_=ptr[g][:].rearrange("p (k t) -> p k t", k=8), func=AF.Copy),
                         reads=["ptr%d" % g], writes=[("aT", ai, g)])
                for kb in range(16):
                    S.op("pe", lambda e, kb=kb: e.matmul(out=po[:, 0:ve], lhsT=aT[ai][:, kb, :], rhs=vS[b][:, kb, 0:ve],
                                                         start=(kb == 0), stop=(kb == 15)),
                         reads=[("aT", ai, kb // 8), "vS%d" % b], writes=["po"])
                oi = cnt["oT"] % 2
                cnt["oT"] += 1
                qc = qbase + i * 128
                if kind == "a":
                    S.op("act", lambda e: e.activation(out=ob16[:, 0:128], in_=po[:, 0:128], func=AF.Copy, scale=sm[:, 6:7]),
                         reads=["po", "rinv"], writes=["ob16"])
                    S.op("pe", lambda e: e.transpose(out=pot[:, 0:128], in_=ob16[:, 0:128], identity=idb[:]),
                         reads=["ob16", "idb"], writes=["pot"])
                    S.op("dve", lambda e: e.tensor_copy(out=oT[oi][:, 0, :], in_=pot[:, 0:128]),
                         reads=["pot"], writes=["oT%d" % oi])
                    S.dma("act", lambda e: e.dma_start(out=odst[h * 128:(h + 1) * 128, qc:qc + 128], in_=oT[oi][:, 0, :]),
                          reads=["oT%d" % oi])
                else:
                    S.op("act", lambda e: e.activation(out=oS[:], in_=po[:, 0:256], func=AF.Copy, scale=sm[:, 6:7]),
                         reads=["po", "rinv"], writes=["oS"])
                    S.op("act", lambda e: e.activation(out=junk[:], in_=oS[:], func=AF.Square, accum_out=sm[:, 9:10]),
                         reads=["oS"], writes=["junk", "ssq"])
                    S.op("act", lambda e: e.activation(out=sm[:, 10:11], in_=sm[:, 9:10], func=AF.Sqrt, scale=1.0 / 256,
                                                       bias=epsb[:, 0:1]), reads=["ssq", "epsb"], writes=["srt"])
                    S.op("dve", lambda e: e.reciprocal(out=sm[:, 11:12], in_=sm[:, 10:11]), reads=["srt"], writes=["rstd"])
                    S.op("dve", lambda e: e.scalar_tensor_tensor(out=ob16[:], in0=oS[:], scalar=sm[:, 11:12], in1=gsub[:],
                                                                 op0=ALU.mult, op1=ALU.mult),
                         reads=["oS", "rstd", "gsub"], writes=["ob16"])
                    for t in range(2):
                        S.op("pe", lambda e, t=t: e.transpose(out=pot[:, t * 128:(t + 1) * 128],
                                                              in_=ob16[:, t * 128:(t + 1) * 128], identity=idb[:]),
                             reads=["ob16", "idb"], writes=["pot"])
                    S.op("dve", lambda e: e.tensor_copy(out=oT[oi][:], in_=pot[:, 0:256].rearrange("p (t c) -> p t c", t=2)),
                         reads=["pot"], writes=["oT%d" % oi])
                    S.dma("act", lambda e: e.dma_start(
                        out=odst[h * 256:(h + 1) * 256, qc:qc + 128].rearrange("(t p) c -> p t c", p=128), in_=oT[oi][:]),
                        reads=["oT%d" % oi])

            for i in range(nq // 128):
                qblock(i)

        for (qbase, nq, kbase) in seqs:
            for h in a_heads:
                head("a", h, qbase, nq, kbase)
            for h in b_heads:
                head("b", h, qbase, nq, kbase)
        S.barrier()
        S.flush()


def stage3(nc, S, C, tiles=range(6)):
    with ExitStack() as st:
        oat = _sb(nc, st, "oat", [128, 16, 512], BF16)
        obt = _sb(nc, st, "obt", [128, 16, 512], BF16)
        mT = _sb(nc, st, "mT", [128, 32, 512], BF16)
        wst = [_sb(nc, st, "wst%d" % i, [128, 32, 128], F32) for i in range(2)]
        wb = [_sb(nc, st, "wb%d" % i, [128, 32, 128], BF16) for i in range(2)]
        sg = [_sb(nc, st, "sg%d" % i, [128, 2, 512], F32) for i in range(2)]
        m1 = _sb(nc, st, "m1", [128, 512], F32)
        m2 = _sb(nc, st, "m2", [128, 512], F32)
        hf = [_sb(nc, st, "hf%d" % i, [128, 512], F32) for i in range(2)]
        xt = [_sb(nc, st, "xt%d" % i, [128, 4, 128], F32) for i in range(2)]
        ho = [_sb(nc, st, "ho%d" % i, [128, 4, 128], F32) for i in range(2)]
        idf = _sb(nc, st, "idf", [128, 128], F32)
        pm = [_ps(nc, st, "pm%d" % i, [128, 512], F32) for i in range(4)]
        pT = [_ps(nc, st, "pT%d" % i, [128, 512], F32) for i in range(2)]
        S.dma("sp", lambda e: e.dma_start(out=idf[:], in_=C.ident_f), writes=["idf"])
        cnt = {"w": 0, "pm": 0, "sg": 0, "hf": 0, "xt": 0, "pT": 0}
        for tt in tiles:
            c0 = tt * 512
            S.dma("sp", lambda e, c0=c0: e.dma_start(out=oat[:], in_=C.oaT[:, c0:c0 + 512].rearrange("(kc p) t -> p kc t", p=128)),
                  writes=["oat"])
            S.dma("sp", lambda e, c0=c0: e.dma_start(out=obt[:], in_=C.obT[:, c0:c0 + 512].rearrange("(kc p) t -> p kc t", p=128)),
                  writes=["obt"])
            for fb in range(32):
                wi = cnt["w"] % 2
                cnt["w"] += 1
                S.dma("sp", lambda e, wi=wi, fb=fb: e.dma_start(
                    out=wst[wi][:, 0:16, :], in_=C.w_pa[:, fb * 128:(fb + 1) * 128].rearrange("(kc p) f -> p kc f", p=128)),
                    writes=["wst%d" % wi])
                S.dma("sp", lambda e, wi=wi, fb=fb: e.dma_start(
                    out=wst[wi][:, 16:32, :], in_=C.w_pb[:, fb * 128:(fb + 1) * 128].rearrange("(kc p) f -> p kc f", p=128)),
                    writes=["wst%d" % wi])
                si = cnt["sg"] % 2
                cnt["sg"] += 1
                S.dma("sp", lambda e, si=si, fb=fb, c0=c0: e.dma_start(out=sg[si][:, 0, :], in_=C.sga[fb * 128:(fb + 1) * 128, c0:c0 + 512]),
                      writes=["sg%d" % si])
                S.dma("sp", lambda e, si=si, fb=fb, c0=c0: e.dma_start(out=sg[si][:, 1, :], in_=C.sgb[fb * 128:(fb + 1) * 128, c0:c0 + 512]),
                      writes=["sg%d" % si])
                S.op("pool", lambda e, wi=wi: e.tensor_copy(out=wb[wi][:], in_=wst[wi][:]),
                     reads=["wst%d" % wi], writes=["wb%d" % wi])
                pa = cnt["pm"] % 4
                pb = (cnt["pm"] + 1) % 4
                cnt["pm"] += 2
                for kc in range(16):
                    S.op("pe", lambda e, pa=pa, wi=wi, kc=kc: e.matmul(out=pm[pa][:], lhsT=wb[wi][:, kc, :], rhs=oat[:, kc, :],
                                                                        start=(kc == 0), stop=(kc == 15)),
                         reads=["wb%d" % wi, "oat"], writes=["pm%d" % pa])
                for kc in range(16):
                    S.op("pe", lambda e, pb=pb, wi=wi, kc=kc: e.matmul(out=pm[pb][:], lhsT=wb[wi][:, 16 + kc, :], rhs=obt[:, kc, :],
                                                                        start=(kc == 0), stop=(kc == 15)),
                         reads=["wb%d" % wi, "obt"], writes=["pm%d" % pb])
                S.op("dve", lambda e, pa=pa, si=si: e.tensor_tensor(out=m1[:], in0=pm[pa][:], in1=sg[si][:, 0, :], op=ALU.mult),
                     reads=["pm%d" % pa, "sg%d" % si], writes=["m1"])
                S.op("dve", lambda e, pb=pb, si=si: e.tensor_tensor(out=m2[:], in0=pm[pb][:], in1=sg[si][:, 1, :], op=ALU.mult),
                     reads=["pm%d" % pb, "sg%d" % si], writes=["m2"])
                S.op("dve", lambda e, fb=fb: e.tensor_tensor(out=mT[:, fb, :], in0=m1[:], in1=m2[:], op=ALU.add),
                     reads=["m1", "m2"], writes=[("mT", fb)])
            mkeys = [("mT", fb) for fb in range(32)]
            for fb in range(32):
                wi = cnt["w"] % 2
                cnt["w"] += 1
                S.dma("sp", lambda e, wi=wi, fb=fb: e.dma_start(
                    out=wst[wi][:], in_=C.w_out[:, fb * 128:(fb + 1) * 128].rearrange("(kc p) f -> p kc f", p=128)),
                    writes=["wst%d" % wi])
                S.op("pool", lambda e, wi=wi: e.tensor_copy(out=wb[wi][:], in_=wst[wi][:]),
                     reads=["wst%d" % wi], writes=["wb%d" % wi])
                xi = cnt["xt"] % 2
                cnt["xt"] += 1
                S.dma("sp", lambda e, xi=xi, fb=fb, c0=c0: e.dma_start(
                    out=xt[xi][:], in_=C.x[c0:c0 + 512, fb * 128:(fb + 1) * 128].rearrange("(t p) c -> p t c", p=128)),
                    writes=["xt%d" % xi])
                pi = cnt["pm"] % 4
                cnt["pm"] += 1
                for kc in range(32):
                    S.op("pe", lambda e, pi=pi, wi=wi, kc=kc: e.matmul(out=pm[pi][:], lhsT=wb[wi][:, kc, :], rhs=mT[:, kc, :],
                                                                        start=(kc == 0), stop=(kc == 31)),
                         reads=["wb%d" % wi] + (mkeys if kc in (0, 31) else []), writes=["pm%d" % pi])
                hi = cnt["hf"] % 2
                cnt["hf"] += 1
                S.op("act", lambda e, hi=hi, pi=pi: e.activation(out=hf[hi][:], in_=pm[pi][:], func=AF.Copy),
                     reads=["pm%d" % pi], writes=["hf%d" % hi])
                ti = cnt["pT"] % 2
                cnt["pT"] += 1
                for t in range(4):
                    S.op("pe", lambda e, hi=hi, ti=ti, t=t: e.transpose(out=pT[ti][:, t * 128:(t + 1) * 128],
                                                                        in_=hf[hi][:, t * 128:(t + 1) * 128], identity=idf[:]),
                         reads=["hf%d" % hi, "idf"], writes=["pT%d" % ti])
                S.op("dve", lambda e, xi=xi, ti=ti: e.tensor_tensor(out=ho[xi][:], in0=pT[ti][:].rearrange("p (t c) -> p t c", t=4),
                                                                    in1=xt[xi][:], op=ALU.add),
                     reads=["pT%d" % ti, "xt%d" % xi], writes=["ho%d" % xi])
                S.dma("act", lambda e, xi=xi, fb=fb, c0=c0: e.dma_start(
                    out=C.h[c0:c0 + 512, fb * 128:(fb + 1) * 128].rearrange("(t p) c -> p t c", p=128), in_=ho[xi][:]),
                    reads=["ho%d" % xi])
        S.barrier()
        S.flush()


def stage4(nc, S, C, tiles=range(6)):
    with ExitStack() as st:
        T = {}
        T["xnT"] = _sb(nc, st, "xnT", [128, 32, 512], BF16)
        T["xs"] = _sb(nc, st, "xs", [128, D], F32)
        T["gb"] = _sb(nc, st, "gb", [128, D], F32)
        T["xnb"] = _sb(nc, st, "xnb", [128, D], BF16)
        T["ss"] = _sb(nc, st, "ss", [128, 4], F32)
        T["idb"] = _sb(nc, st, "idb", [128, 128], BF16)
        T["epsb"] = _sb(nc, st, "epsb", [128, 1], F32)
        idf = _sb(nc, st, "idf", [128, 128], F32)
        wst = [_sb(nc, st, "wst%d" % i, [128, 32, 128], F32) for i in range(2)]
        wb = [_sb(nc, st, "wb%d" % i, [128, 32, 128], BF16) for i in range(2)]
        pq = _sb(nc, st, "pq", [128, 16, 512], BF16)
        skl = _sb(nc, st, "skl", [128, 16, 128], F32)
        KT = _sb(nc, st, "KT", [128, 16, 128], BF16)
        sc = _sb(nc, st, "sc", [128, 16, 128], F32)
        tmp = _sb(nc, st, "tmp", [128, 16, 128], F32)
        v16 = _sb(nc, st, "v16", [128, 16, 16], F32)
        cand = [_sb(nc, st, "cand%d" % i, [128, 256], F32) for i in range(3)]
        c24 = _sb(nc, st, "c24", [128, 8, 24], F32)
        e16 = _sb(nc, st, "e16", [128, 16], F32)
        rt = _sb(nc, st, "rt", [128, 8, 8], F32)
        P2 = [_sb(nc, st, "P2_%d" % i, [128, 8, 128], F32) for i in range(2)]
        TH = [_sb(nc, st, "TH_%d" % i, [128, 8, 128], F32) for i in range(2)]
        WG = [_sb(nc, st, "WG_%d" % i, [128, 8, 128], F32) for i in range(2)]
        pm = [_ps(nc, st, "pm%d" % i, [128, 512], F32) for i in range(4)]
        T["pt"] = [_ps(nc, st, "pt%d" % i, [128, 1024], BF16) for i in range(2)]
        pk = _ps(nc, st, "pk", [128, 512], F32)
        xnT, idb = T["xnT"], T["idb"]
        S.dma("sp", lambda e: e.dma_start(out=T["gb"][:], in_=C.g_ffn.partition_broadcast(128)), writes=["gb"])
        S.dma("sp", lambda e: e.dma_start(out=idb[:], in_=C.ident_bf), writes=["idb"])
        S.dma("sp", lambda e: e.dma_start(out=idf[:], in_=C.ident_f), writes=["idf"])
        S.op("dve", lambda e: e.memset(T["epsb"][:], EPS), writes=["epsb"])
        S.dma("sp", lambda e: e.dma_start(out=skl[:].rearrange("p (h s) c -> p h s c", s=2)[:, :, 0, :],
                                          in_=C.sk1.rearrange("h n c -> n h c")), writes=["skl"])
        S.dma("sp", lambda e: e.dma_start(out=skl[:].rearrange("p (h s) c -> p h s c", s=2)[:, :, 1, :],
                                          in_=C.sk2.rearrange("h n c -> n h c")), writes=["skl"])
        for g in range(4):
            for k in range(4):
                hs = g * 4 + k
                S.op("pe", lambda e, k=k, hs=hs: e.transpose(out=pk[:, k * 128:(k + 1) * 128], in_=skl[:, hs, :], identity=idf[:]),
                     reads=["skl", "idf"], writes=["pk"])
            S.op("dve", lambda e, g=g: e.tensor_copy(out=KT[:, g * 4:(g + 1) * 4, :], in_=pk[:].rearrange("p (k n) -> p k n", k=4)),
                 reads=["pk"], writes=["KT"])
        cnt = {"w": 0, "pm": 0, "tb": 0}

        def tile(tt):
            tok0 = tt * 512
            for tb in range(4):
                norm_transpose(S, T, C.h[tok0 + tb * 128: tok0 + (tb + 1) * 128, :], tb * 128, tb)
            xkeys = xnT_keys(range(4))
            S.dma("act", lambda e: e.dma_start(
                out=C.xn2T[:, tok0:tok0 + 512].rearrange("(kc p) t -> p kc t", p=128), in_=xnT[:]), reads=xkeys)
            for fb in range(16):
                wi = cnt["w"] % 2
                cnt["w"] += 1
                S.dma("sp", lambda e, wi=wi, fb=fb: e.dma_start(
                    out=wst[wi][:], in_=C.w_query[:, fb * 128:(fb + 1) * 128].rearrange("(kc p) f -> p kc f", p=128)),
                    writes=["wst%d" % wi])
                S.op("pool", lambda e, wi=wi: e.tensor_copy(out=wb[wi][:], in_=wst[wi][:]),
                     reads=["wst%d" % wi], writes=["wb%d" % wi])
                for half in range(1):
                    pi = cnt["pm"] % 4
                    cnt["pm"] += 1
                    for kc in range(32):
                        S.op("pe", lambda e, pi=pi, wi=wi, kc=kc, half=half: e.matmul(
                            out=pm[pi][:], lhsT=wb[wi][:, kc, :], rhs=xnT[:, kc, half * 512:(half + 1) * 512],
                            start=(kc == 0), stop=(kc == 31)),
                            reads=["wb%d" % wi] + (xkeys if kc in (0, 31) else []), writes=["pm%d" % pi])
                    S.op("act", lambda e, pi=pi, fb=fb, half=half: e.activation(
                        out=pq[:, fb, half * 512:(half + 1) * 512], in_=pm[pi][:], func=AF.Copy),
                        reads=["pm%d" % pi], writes=[("pq", fb)])
            pqkeys = [("pq", fb) for fb in range(16)]

            def route(tb):
                ri = cnt["tb"] % 2
                cnt["tb"] += 1
                r0 = tok0 + tb * 128
                for g in range(4):
                    pi = cnt["pm"] % 4
                    cnt["pm"] += 1
                    for k in range(4):
                        hs = g * 4 + k
                        S.op("pe", lambda e, pi=pi, k=k, hs=hs: e.matmul(
                            out=pm[pi][:, k * 128:(k + 1) * 128], lhsT=pq[:, hs, tb * 128:(tb + 1) * 128], rhs=KT[:, hs, :],
                            start=True, stop=True), reads=pqkeys + ["KT"], writes=["pm%d" % pi])
                    S.op("act", lambda e, pi=pi, g=g: e.activation(
                        out=sc[:, g * 4:(g + 1) * 4, :], in_=pm[pi][:].rearrange("p (k n) -> p k n", k=4), func=AF.Copy),
                        reads=["pm%d" % pi], writes=[("sc", g)])
                sck = [("sc", g) for g in range(4)]
                for hs in range(16):
                    S.op("dve", lambda e, hs=hs: e.max(out=v16[:, hs, 0:8], in_=sc[:, hs, :]),
                         reads=sck, writes=[("v16a", hs)])
                    S.op("dve", lambda e, hs=hs: e.match_replace(out=tmp[:, hs, :], in_to_replace=v16[:, hs, 0:8],
                                                                 in_values=sc[:, hs, :], imm_value=-1e30),
                         reads=[("v16a", hs)] + sck, writes=[("tmp", hs)])
                    S.op("dve", lambda e, hs=hs: e.max(out=v16[:, hs, 8:16], in_=tmp[:, hs, :]),
                         reads=[("tmp", hs)], writes=[("v16b", hs)])
                for h in range(8):
                    S.op("dve", lambda e, h=h: e.tensor_tensor(
                        out=cand[0][:].rearrange("p (a b) -> p a b", a=16),
                        in0=v16[:, 2 * h, :].unsqueeze(2).to_broadcast([128, 16, 16]),
                        in1=v16[:, 2 * h + 1, :].unsqueeze(1).to_broadcast([128, 16, 16]), op=ALU.add),
                        reads=[("v16a", 2 * h), ("v16b", 2 * h), ("v16a", 2 * h + 1), ("v16b", 2 * h + 1)], writes=["cand0"])
                    S.op("dve", lambda e, h=h: e.max(out=c24[:, h, 0:8], in_=cand[0][:]), reads=["cand0"], writes=["c24a"])
                    S.op("dve", lambda e, h=h: e.match_replace(out=cand[1][:], in_to_replace=c24[:, h, 0:8], in_values=cand[0][:],
                                                               imm_value=-1e30), reads=["cand0", "c24a"], writes=["cand1"])
                    S.op("dve", lambda e, h=h: e.max(out=c24[:, h, 8:16], in_=cand[1][:]), reads=["cand1"], writes=["c24b"])
                    S.op("dve", lambda e, h=h: e.match_replace(out=cand[2][:], in_to_replace=c24[:, h, 8:16], in_values=cand[1][:],
                                                               imm_value=-1e30), reads=["cand1", "c24b"], writes=["cand2"])
                    S.op("dve", lambda e, h=h: e.max(out=c24[:, h, 16:24], in_=cand[2][:]), reads=["cand2"], writes=[("c24", h)])
                ck = [("c24", h) for h in range(8)] + ["c24a", "c24b"]
                vk = [("v16a", hs) for hs in range(16)]
                S.op("dve", lambda e: e.tensor_tensor(out=rt[:, :, 0], in0=c24[:, :, 15], in1=c24[:, :, 16], op=ALU.add),
                     reads=ck, writes=["rt0"])
                S.op("dve", lambda e: e.tensor_scalar_mul(out=rt[:, :, 0], in0=rt[:, :, 0], scalar1=0.5), reads=["rt0"], writes=["rt0"])
                S.op("dve", lambda e: e.tensor_scalar_mul(out=rt[:, :, 1], in0=c24[:, :, 0], scalar1=-1.0), reads=ck, writes=["rt1"])
                v16h = v16[:].rearrange("p (h s) k -> p h s k", s=2)
                S.op("dve", lambda e: e.tensor_tensor(out=rt[:, :, 2], in0=rt[:, :, 0], in1=v16h[:, :, 1, 0], op=ALU.subtract),
                     reads=["rt0"] + vk, writes=["rt2"])
                S.op("dve", lambda e: e.tensor_scalar_mul(out=rt[:, :, 3], in0=v16h[:, :, 0, 0], scalar1=-1.0), reads=vk, writes=["rt3"])
                S.op("dve", lambda e: e.tensor_scalar_mul(out=rt[:, :, 4], in0=v16h[:, :, 1, 0], scalar1=-1.0), reads=vk, writes=["rt4"])
                for h in range(8):
                    S.op("act", lambda e, h=h: e.activation(out=e16[:], in_=c24[:, h, 0:16], func=AF.Exp, bias=rt[:, h, 1:2],
                                                            accum_out=rt[:, h, 5:6]),
                         reads=ck + ["rt1"], writes=["e16", ("Z", h)])
                    S.op("act", lambda e, h=h: e.activation(out=P2[ri][:, h, :], in_=sc[:, 2 * h + 1, :], func=AF.Exp,
                                                            bias=rt[:, h, 4:5]), reads=sck + ["rt4"], writes=["P2_%d" % ri])
                    S.op("act", lambda e, h=h: e.activation(out=TH[ri][:, h, :], in_=sc[:, 2 * h, :], func=AF.Exp,
                                                            bias=rt[:, h, 2:3], scale=-1.0), reads=sck + ["rt2"], writes=["TH_%d" % ri])
                    S.op("act", lambda e, h=h: e.activation(out=WG[ri][:, h, :], in_=sc[:, 2 * h, :], func=AF.Exp,
                                                            bias=rt[:, h, 3:4]), reads=sck + ["rt3"], writes=[("WG", ri, h)])
                S.op("dve", lambda e: e.reciprocal(out=rt[:, :, 6], in_=rt[:, :, 5]), reads=[("Z", h) for h in range(8)], writes=["rZ"])
                for h in range(8):
                    S.op("dve", lambda e, h=h: e.tensor_scalar(out=tmp[:, h, :], in0=sc[:, 2 * h, :], scalar1=v16[:, 2 * h, 15:16],
                                                               scalar2=1e30, op0=ALU.is_lt, op1=ALU.mult),
                         reads=sck + [("v16b", 2 * h)], writes=[("tmp", h)])
                    S.op("dve", lambda e, h=h: e.tensor_tensor(out=TH[ri][:, h, :], in0=TH[ri][:, h, :], in1=tmp[:, h, :], op=ALU.add),
                         reads=[("tmp", h), "TH_%d" % ri], writes=["TH_%d" % ri])
                    S.op("dve", lambda e, h=h: e.scalar_tensor_tensor(out=P2[ri][:, h, :], in0=sc[:, 2 * h + 1, :],
                                                                      scalar=v16[:, 2 * h + 1, 15:16], in1=P2[ri][:, h, :],
                                                                      op0=ALU.is_ge, op1=ALU.mult),
                         reads=sck + [("v16b", 2 * h + 1), "P2_%d" % ri], writes=["P2_%d" % ri])
                for h in range(8):
                    S.op("dve", lambda e, h=h: e.tensor_scalar_mul(out=WG[ri][:, h, :], in0=WG[ri][:, h, :], scalar1=rt[:, h, 6:7]),
                         reads=[("WG", ri, h), "rZ"], writes=[("WG", ri, h)])
                S.dma("act", lambda e: e.dma_start(out=C.P2[r0:r0 + 128, :], in_=P2[ri][:].rearrange("p h n -> p (h n)")),
                      reads=["P2_%d" % ri])
                S.dma("act", lambda e: e.dma_start(out=C.TH[r0:r0 + 128, :], in_=TH[ri][:].rearrange("p h n -> p (h n)")),
                      reads=["TH_%d" % ri])
                S.dma("act", lambda e: e.dma_start(out=C.WG[r0:r0 + 128, :], in_=WG[ri][:].rearrange("p h n -> p (h n)")),
                      reads=[("WG", ri, h) for h in range(8)])

            for tb in range(4):
                route(tb)

        for tt in tiles:
            tile(tt)
        S.barrier()
        S.flush()


def stage5a(nc, S, C, egs=range(32), vblks=range(128)):
    with ExitStack() as st:
        us = [_sb(nc, st, "us%d" % i, [128, D], F32) for i in range(2)]
        ub = [_sb(nc, st, "ub%d" % i, [128, D], BF16) for i in range(2)]
        ut = [_sb(nc, st, "ut%d" % i, [128, 32, 512], BF16) for i in range(2)]
        idb = _sb(nc, st, "idb", [128, 128], BF16)
        pt = [_ps(nc, st, "pt%d" % i, [128, 1024], BF16) for i in range(4)]
        S.dma("sp", lambda e: e.dma_start(out=idb[:], in_=C.ident_bf), writes=["idb"])
        cnt = {"u": 0, "pt": 0, "ev": 0}
        for eg in egs:
            ui = eg % 2
            for sub in range(4):
                eb = eg * 4 + sub
                bi = cnt["u"] % 2
                cnt["u"] += 1
                S.dma("sp", lambda e, bi=bi, eb=eb: e.dma_start(out=us[bi][:], in_=C.eu[eb * 128:(eb + 1) * 128, :]),
                      writes=["us%d" % bi])
                S.op("pool", lambda e, bi=bi: e.tensor_copy(out=ub[bi][:], in_=us[bi][:]), reads=["us%d" % bi], writes=["ub%d" % bi])
                for g in range(4):
                    pi = cnt["pt"] % 4
                    cnt["pt"] += 1
                    for k in range(8):
                        kc = g * 8 + k
                        S.op("pe", lambda e, pi=pi, bi=bi, k=k, kc=kc: e.transpose(
                            out=pt[pi][:, k * 128:(k + 1) * 128], in_=ub[bi][:, kc * 128:(kc + 1) * 128], identity=idb[:]),
                            reads=["ub%d" % bi, "idb"], writes=["pt%d" % pi])
                    eng = "act" if cnt["ev"] % 2 == 0 else "dve"
                    cnt["ev"] += 1
                    if eng == "act":
                        S.op("act", lambda e, pi=pi, ui=ui, g=g, sub=sub: e.activation(
                            out=ut[ui][:, g * 8:(g + 1) * 8, sub * 128:(sub + 1) * 128],
                            in_=pt[pi][:].rearrange("p (k t) -> p k t", k=8), func=AF.Copy),
                            reads=["pt%d" % pi], writes=["ut%d" % ui])
                    else:
                        S.op("dve", lambda e, pi=pi, ui=ui, g=g, sub=sub: e.tensor_copy(
                            out=ut[ui][:, g * 8:(g + 1) * 8, sub * 128:(sub + 1) * 128],
                            in_=pt[pi][:].rearrange("p (k t) -> p k t", k=8)),
                            reads=["pt%d" % pi], writes=["ut%d" % ui])
            S.dma("act", lambda e, ui=ui, eg=eg: e.dma_start(
                out=C.UT[:, eg * 512:(eg + 1) * 512].rearrange("(kc p) n -> p kc n", p=128), in_=ut[ui][:]),
                reads=["ut%d" % ui])
        for vbk in vblks:
            bi = cnt["u"] % 2
            cnt["u"] += 1
            S.dma("sp", lambda e, bi=bi, vbk=vbk: e.dma_start(out=us[bi][:], in_=C.ev[vbk * 128:(vbk + 1) * 128, :]),
                  writes=["us%d" % bi])
            S.op("pool", lambda e, bi=bi: e.tensor_copy(out=ub[bi][:], in_=us[bi][:]), reads=["us%d" % bi], writes=["ub%d" % bi])
            S.dma("act", lambda e, bi=bi, vbk=vbk: e.dma_start(out=C.Vb[vbk * 128:(vbk + 1) * 128, :], in_=ub[bi][:]),
                  reads=["ub%d" % bi])
        S.barrier()
        S.flush()


def stage5b(nc, S, C, tiles=range(12), egs=range(32), dps=range(4), necs=16):
    with ExitStack() as st:
        xt = _sb(nc, st, "xt", [128, 32, 256], BF16)
        P2 = _sb(nc, st, "P2", [128, 2, 1024], F32)
        TH = _sb(nc, st, "TH", [128, 2, 1024], F32)
        WG = _sb(nc, st, "WG", [128, 2, 1024], F32)
        ut = [_sb(nc, st, "ut%d" % i, [128, 32, 512], BF16) for i in range(2)]
        gl = [_sb(nc, st, "gl%d" % i, [128, 512], F32) for i in range(2)]
        G = _sb(nc, st, "G", [128, 512], F32)
        mk = [_sb(nc, st, "mk%d" % i, [128, 8, 128], F32) for i in range(2)]
        Ab = [_sb(nc, st, "Ab%d" % i, [128, 512], BF16) for i in range(2)]
        at = [_sb(nc, st, "at%d" % i, [128, 4, 128], BF16) for i in range(2)]
        atc = [_sb(nc, st, "atc%d" % i, [128, 8, 256], BF16) for i in range(2)]
        vbc = [_sb(nc, st, "vbc%d" % i, [128, 8, 1024], BF16) for i in range(2)]
        ht = [_sb(nc, st, "ht%d" % i, [128, 512], F32) for i in range(2)]
        idb = _sb(nc, st, "idb", [128, 128], BF16)
        pm = [_ps(nc, st, "pm%d" % i, [128, 512], F32) for i in range(2)]
        pa = _ps(nc, st, "pa", [128, 1024], BF16)
        acc = [_ps(nc, st, "acc%d" % i, [128, 512], F32) for i in range(4)]
        S.dma("sp", lambda e: e.dma_start(out=idb[:], in_=C.ident_bf), writes=["idb"])
        cnt = {"ut": 0, "pm": 0, "gl": 0, "mk": 0, "Ab": 0, "at": 0, "c": 0, "ht": 0}

        def tile(tt):
            tok0 = tt * 256
            S.dma("sp", lambda e: e.dma_start(out=xt[:], in_=C.xn2T[:, tok0:tok0 + 256].rearrange("(kc p) t -> p kc t", p=128)),
                  writes=["xt"])
            for (dst, src, nm) in ((P2, C.P2, "P2"), (TH, C.TH, "TH"), (WG, C.WG, "WG")):
                S.dma("sp", lambda e, dst=dst, src=src: e.dma_start(
                    out=dst[:], in_=src[tok0:tok0 + 256, :].rearrange("(t p) n -> p t n", p=128)), writes=[nm])

            def hblock(eg, tkb):
                pi = cnt["pm"] % 2
                cnt["pm"] += 1
                ui = cnt["ut"] % 2
                for kc in range(32):
                    S.op("pe", lambda e, kc=kc: e.matmul(out=pm[pi][:], lhsT=xt[:, kc, tkb * 128:(tkb + 1) * 128], rhs=ut[ui][:, kc, :],
                                                         start=(kc == 0), stop=(kc == 31)),
                         reads=["xt", "ut%d" % ui], writes=["pm%d" % pi])
                gi = cnt["gl"] % 2
                cnt["gl"] += 1
                S.op("act", lambda e: e.activation(out=gl[gi][:], in_=pm[pi][:], func=AF.Gelu_apprx_tanh),
                     reads=["pm%d" % pi], writes=["gl%d" % gi])
                p2v = P2[:, tkb, :].rearrange("p (h n) -> p h n", h=8)
                thv = TH[:, tkb, :].rearrange("p (h n) -> p h n", h=8)
                wgv = WG[:, tkb, :].rearrange("p (h n) -> p h n", h=8)
                for bb in range(4):
                    b = eg * 4 + bb
                    mi = cnt["mk"] % 2
                    cnt["mk"] += 1
                    S.op("dve", lambda e, b=b, mi=mi: e.tensor_tensor(out=mk[mi][:], in0=p2v,
                                                                     in1=thv[:, :, b:b + 1].to_broadcast([128, 8, 128]), op=ALU.is_ge),
                         reads=["P2", "TH"], writes=["mk%d" % mi])
                    S.op("dve", lambda e, mi=mi: e.tensor_tensor(out=mk[mi][:], in0=mk[mi][:], in1=p2v, op=ALU.mult),
                         reads=["mk%d" % mi, "P2"], writes=["mk%d" % mi])
                    S.op("dve", lambda e, b=b, mi=mi: e.tensor_tensor(out=mk[mi][:], in0=mk[mi][:],
                                                                     in1=wgv[:, :, b:b + 1].to_broadcast([128, 8, 128]), op=ALU.mult),
                         reads=["mk%d" % mi, "WG"], writes=["mk%d" % mi])
                    S.op("dve", lambda e, bb=bb, mi=mi: e.tensor_reduce(out=G[:, bb * 128:(bb + 1) * 128],
                                                                       in_=mk[mi][:].rearrange("p h n -> p n h"), axis=AX.X, op=ALU.add),
                         reads=["mk%d" % mi], writes=[("G", bb)])
                ai = cnt["Ab"] % 2
                cnt["Ab"] += 1
                S.op("dve", lambda e: e.tensor_tensor(out=Ab[ai][:], in0=gl[gi][:], in1=G[:], op=ALU.mult),
                     reads=["gl%d" % gi] + [("G", bb) for bb in range(4)], writes=["Ab%d" % ai])
                for bb in range(4):
                    S.op("pe", lambda e, bb=bb: e.transpose(out=pa[:, bb * 128:(bb + 1) * 128], in_=Ab[ai][:, bb * 128:(bb + 1) * 128],
                                                            identity=idb[:]), reads=["Ab%d" % ai, "idb"], writes=["pa"])
                ti = cnt["at"] % 2
                cnt["at"] += 1
                S.op("act", lambda e: e.activation(out=at[ti][:], in_=pa[:, 0:512].rearrange("p (b t) -> p b t", b=4), func=AF.Copy),
                     reads=["pa"], writes=["at%d" % ti])
                S.dma("act", lambda e: e.dma_start(
                    out=C.AT[eg * 512:(eg + 1) * 512, tok0 + tkb * 128:tok0 + (tkb + 1) * 128].rearrange("(b p) t -> p b t", p=128),
                    in_=at[ti][:]), reads=["at%d" % ti], writes=[("AT", eg // 2)])

            for eg in egs:
                ui = cnt["ut"] % 2
                S.dma("sp", lambda e, ui=ui, eg=eg: e.dma_start(
                    out=ut[ui][:], in_=C.UT[:, eg * 512:(eg + 1) * 512].rearrange("(kc p) n -> p kc n", p=128)),
                    writes=["ut%d" % ui])
                for tkb in range(2):
                    hblock(eg, tkb)
                cnt["ut"] += 1

            def avpass(dp):
                for ec in range(necs):
                    ci = cnt["c"] % 2
                    cnt["c"] += 1
                    S.dma("sp", lambda e, ci=ci, ec=ec: e.dma_start(
                        out=atc[ci][:], in_=C.AT[ec * 1024:(ec + 1) * 1024, tok0:tok0 + 256].rearrange("(k p) t -> p k t", p=128)),
                        reads=[("AT", ec)], writes=["atc%d" % ci])
                    S.dma("sp", lambda e, ci=ci, ec=ec: e.dma_start(
                        out=vbc[ci][:], in_=C.Vb[ec * 1024:(ec + 1) * 1024, dp * 1024:(dp + 1) * 1024].rearrange("(k p) n -> p k n", p=128)),
                        writes=["vbc%d" % ci])
                    for k in range(8):
                        for tkb in range(2):
                            for dg in range(2):
                                ac = tkb * 2 + dg
                                S.op("pe", lambda e, ci=ci, k=k, tkb=tkb, dg=dg, ac=ac, ec=ec: e.matmul(
                                    out=acc[ac][:], lhsT=atc[ci][:, k, tkb * 128:(tkb + 1) * 128],
                                    rhs=vbc[ci][:, k, dg * 512:(dg + 1) * 512],
                                    start=(ec == 0 and k == 0), stop=(ec == necs - 1 and k == 7)),
                                    reads=["atc%d" % ci, "vbc%d" % ci], writes=["acc%d" % ac])
                for tkb in range(2):
                    for dg in range(2):
                        ac = tkb * 2 + dg
                        hi = cnt["ht"] % 2
                        cnt["ht"] += 1
                        r0 = tok0 + tkb * 128
                        c0 = dp * 1024 + dg * 512
                        S.dma("sp", lambda e, hi=hi, r0=r0, c0=c0: e.dma_start(out=ht[hi][:], in_=C.h[r0:r0 + 128, c0:c0 + 512]),
                              writes=["ht%d" % hi])
                        S.op("dve", lambda e, hi=hi, ac=ac: e.tensor_tensor(out=ht[hi][:], in0=acc[ac][:], in1=ht[hi][:], op=ALU.add),
                             reads=["acc%d" % ac, "ht%d" % hi], writes=["ht%d" % hi])
                        S.dma("act", lambda e, hi=hi, r0=r0, c0=c0: e.dma_start(out=C.hp[r0:r0 + 128, c0:c0 + 512], in_=ht[hi][:]),
                              reads=["ht%d" % hi])

            for dp in dps:
                avpass(dp)

        for tt in tiles:
            tile(tt)
        S.barrier()
        S.flush()


def stage6(nc, S, C, blks=range(24)):
    with ExitStack() as st:
        xs = [_sb(nc, st, "xs%d" % i, [128, D], F32) for i in range(2)]
        yo = [_sb(nc, st, "yo%d" % i, [128, D], F32) for i in range(2)]
        gb = _sb(nc, st, "gb", [128, D], F32)
        ss = _sb(nc, st, "ss", [128, 4], F32)
        epsb = _sb(nc, st, "epsb", [128, 1], F32)
        S.dma("sp", lambda e: e.dma_start(out=gb[:], in_=C.g_final.partition_broadcast(128)), writes=["gb"])
        S.op("dve", lambda e: e.memset(epsb[:], EPS), writes=["epsb"])
        for n, tb in enumerate(blks):
            i = n % 2
            S.dma("sp", lambda e, i=i, tb=tb: e.dma_start(out=xs[i][:], in_=C.hp[tb * 128:(tb + 1) * 128, :]), writes=["xs%d" % i])
            S.op("act", lambda e, i=i: e.activation(out=yo[i][:], in_=xs[i][:], func=AF.Square, accum_out=ss[:, 0:1]),
                 reads=["xs%d" % i], writes=["yo%d" % i, "ss0"])
            S.op("act", lambda e: e.activation(out=ss[:, 1:2], in_=ss[:, 0:1], func=AF.Sqrt, scale=1.0 / D, bias=epsb[:, 0:1]),
                 reads=["ss0", "epsb"], writes=["ss1"])
            S.op("dve", lambda e: e.reciprocal(out=ss[:, 2:3], in_=ss[:, 1:2]), reads=["ss1"], writes=["ss2"])
            S.op("dve", lambda e, i=i: e.scalar_tensor_tensor(out=yo[i][:], in0=xs[i][:], scalar=ss[:, 2:3], in1=gb[:],
                                                              op0=ALU.mult, op1=ALU.mult),
                 reads=["xs%d" % i, "ss2", "gb"], writes=["yo%d" % i])
            S.dma("act", lambda e, i=i, tb=tb: e.dma_start(out=C.y[tb * 128:(tb + 1) * 128, :], in_=yo[i][:]), reads=["yo%d" % i])
        S.barrier()
        S.flush()

def build(stages=("s1",), debug_outs=(), s1_tiles=(0, 1, 2, 3), s2_kw={}, s3_kw={}, s4_kw={}, s5a_kw={}, s5_kw={}, s6_kw={}):
    nc = bass.Bass("TRN2", target_bir_lowering=False)
    C = Ctx()

    def din(name, shape, dt=F32):
        return nc.dram_tensor(name, shape, dt, kind="ExternalInput").ap()

    def dscr(name, shape, dt):
        kind = "ExternalOutput" if name in debug_outs else "Internal"
        return nc.dram_tensor(name, shape, dt, kind=kind).ap()

    C.x = din("x", [TK, D])
    C.w_in = din("w_in", [D, INW])
    C.w_pa = din("w_proj_a", [AW, D])
    C.w_pb = din("w_proj_b", [BW, D])
    C.w_out = din("w_out", [D, D])
    C.g_mix = din("g_mix_norm", [1, D])
    C.lam = din("lam4", [1, 512])
    C.g_subln = din("g_subln", [1, 256])
    C.g_ffn = din("g_ffn_norm", [1, D])
    C.w_query = din("w_query", [D, 2048])
    C.sk1 = din("sub_keys_1", [8, 128, 128])
    C.sk2 = din("sub_keys_2", [8, 128, 128])
    C.eu = din("expert_u", [NE, D])
    C.ev = din("expert_v", [NE, D])
    C.g_final = din("g_final", [1, D])
    C.ident_bf = din("ident_bf", [128, 128], BF16)
    C.ident_f = din("ident_f", [128, 128], F32)
    C.abt = din("abt", [128, ABW])
    C.lct = din("lct", [128, ABW])
    C.y = nc.dram_tensor("y", [TQ, D], F32, kind="ExternalOutput").ap()

    C.qaT = dscr("qaT", [AW, TQ], BF16)
    C.kaT = dscr("kaT", [AW, TK], BF16)
    C.va = dscr("va", [TK, AW], BF16)
    C.qbT = dscr("qbT", [BW, TQ], BF16)
    C.kbT = dscr("kbT", [BW, TK], BF16)
    C.vb = dscr("vb", [TK, BW], BF16)
    C.sga = dscr("sga", [D, TQ], F32)
    C.sgb = dscr("sgb", [D, TQ], F32)
    C.oaT = dscr("oaT", [AW, TQ], BF16)
    C.obT = dscr("obT", [BW, TQ], BF16)
    C.h = dscr("h", [TQ, D], F32)
    C.xn2T = dscr("xn2T", [D, TQ], BF16)
    C.P2 = dscr("P2", [TQ, 1024], F32)
    C.TH = dscr("TH", [TQ, 1024], F32)
    C.WG = dscr("WG", [TQ, 1024], F32)
    C.UT = dscr("UT", [D, NE], BF16)
    C.Vb = dscr("Vb", [NE, D], BF16)
    C.AT = dscr("AT", [NE, TQ], BF16)
    C.hp = dscr("hp", [TQ, D], F32)

    with ExitStack() as st:
        S = Sched(nc, st)
        if "s1" in stages:
            stage1(nc, S, C, tiles=s1_tiles)
        if "s2" in stages:
            stage2(nc, S, C, **s2_kw)
        if "s3" in stages:
            stage3(nc, S, C, **s3_kw)
        if "s4" in stages:
            stage4(nc, S, C, **s4_kw)
        if "s5a" in stages:
            stage5a(nc, S, C, **s5a_kw)
        if "s5b" in stages:
            stage5b(nc, S, C, **s5_kw)
        if "s6" in stages:
            stage6(nc, S, C, **s6_kw)
        C.n_inst = S.n_inst
    return nc, C


def host_consts():
    p = np.arange(128)[:, None].astype(np.int64)
    c = np.arange(ABW)[None, :].astype(np.int64)
    delta = p - c + 1920
    ad = np.abs(delta)
    cnt = ((ad <= 64).astype(np.int64) + ((delta % 4 == 0) & (ad <= 256)).astype(np.int64)
           + ((delta % 16 == 0) & (ad <= 1024)).astype(np.int64))
    lct = np.where(cnt > 0, np.log(np.maximum(cnt, 1).astype(np.float64)), NEGBIG).astype(np.float32)
    return {
        "ident_bf": np.eye(128, dtype=np.float32).astype(ml_dtypes.bfloat16),
        "ident_f": np.eye(128, dtype=np.float32),
        "abt": ad.astype(np.float32),
        "lct": lct,
    }


def make_in_maps(inputs, n_cores=8):
    xp = np.asarray(inputs["x_prompt"])
    xsm = np.asarray(inputs["x_sample"])
    shared = {
        "w_in": np.ascontiguousarray(np.asarray(inputs["w_in"])[0]),
        "w_proj_a": np.ascontiguousarray(np.asarray(inputs["w_proj_a"])[0]),
        "w_proj_b": np.ascontiguousarray(np.asarray(inputs["w_proj_b"])[0]),
        "w_out": np.ascontiguousarray(np.asarray(inputs["w_out"])[0]),
        "g_mix_norm": np.ascontiguousarray(np.asarray(inputs["g_mix_norm"])[0:1]),
        "lam4": np.ascontiguousarray(np.concatenate([np.asarray(inputs[k])[0:1] for k in
                                                     ("lam_q1", "lam_k1", "lam_q2", "lam_k2")], axis=1)),
        "g_subln": np.ascontiguousarray(np.asarray(inputs["g_subln"])[0:1]),
        "g_ffn_norm": np.ascontiguousarray(np.asarray(inputs["g_ffn_norm"])[0:1]),
        "w_query": np.ascontiguousarray(np.asarray(inputs["w_query"])[0]),
        "sub_keys_1": np.ascontiguousarray(np.asarray(inputs["sub_keys_1"])[0]),
        "sub_keys_2": np.ascontiguousarray(np.asarray(inputs["sub_keys_2"])[0]),
        "expert_u": np.ascontiguousarray(np.asarray(inputs["expert_u"])[0]),
        "expert_v": np.ascontiguousarray(np.asarray(inputs["expert_v"])[0]),
        "g_final": np.ascontiguousarray(np.asarray(inputs["g_final"]).reshape(1, D)),
    }
    shared.update(host_consts())
    maps = []
    for c in range(n_cores):
        xb = xsm[c // 2]
        if c % 2 == 1:
            xb = xb[::-1]
        xall = np.ascontiguousarray(np.concatenate([xp[c], xb], axis=0).astype(np.float32))
        m = dict(shared)
        m["x"] = xall
        maps.append(m)
    return maps


def kernel(**inputs):
    nc, C = build(stages=ALL_STAGES)
    maps = make_in_maps(inputs)
    res = run_bass_kernel_spmd(nc, maps, core_ids=list(range(8)))
    y_prompt = np.zeros((8, SEQ, D), np.float32)
    y_sample = np.zeros((4, SEQ, D), np.float32)
    for c in range(8):
        y = np.asarray(res.results[c]["y"])
        y_prompt[c] = y[0:2048]
        if c % 2 == 0:
            y_sample[c // 2, 0:1024] = y[2048:3072]
        else:
            y_sample[c // 2, 1024:2048] = y[2048:3072][::-1]
    return (y_prompt, y_sample)
```

```python
import math
from contextlib import ExitStack

import numpy as np
import ml_dtypes

import concourse.bass as bass
import concourse.mybir as mybir
from concourse.alu_op_type import AluOpType as ALU
from concourse.bass_utils import run_bass_kernel_spmd

F32 = mybir.dt.float32
BF16 = mybir.dt.bfloat16
AF = mybir.ActivationFunctionType
AX = mybir.AxisListType

D = 4096
SEQ = 2048
TQ = 3072
TK = 4096
AW = 2048
BW = 2048
INW = 20480
NE = 16384
EPS = 1e-6
SCALE = 128 ** -0.5
LAM_INIT = 0.8 - 0.6 * math.exp(-0.0)
NEGBIG = -30000.0
ABW = 3968

ALL_STAGES = ("s1", "s2", "s3", "s4", "s5a", "s5b", "s6")
ENGS = ("pe", "dve", "act", "pool", "sp")
NDMA = 8


class Sched:
    def __init__(self, nc, stack):
        self.nc = nc
        self.sem = {e: stack.enter_context(nc.semaphore("s_" + e)) for e in ENGS}
        self.dsem = {q: [stack.enter_context(nc.semaphore("d_%s%d" % (q, i))) for i in range(NDMA)]
                     for q in ("sp", "act", "pool")}
        self.cnt = {e: 0 for e in ENGS}
        self.dcnt = {q: 0 for q in self.dsem}
        self.lists = {e: [] for e in ENGS}
        self.waited = {e: {} for e in ENGS}
        self.lastw = {}
        self.readers = {}
        self.n_inst = 0

    def _semobj(self, key):
        if isinstance(key, tuple):
            return self.dsem[key[0]][key[1]]
        return self.sem[key]

    def _need(self, eng, ev, waits):
        if ev is None:
            return
        key, val = ev
        if self.waited[eng].get(key, 0) >= val:
            return
        self.waited[eng][key] = val
        waits.append((key, val))

    def _deps(self, eng, reads, writes, waits):
        for r in reads:
            self._need(eng, self.lastw.get(r), waits)
        for w in writes:
            self._need(eng, self.lastw.get(w), waits)
            for ev in self.readers.get(w, ()):
                self._need(eng, ev, waits)

    def _commit(self, ev, reads, writes):
        for r in reads:
            self.readers.setdefault(r, []).append(ev)
        for w in writes:
            self.lastw[w] = ev
            self.readers[w] = []

    def op(self, eng, fn, reads=(), writes=()):
        waits = []
        self._deps(eng, reads, writes, waits)
        if eng == "pe":
            waits = [w for w in waits if w[0] != "pe"]
        self.cnt[eng] += 1
        ev = (eng, self.cnt[eng])
        self.lists[eng].append((fn, waits, (eng, 1)))
        self._commit(ev, reads, writes)
        self.n_inst += 1 + len(waits)
        return ev

    def dma(self, q, fn, reads=(), writes=()):
        waits = []
        j = self.dcnt[q]
        slot = j % NDMA
        prev = 16 * (j // NDMA)
        key = (q, slot)
        if prev > 0:
            self._need(q, (key, prev), waits)
        self._deps(q, reads, writes, waits)
        self.dcnt[q] += 1
        ev = (key, prev + 16)
        self.lists[q].append((fn, waits, (key, 16)))
        self._commit(ev, reads, writes)
        self.n_inst += 1 + len(waits)
        return ev

    def barrier(self):
        evs = [(e, self.cnt[e]) for e in ENGS if self.cnt[e] > 0]
        for q in self.dsem:
            j = self.dcnt[q]
            for slot in range(NDMA):
                if j > slot:
                    n = (j - 1 - slot) // NDMA + 1
                    evs.append(((q, slot), 16 * n))
        for e in ENGS:
            waits = []
            for ev in evs:
                self._need(e, ev, waits)
            if waits:
                self.lists[e].append((None, waits, None))
        self.lastw = {}
        self.readers = {}

    def flush(self):
        nc = self.nc
        lists = self.lists
        self.lists = {e: [] for e in ENGS}
        sched = self

        def run(engobj, lst):
            for fn, waits, inc in lst:
                for key, val in waits:
                    engobj.wait_ge(sched._semobj(key), val)
                if fn is not None:
                    ins = fn(engobj)
                    ins.then_inc(sched._semobj(inc[0]), inc[1])

        with nc.Block() as block:
            @block.tensor
            def _(e):
                run(e, lists["pe"])

            @block.vector
            def _(e):
                run(e, lists["dve"])

            @block.scalar
            def _(e):
                run(e, lists["act"])

            @block.gpsimd
            def _(e):
                run(e, lists["pool"])

            @block.sync
            def _(e):
                run(e, lists["sp"])


class Ctx:
    pass


_uid = [0]


def _sb(nc, st, name, shape, dt):
    _uid[0] += 1
    return st.enter_context(nc.sbuf_tensor("sb%d_%s" % (_uid[0], name), shape, dt))


def _ps(nc, st, name, shape, dt):
    _uid[0] += 1
    return st.enter_context(nc.psum_tensor("ps%d_%s" % (_uid[0], name), shape, dt))


def norm_transpose(S, T, src_rows, col0, uid):
    xs, gb, xnb, ss, idb, pt, xnT = T["xs"], T["gb"], T["xnb"], T["ss"], T["idb"], T["pt"], T["xnT"]
    S.dma("sp", lambda e: e.dma_start(out=xs[:], in_=src_rows), writes=["xs"])
    S.op("act", lambda e: e.activation(out=xnb[:], in_=xs[:], func=AF.Square, accum_out=ss[:, 0:1]),
         reads=["xs"], writes=["xnb", "ss0"])
    S.op("act", lambda e: e.activation(out=ss[:, 1:2], in_=ss[:, 0:1], func=AF.Sqrt, scale=1.0 / D, bias=T["epsb"][:, 0:1]),
         reads=["ss0", "epsb"], writes=["ss1"])
    S.op("dve", lambda e: e.reciprocal(out=ss[:, 2:3], in_=ss[:, 1:2]), reads=["ss1"], writes=["ss2"])
    S.op("dve", lambda e: e.scalar_tensor_tensor(out=xnb[:], in0=xs[:], scalar=ss[:, 2:3], in1=gb[:],
                                                 op0=ALU.mult, op1=ALU.mult),
         reads=["xs", "ss2", "gb"], writes=["xnb"])
    for g in range(4):
        p = pt[g % 2]
        pk = "pt%d" % (g % 2)
        for k in range(8):
            kc = g * 8 + k
            S.op("pe", lambda e, p=p, k=k, kc=kc: e.transpose(out=p[:, k * 128:(k + 1) * 128],
                                                               in_=xnb[:, kc * 128:(kc + 1) * 128],
                                                               identity=idb[:]),
                 reads=["xnb", "idb"], writes=[pk])
        eng = "act" if g % 2 == 0 else "dve"
        if eng == "act":
            S.op("act", lambda e, p=p, g=g: e.activation(
                out=xnT[:, g * 8:(g + 1) * 8, col0:col0 + 128],
                in_=p[:].rearrange("p (k t) -> p k t", k=8), func=AF.Copy),
                reads=[pk], writes=[("xnT", uid, g)])
        else:
            S.op("dve", lambda e, p=p, g=g: e.tensor_copy(
                out=xnT[:, g * 8:(g + 1) * 8, col0:col0 + 128],
                in_=p[:].rearrange("p (k t) -> p k t", k=8)),
                reads=[pk], writes=[("xnT", uid, g)])


def xnT_keys(uids):
    return [("xnT", u, g) for u in uids for g in range(4)]


def stage1(nc, S, C, tiles=(0, 1, 2, 3)):
    with ExitStack() as st:
        T = {}
        T["xnT"] = _sb(nc, st, "xnT", [128, 32, 1024], BF16)
        T["xs"] = _sb(nc, st, "xs", [128, D], F32)
        T["gb"] = _sb(nc, st, "gb", [128, D], F32)
        T["xnb"] = _sb(nc, st, "xnb", [128, D], BF16)
        T["ss"] = _sb(nc, st, "ss", [128, 4], F32)
        T["idb"] = _sb(nc, st, "idb", [128, 128], BF16)
        T["epsb"] = _sb(nc, st, "epsb", [128, 1], F32)
        S.op("dve", lambda e: e.memset(T["epsb"][:], EPS), writes=["epsb"])
        wst = [_sb(nc, st, "wst%d" % i, [128, 32, 128], F32) for i in range(2)]
        wb = [_sb(nc, st, "wb%d" % i, [128, 32, 128], BF16) for i in range(2)]
        ob = [_sb(nc, st, "ob%d" % i, [128, 512], BF16) for i in range(2)]
        of = [_sb(nc, st, "of%d" % i, [128, 512], F32) for i in range(2)]
        vt = [_sb(nc, st, "vt%d" % i, [128, 4, 128], BF16) for i in range(2)]
        pm = [_ps(nc, st, "pm%d" % i, [128, 512], F32) for i in range(4)]
        T["pt"] = [_ps(nc, st, "pt%d" % i, [128, 1024], BF16) for i in range(2)]
        pv = [_ps(nc, st, "pv%d" % i, [128, 1024], BF16) for i in range(2)]
        xnT, idb = T["xnT"], T["idb"]

        S.dma("sp", lambda e: e.dma_start(out=T["gb"][:], in_=C.g_mix.partition_broadcast(128)), writes=["gb"])
        S.dma("sp", lambda e: e.dma_start(out=idb[:], in_=C.ident_bf), writes=["idb"])

        cnt = {"w": 0, "pm": 0, "ob": 0, "of": 0, "vt": 0}
        w_in = C.w_in
        for tt in tiles:
            tok0 = tt * 1024
            for tb in range(8):
                norm_transpose(S, T, C.x[tok0 + tb * 128: tok0 + (tb + 1) * 128, :], tb * 128, tb)
            xkeys = xnT_keys(range(8))
            plan = []
            kv_only = (tt == 3)
            for fb in range(160):
                if fb < 16:
                    if not kv_only:
                        plan.append((fb, "qk", C.qaT, fb))
                elif fb < 32:
                    plan.append((fb, "qk", C.kaT, fb - 16))
                elif fb < 48:
                    plan.append((fb, "v", C.va, fb - 32))
                elif fb < 64:
                    if not kv_only:
                        plan.append((fb, "qk", C.qbT, fb - 48))
                elif fb < 80:
                    plan.append((fb, "qk", C.kbT, fb - 64))
                elif fb < 96:
                    plan.append((fb, "v", C.vb, fb - 80))
                elif fb < 128:
                    if not kv_only:
                        plan.append((fb, "gate", C.sga, fb - 96))
                else:
                    if not kv_only:
                        plan.append((fb, "gate", C.sgb, fb - 128))
            for (fb, kind, dst, db) in plan:
                wi = cnt["w"] % 2
                cnt["w"] += 1
                S.dma("sp", lambda e, wi=wi, fb=fb: e.dma_start(
                    out=wst[wi][:], in_=w_in[:, fb * 128:(fb + 1) * 128].rearrange("(kc p) f -> p kc f", p=128)),
                    writes=["wst%d" % wi])
                S.op("pool", lambda e, wi=wi: e.tensor_copy(out=wb[wi][:], in_=wst[wi][:]),
                     reads=["wst%d" % wi], writes=["wb%d" % wi])
                for half in range(2):
                    pi = cnt["pm"] % 4
                    cnt["pm"] += 1
                    for kc in range(32):
                        S.op("pe", lambda e, pi=pi, wi=wi, kc=kc, half=half: e.matmul(
                            out=pm[pi][:], lhsT=wb[wi][:, kc, :], rhs=xnT[:, kc, half * 512:(half + 1) * 512],
                            start=(kc == 0), stop=(kc == 31)),
                            reads=["wb%d" % wi] + (xkeys if kc in (0, 31) else []), writes=["pm%d" % pi])
                    c0 = tok0 + half * 512
                    if kind == "qk":
                        oi = cnt["ob"] % 2
                        cnt["ob"] += 1
                        S.op("act", lambda e, oi=oi, pi=pi: e.activation(out=ob[oi][:], in_=pm[pi][:], func=AF.Copy),
                             reads=["pm%d" % pi], writes=["ob%d" % oi])
                        S.dma("act", lambda e, oi=oi, dst=dst, db=db, c0=c0: e.dma_start(
                            out=dst[db * 128:(db + 1) * 128, c0:c0 + 512], in_=ob[oi][:]),
                            reads=["ob%d" % oi])
                    elif kind == "gate":
                        oi = cnt["of"] % 2
                        cnt["of"] += 1
                        S.op("act", lambda e, oi=oi, pi=pi: e.activation(out=of[oi][:], in_=pm[pi][:], func=AF.Sigmoid),
                             reads=["pm%d" % pi], writes=["of%d" % oi])
                        S.dma("act", lambda e, oi=oi, dst=dst, db=db, c0=c0: e.dma_start(
                            out=dst[db * 128:(db + 1) * 128, c0:c0 + 512], in_=of[oi][:]),
                            reads=["of%d" % oi])
                    else:
                        oi = cnt["ob"] % 2
                        cnt["ob"] += 1
                        S.op("act", lambda e, oi=oi, pi=pi: e.activation(out=ob[oi][:], in_=pm[pi][:], func=AF.Copy),
                             reads=["pm%d" % pi], writes=["ob%d" % oi])
                        vi = cnt["vt"] % 2
                        cnt["vt"] += 1
                        for t in range(4):
                            S.op("pe", lambda e, oi=oi, vi=vi, t=t: e.transpose(
                                out=pv[vi][:, t * 128:(t + 1) * 128], in_=ob[oi][:, t * 128:(t + 1) * 128],
                                identity=idb[:]), reads=["ob%d" % oi, "idb"], writes=["pv%d" % vi])
                        S.op("dve", lambda e, vi=vi: e.tensor_copy(
                            out=vt[vi][:], in_=pv[vi][:, 0:512].rearrange("p (t c) -> p t c", t=4)),
                            reads=["pv%d" % vi], writes=["vt%d" % vi])
                        S.dma("act", lambda e, vi=vi, dst=dst, db=db, c0=c0: e.dma_start(
                            out=dst[c0:c0 + 512, db * 128:(db + 1) * 128].rearrange("(t p) c -> p t c", p=128),
                            in_=vt[vi][:]), reads=["vt%d" % vi])
        S.barrier()
        S.flush()


def stage2(nc, S, C, seqs=((0, 2048, 0), (2048, 1024, 2048)), a_heads=range(16), b_heads=range(8)):
    with ExitStack() as st:
        idb = _sb(nc, st, "idb2", [128, 128], BF16)
        abt = _sb(nc, st, "abt", [128, ABW], F32)
        lct = _sb(nc, st, "lct", [128, ABW], F32)
        tb = [_sb(nc, st, "tb%d" % i, [128, ABW], F32) for i in range(2)]
        qT = [_sb(nc, st, "qT%d" % i, [128, 2, 2048], BF16) for i in range(2)]
        kT = [_sb(nc, st, "kT%d" % i, [128, 2, 2048], BF16) for i in range(2)]
        vS = [_sb(nc, st, "vS%d" % i, [128, 16, 256], BF16) for i in range(2)]
        St = [[_sb(nc, st, "St%d_%d" % (i, p), [128, 2048], F32) for p in range(2)] for i in range(2)]
        Pf = [[_sb(nc, st, "Pf%d_%d" % (i, p), [128, 2048], F32) for p in range(2)] for i in range(2)]
        Pbs = [_sb(nc, st, "Pb%d" % p, [128, 2048], BF16) for p in range(2)]
        aT = [_sb(nc, st, "aT%d" % i, [128, 16, 128], BF16) for i in range(2)]
        sms = [_sb(nc, st, "sm%d" % p, [128, 16], F32) for p in range(2)]
        oS = _sb(nc, st, "oS", [128, 256], F32)
        junk = _sb(nc, st, "junk2", [128, 256], F32)
        ob16 = _sb(nc, st, "ob16", [128, 256], BF16)
        oT = [_sb(nc, st, "oT%d" % i, [128, 2, 128], BF16) for i in range(2)]
        gsub = _sb(nc, st, "gsub", [128, 256], F32)
        lamt = _sb(nc, st, "lamt", [128, 512], F32)
        lamv = _sb(nc, st, "lamv", [128, 8], F32)
        epsb = _sb(nc, st, "epsb2", [128, 1], F32)
        ps = [_ps(nc, st, "ps%d" % i, [128, 512], F32) for i in range(4)]
        ptr = [_ps(nc, st, "ptr%d" % i, [128, 1024], BF16) for i in range(2)]
        po = _ps(nc, st, "po", [128, 512], F32)
        pot = _ps(nc, st, "pot", [128, 1024], BF16)

        S.dma("sp", lambda e: e.dma_start(out=idb[:], in_=C.ident_bf), writes=["idb"])
        S.dma("sp", lambda e: e.dma_start(out=abt[:], in_=C.abt), writes=["abt"])
        S.dma("sp", lambda e: e.dma_start(out=lct[:], in_=C.lct), writes=["lct"])
        S.dma("sp", lambda e: e.dma_start(out=gsub[:], in_=C.g_subln.partition_broadcast(128)), writes=["gsub"])
        S.dma("sp", lambda e: e.dma_start(out=lamt[:], in_=C.lam.partition_broadcast(128)), writes=["lamt"])
        S.op("dve", lambda e: e.memset(epsb[:], EPS), writes=["epsb"])
        S.op("dve", lambda e: e.tensor_scalar_mul(out=gsub[:], in0=gsub[:], scalar1=1.0 - LAM_INIT),
             reads=["gsub"], writes=["gsub"])
        for j in range(2):
            S.op("dve", lambda e, j=j: e.tensor_tensor(out=junk[:, 0:128], in0=lamt[:, j * 256:j * 256 + 128],
                                                       in1=lamt[:, j * 256 + 128:j * 256 + 256], op=ALU.mult),
                 reads=["lamt"], writes=["junk"])
            S.op("dve", lambda e, j=j: e.tensor_reduce(out=lamv[:, j:j + 1], in_=junk[:, 0:128], axis=AX.X, op=ALU.add),
                 reads=["junk"], writes=["lamv%d" % j])
            S.op("act", lambda e, j=j: e.activation(out=lamv[:, 2 + j:3 + j], in_=lamv[:, j:j + 1], func=AF.Exp),
                 reads=["lamv%d" % j], writes=["lame%d" % j])
        S.op("dve", lambda e: e.tensor_tensor(out=lamv[:, 4:5], in0=lamv[:, 2:3], in1=lamv[:, 3:4], op=ALU.subtract),
             reads=["lame0", "lame1"], writes=["lamd"])
        S.op("dve", lambda e: e.tensor_scalar(out=lamv[:, 5:6], in0=lamv[:, 4:5], scalar1=LAM_INIT, scalar2=-1.0,
                                              op0=ALU.add, op1=ALU.mult), reads=["lamd"], writes=["nlam"])

        cnt = {"h": 0, "ps": 0, "aT": 0, "oT": 0}

        def head(kind, h, qbase, nq, kbase):
            b = cnt["h"] % 2
            cnt["h"] += 1
            nj = 1 if kind == "a" else 2
            ve = 128 if kind == "a" else 256
            if kind == "a":
                slope = 2.0 ** (-8.0 * (h + 1) / 16)
                qsrc, ksrc, vsrc, odst = C.qaT, C.kaT, C.va, C.oaT
            else:
                slope = 2.0 ** (-8.0 * (h + 1) / 8)
                qsrc, ksrc, vsrc, odst = C.qbT, C.kbT, C.vb, C.obT
            f0 = h * 128 * nj
            for j in range(nj):
                S.dma("sp", lambda e, j=j: e.dma_start(out=qT[b][:, j, 0:nq],
                                                        in_=qsrc[f0 + j * 128:f0 + (j + 1) * 128, qbase:qbase + nq]),
                      writes=["qT%d" % b])
                S.dma("sp", lambda e, j=j: e.dma_start(out=kT[b][:, j, :],
                                                        in_=ksrc[f0 + j * 128:f0 + (j + 1) * 128, kbase:kbase + 2048]),
                      writes=["kT%d" % b])
            S.dma("sp", lambda e: e.dma_start(
                out=vS[b][:, :, 0:ve],
                in_=vsrc[kbase:kbase + 2048, h * ve:(h + 1) * ve].rearrange("(kb p) c -> p kb c", p=128)),
                writes=["vS%d" % b])
            if kind == "a":
                S.op("dve", lambda e: e.scalar_tensor_tensor(out=tb[b][:], in0=abt[:], scalar=-slope, in1=lct[:],
                                                             op0=ALU.mult, op1=ALU.add),
                     reads=["abt", "lct"], writes=["tb%d" % b])
            else:
                S.op("dve", lambda e: e.tensor_scalar_mul(out=tb[b][:], in0=abt[:], scalar1=-slope),
                     reads=["abt"], writes=["tb%d" % b])
            def qblock(i):
                off = 1920 - 128 * i
                p = i % 2
                sm = sms[p]
                Pb = Pbs[p]
                for j in range(nj):
                    for c in range(4):
                        r = cnt["ps"] % 4
                        cnt["ps"] += 1
                        S.op("pe", lambda e, r=r, j=j, c=c: e.matmul(
                            out=ps[r][:], lhsT=qT[b][:, j, i * 128:(i + 1) * 128],
                            rhs=kT[b][:, j, c * 512:(c + 1) * 512], start=True, stop=True),
                            reads=["qT%d" % b, "kT%d" % b], writes=["ps%d" % r])
                        S.op("dve", lambda e, r=r, j=j, c=c: e.scalar_tensor_tensor(
                            out=St[j][p][:, c * 512:(c + 1) * 512], in0=ps[r][:], scalar=SCALE,
                            in1=tb[b][:, off + c * 512:off + (c + 1) * 512], op0=ALU.mult, op1=ALU.add),
                            reads=["ps%d" % r, "tb%d" % b], writes=[("St", j, p)])
                    S.op("dve", lambda e, j=j: e.tensor_reduce(out=sm[:, j:j + 1], in_=St[j][p][:], axis=AX.X, op=ALU.max),
                         reads=[("St", j, p)], writes=[("mx", j, p)])
                    S.op("dve", lambda e, j=j: e.tensor_scalar_mul(out=sm[:, 2 + j:3 + j], in0=sm[:, j:j + 1], scalar1=-1.0),
                         reads=[("mx", j, p)], writes=[("nm", j, p)])
                    if kind == "a":
                        S.op("act", lambda e: e.activation(out=Pb[:], in_=St[0][p][:], func=AF.Exp, bias=sm[:, 2:3],
                                                           accum_out=sm[:, 4:5]),
                             reads=[("St", 0, p), ("nm", 0, p)], writes=[("Pb", p), ("rs", 0, p)])
                    else:
                        S.op("act", lambda e, j=j: e.activation(out=Pf[j][p][:], in_=St[j][p][:], func=AF.Exp,
                                                                bias=sm[:, 2 + j:3 + j], accum_out=sm[:, 4 + j:5 + j]),
                             reads=[("St", j, p), ("nm", j, p)], writes=[("Pf", j, p), ("rs", j, p)])
                if kind == "b":
                    S.op("dve", lambda e: e.reciprocal(out=sm[:, 6:8], in_=sm[:, 4:6]), reads=[("rs", 0, p), ("rs", 1, p)], writes=[("rinv", p)])
                    S.op("dve", lambda e: e.scalar_tensor_tensor(out=sm[:, 8:9], in0=sm[:, 4:5], scalar=sm[:, 7:8],
                                                                 in1=lamv[:, 5:6], op0=ALU.mult, op1=ALU.mult),
                         reads=[("rs", 0, p), ("rinv", p), "nlam"], writes=[("ratio", p)])
                    S.op("dve", lambda e: e.scalar_tensor_tensor(out=Pb[:], in0=Pf[1][p][:], scalar=sm[:, 8:9], in1=Pf[0][p][:],
                                                                 op0=ALU.mult, op1=ALU.add),
                         reads=[("Pf", 0, p), ("Pf", 1, p), ("ratio", p)], writes=[("Pb", p)])
                else:
                    S.op("dve", lambda e: e.reciprocal(out=sm[:, 6:7], in_=sm[:, 4:5]), reads=[("rs", 0, p)], writes=[("rinv", p)])
                ai = cnt["aT"] % 2
                cnt["aT"] += 1
                for g in range(2):
                    for k in range(8):
                        kb = g * 8 + k
                        S.op("pe", lambda e, g=g, k=k, kb=kb: e.transpose(
                            out=ptr[g][:, k * 128:(k + 1) * 128], in_=Pb[:, kb * 128:(kb + 1) * 128], identity=idb[:]),
                            reads=[("Pb", p), "idb"], writes=["ptr%d" % g])
                    S.op("act", lambda e, g=g: e.activation(out=aT[ai][:, g * 8:(g + 1) * 8, :],
                                                            in_=ptr[g][:].rearrange("p (k t) -> p k t", k=8), func=AF.Copy),
                         reads=["ptr%d" % g], writes=[("aT", ai, g)])
                for kb in range(16):
                    S.op("pe", lambda e, kb=kb: e.matmul(out=po[:, 0:ve], lhsT=aT[ai][:, kb, :], rhs=vS[b][:, kb, 0:ve],
                                                         start=(kb == 0), stop=(kb == 15)),
                         reads=[("aT", ai, kb // 8), "vS%d" % b], writes=["po"])
                oi = cnt["oT"] % 2
                cnt["oT"] += 1
                qc = qbase + i * 128
                if kind == "a":
                    S.op("act", lambda e: e.activation(out=ob16[:, 0:128], in_=po[:, 0:128], func=AF.Copy, scale=sm[:, 6:7]),
                         reads=["po", ("rinv", p)], writes=["ob16"])
                    S.op("pe", lambda e: e.transpose(out=pot[:, 0:128], in_=ob16[:, 0:128], identity=idb[:]),
                         reads=["ob16", "idb"], writes=["pot"])
                    S.op("dve", lambda e: e.tensor_copy(out=oT[oi][:, 0, :], in_=pot[:, 0:128]),
                         reads=["pot"], writes=["oT%d" % oi])
                    S.dma("act", lambda e: e.dma_start(out=odst[h * 128:(h + 1) * 128, qc:qc + 128], in_=oT[oi][:, 0, :]),
                          reads=["oT%d" % oi])
                else:
                    S.op("act", lambda e: e.activation(out=oS[:], in_=po[:, 0:256], func=AF.Copy, scale=sm[:, 6:7]),
                         reads=["po", ("rinv", p)], writes=["oS"])
                    S.op("act", lambda e: e.activation(out=junk[:], in_=oS[:], func=AF.Square, accum_out=sm[:, 9:10]),
                         reads=["oS"], writes=["junk", ("ssq", p)])
                    S.op("act", lambda e: e.activation(out=sm[:, 10:11], in_=sm[:, 9:10], func=AF.Sqrt, scale=1.0 / 256,
                                                       bias=epsb[:, 0:1]), reads=[("ssq", p), "epsb"], writes=[("srt", p)])
                    S.op("dve", lambda e: e.reciprocal(out=sm[:, 11:12], in_=sm[:, 10:11]), reads=[("srt", p)], writes=[("rstd", p)])
                    S.op("dve", lambda e: e.scalar_tensor_tensor(out=ob16[:], in0=oS[:], scalar=sm[:, 11:12], in1=gsub[:],
                                                                 op0=ALU.mult, op1=ALU.mult),
                         reads=["oS", ("rstd", p), "gsub"], writes=["ob16"])
                    for t in range(2):
                        S.op("pe", lambda e, t=t: e.transpose(out=pot[:, t * 128:(t + 1) * 128],
                                                              in_=ob16[:, t * 128:(t + 1) * 128], identity=idb[:]),
                             reads=["ob16", "idb"], writes=["pot"])
                    S.op("dve", lambda e: e.tensor_copy(out=oT[oi][:], in_=pot[:, 0:256].rearrange("p (t c) -> p t c", t=2)),
                         reads=["pot"], writes=["oT%d" % oi])
                    S.dma("act", lambda e: e.dma_start(
                        out=odst[h * 256:(h + 1) * 256, qc:qc + 128].rearrange("(t p) c -> p t c", p=128), in_=oT[oi][:]),
                        reads=["oT%d" % oi])

            for i in range(nq // 128):
                qblock(i)

        for (qbase, nq, kbase) in seqs:
            for h in a_heads:
                head("a", h, qbase, nq, kbase)
            for h in b_heads:
                head("b", h, qbase, nq, kbase)
        S.barrier()
        S.flush()


def stage3(nc, S, C, tiles=range(3)):
    with ExitStack() as st:
        oat = _sb(nc, st, "oat", [128, 16, 1024], BF16)
        obt = _sb(nc, st, "obt", [128, 16, 1024], BF16)
        mT = _sb(nc, st, "mT", [128, 32, 1024], BF16)
        wst = [_sb(nc, st, "wst%d" % i, [128, 32, 128], F32) for i in range(2)]
        wb = [_sb(nc, st, "wb%d" % i, [128, 32, 128], BF16) for i in range(2)]
        sg = [_sb(nc, st, "sg%d" % i, [128, 2, 512], F32) for i in range(2)]
        m1 = _sb(nc, st, "m1", [128, 512], F32)
        m2 = _sb(nc, st, "m2", [128, 512], F32)
        hf = [_sb(nc, st, "hf%d" % i, [128, 512], F32) for i in range(2)]
        xt = [_sb(nc, st, "xt%d" % i, [128, 4, 128], F32) for i in range(2)]
        ho = [_sb(nc, st, "ho%d" % i, [128, 4, 128], F32) for i in range(2)]
        idf = _sb(nc, st, "idf", [128, 128], F32)
        pm = [_ps(nc, st, "pm%d" % i, [128, 512], F32) for i in range(4)]
        pT = [_ps(nc, st, "pT%d" % i, [128, 512], F32) for i in range(2)]
        S.dma("sp", lambda e: e.dma_start(out=idf[:], in_=C.ident_f), writes=["idf"])
        cnt = {"w": 0, "pm": 0, "sg": 0, "hf": 0, "xt": 0, "pT": 0}
        for tt in tiles:
            t0 = tt * 1024
            S.dma("sp", lambda e, t0=t0: e.dma_start(out=oat[:], in_=C.oaT[:, t0:t0 + 1024].rearrange("(kc p) t -> p kc t", p=128)),
                  writes=["oat"])
            S.dma("sp", lambda e, t0=t0: e.dma_start(out=obt[:], in_=C.obT[:, t0:t0 + 1024].rearrange("(kc p) t -> p kc t", p=128)),
                  writes=["obt"])
            for fb in range(32):
                wi = cnt["w"] % 2
                cnt["w"] += 1
                S.dma("sp", lambda e, wi=wi, fb=fb: e.dma_start(
                    out=wst[wi][:, 0:16, :], in_=C.w_pa[:, fb * 128:(fb + 1) * 128].rearrange("(kc p) f -> p kc f", p=128)),
                    writes=["wst%d" % wi])
                S.dma("sp", lambda e, wi=wi, fb=fb: e.dma_start(
                    out=wst[wi][:, 16:32, :], in_=C.w_pb[:, fb * 128:(fb + 1) * 128].rearrange("(kc p) f -> p kc f", p=128)),
                    writes=["wst%d" % wi])
                S.op("pool", lambda e, wi=wi: e.tensor_copy(out=wb[wi][:], in_=wst[wi][:]),
                     reads=["wst%d" % wi], writes=["wb%d" % wi])
                for half in range(2):
                    c0 = t0 + half * 512
                    hs = slice(half * 512, (half + 1) * 512)
                    si = cnt["sg"] % 2
                    cnt["sg"] += 1
                    S.dma("sp", lambda e, si=si, fb=fb, c0=c0: e.dma_start(out=sg[si][:, 0, :], in_=C.sga[fb * 128:(fb + 1) * 128, c0:c0 + 512]),
                          writes=["sg%d" % si])
                    S.dma("sp", lambda e, si=si, fb=fb, c0=c0: e.dma_start(out=sg[si][:, 1, :], in_=C.sgb[fb * 128:(fb + 1) * 128, c0:c0 + 512]),
                          writes=["sg%d" % si])
                    pa = cnt["pm"] % 4
                    pb = (cnt["pm"] + 1) % 4
                    cnt["pm"] += 2
                    for kc in range(16):
                        S.op("pe", lambda e, pa=pa, wi=wi, kc=kc, hs=hs: e.matmul(out=pm[pa][:], lhsT=wb[wi][:, kc, :], rhs=oat[:, kc, hs],
                                                                                   start=(kc == 0), stop=(kc == 15)),
                             reads=["wb%d" % wi, "oat"], writes=["pm%d" % pa])
                    for kc in range(16):
                        S.op("pe", lambda e, pb=pb, wi=wi, kc=kc, hs=hs: e.matmul(out=pm[pb][:], lhsT=wb[wi][:, 16 + kc, :], rhs=obt[:, kc, hs],
                                                                                   start=(kc == 0), stop=(kc == 15)),
                             reads=["wb%d" % wi, "obt"], writes=["pm%d" % pb])
                    S.op("dve", lambda e, pa=pa, si=si: e.tensor_tensor(out=m1[:], in0=pm[pa][:], in1=sg[si][:, 0, :], op=ALU.mult),
                         reads=["pm%d" % pa, "sg%d" % si], writes=["m1"])
                    S.op("dve", lambda e, pb=pb, si=si: e.tensor_tensor(out=m2[:], in0=pm[pb][:], in1=sg[si][:, 1, :], op=ALU.mult),
                         reads=["pm%d" % pb, "sg%d" % si], writes=["m2"])
                    S.op("dve", lambda e, fb=fb, hs=hs: e.tensor_tensor(out=mT[:, fb, hs], in0=m1[:], in1=m2[:], op=ALU.add),
                         reads=["m1", "m2"], writes=[("mT", fb)])
            mkeys = [("mT", fb) for fb in range(32)]
            for fb in range(32):
                wi = cnt["w"] % 2
                cnt["w"] += 1
                S.dma("sp", lambda e, wi=wi, fb=fb: e.dma_start(
                    out=wst[wi][:], in_=C.w_out[:, fb * 128:(fb + 1) * 128].rearrange("(kc p) f -> p kc f", p=128)),
                    writes=["wst%d" % wi])
                S.op("pool", lambda e, wi=wi: e.tensor_copy(out=wb[wi][:], in_=wst[wi][:]),
                     reads=["wst%d" % wi], writes=["wb%d" % wi])
                for half in range(2):
                    c0 = t0 + half * 512
                    hs = slice(half * 512, (half + 1) * 512)
                    xi = cnt["xt"] % 2
                    cnt["xt"] += 1
                    S.dma("sp", lambda e, xi=xi, fb=fb, c0=c0: e.dma_start(
                        out=xt[xi][:], in_=C.x[c0:c0 + 512, fb * 128:(fb + 1) * 128].rearrange("(t p) c -> p t c", p=128)),
                        writes=["xt%d" % xi])
                    pi = cnt["pm"] % 4
                    cnt["pm"] += 1
                    for kc in range(32):
                        S.op("pe", lambda e, pi=pi, wi=wi, kc=kc, hs=hs: e.matmul(out=pm[pi][:], lhsT=wb[wi][:, kc, :], rhs=mT[:, kc, hs],
                                                                                   start=(kc == 0), stop=(kc == 31)),
                             reads=["wb%d" % wi] + (mkeys if kc in (0, 31) else []), writes=["pm%d" % pi])
                    hi = cnt["hf"] % 2
                    cnt["hf"] += 1
                    S.op("act", lambda e, hi=hi, pi=pi: e.activation(out=hf[hi][:], in_=pm[pi][:], func=AF.Copy),
                         reads=["pm%d" % pi], writes=["hf%d" % hi])
                    ti = cnt["pT"] % 2
                    cnt["pT"] += 1
                    for t in range(4):
                        S.op("pe", lambda e, hi=hi, ti=ti, t=t: e.transpose(out=pT[ti][:, t * 128:(t + 1) * 128],
                                                                            in_=hf[hi][:, t * 128:(t + 1) * 128], identity=idf[:]),
                             reads=["hf%d" % hi, "idf"], writes=["pT%d" % ti])
                    S.op("dve", lambda e, xi=xi, ti=ti: e.tensor_tensor(out=ho[xi][:], in0=pT[ti][:].rearrange("p (t c) -> p t c", t=4),
                                                                        in1=xt[xi][:], op=ALU.add),
                         reads=["pT%d" % ti, "xt%d" % xi], writes=["ho%d" % xi])
                    S.dma("act", lambda e, xi=xi, fb=fb, c0=c0: e.dma_start(
                        out=C.h[c0:c0 + 512, fb * 128:(fb + 1) * 128].rearrange("(t p) c -> p t c", p=128), in_=ho[xi][:]),
                        reads=["ho%d" % xi])
        S.barrier()
        S.flush()


def stage4(nc, S, C, tiles=range(6)):
    with ExitStack() as st:
        T = {}
        T["xnT"] = _sb(nc, st, "xnT", [128, 32, 512], BF16)
        T["xs"] = _sb(nc, st, "xs", [128, D], F32)
        T["gb"] = _sb(nc, st, "gb", [128, D], F32)
        T["xnb"] = _sb(nc, st, "xnb", [128, D], BF16)
        T["ss"] = _sb(nc, st, "ss", [128, 4], F32)
        T["idb"] = _sb(nc, st, "idb", [128, 128], BF16)
        T["epsb"] = _sb(nc, st, "epsb", [128, 1], F32)
        idf = _sb(nc, st, "idf", [128, 128], F32)
        wst = [_sb(nc, st, "wst%d" % i, [128, 32, 128], F32) for i in range(2)]
        wb = [_sb(nc, st, "wb%d" % i, [128, 32, 128], BF16) for i in range(2)]
        pq = _sb(nc, st, "pq", [128, 16, 512], BF16)
        skl = _sb(nc, st, "skl", [128, 16, 128], F32)
        KT = _sb(nc, st, "KT", [128, 16, 128], BF16)
        sc = _sb(nc, st, "sc", [128, 16, 128], F32)
        tmp = _sb(nc, st, "tmp", [128, 16, 128], F32)
        v16 = _sb(nc, st, "v16", [128, 16, 16], F32)
        cand = [_sb(nc, st, "cand%d" % i, [128, 256], F32) for i in range(3)]
        c24 = _sb(nc, st, "c24", [128, 8, 24], F32)
        e16 = _sb(nc, st, "e16", [128, 16], F32)
        rt = _sb(nc, st, "rt", [128, 8, 8], F32)
        P2 = [_sb(nc, st, "P2_%d" % i, [128, 8, 128], F32) for i in range(2)]
        TH = [_sb(nc, st, "TH_%d" % i, [128, 8, 128], F32) for i in range(2)]
        WG = [_sb(nc, st, "WG_%d" % i, [128, 8, 128], F32) for i in range(2)]
        pm = [_ps(nc, st, "pm%d" % i, [128, 512], F32) for i in range(4)]
        T["pt"] = [_ps(nc, st, "pt%d" % i, [128, 1024], BF16) for i in range(2)]
        pk = _ps(nc, st, "pk", [128, 512], F32)
        xnT, idb = T["xnT"], T["idb"]
        S.dma("sp", lambda e: e.dma_start(out=T["gb"][:], in_=C.g_ffn.partition_broadcast(128)), writes=["gb"])
        S.dma("sp", lambda e: e.dma_start(out=idb[:], in_=C.ident_bf), writes=["idb"])
        S.dma("sp", lambda e: e.dma_start(out=idf[:], in_=C.ident_f), writes=["idf"])
        S.op("dve", lambda e: e.memset(T["epsb"][:], EPS), writes=["epsb"])
        S.dma("sp", lambda e: e.dma_start(out=skl[:].rearrange("p (h s) c -> p h s c", s=2)[:, :, 0, :],
                                          in_=C.sk1.rearrange("h n c -> n h c")), writes=["skl"])
        S.dma("sp", lambda e: e.dma_start(out=skl[:].rearrange("p (h s) c -> p h s c", s=2)[:, :, 1, :],
                                          in_=C.sk2.rearrange("h n c -> n h c")), writes=["skl"])
        for g in range(4):
            for k in range(4):
                hs = g * 4 + k
                S.op("pe", lambda e, k=k, hs=hs: e.transpose(out=pk[:, k * 128:(k + 1) * 128], in_=skl[:, hs, :], identity=idf[:]),
                     reads=["skl", "idf"], writes=["pk"])
            S.op("dve", lambda e, g=g: e.tensor_copy(out=KT[:, g * 4:(g + 1) * 4, :], in_=pk[:].rearrange("p (k n) -> p k n", k=4)),
                 reads=["pk"], writes=["KT"])
        cnt = {"w": 0, "pm": 0, "tb": 0}

        def tile(tt):
            tok0 = tt * 512
            for tb in range(4):
                norm_transpose(S, T, C.h[tok0 + tb * 128: tok0 + (tb + 1) * 128, :], tb * 128, tb)
            xkeys = xnT_keys(range(4))
            S.dma("act", lambda e: e.dma_start(
                out=C.xn2T[:, tok0:tok0 + 512].rearrange("(kc p) t -> p kc t", p=128), in_=xnT[:]), reads=xkeys)
            for fb in range(16):
                wi = cnt["w"] % 2
                cnt["w"] += 1
                S.dma("sp", lambda e, wi=wi, fb=fb: e.dma_start(
                    out=wst[wi][:], in_=C.w_query[:, fb * 128:(fb + 1) * 128].rearrange("(kc p) f -> p kc f", p=128)),
                    writes=["wst%d" % wi])
                S.op("pool", lambda e, wi=wi: e.tensor_copy(out=wb[wi][:], in_=wst[wi][:]),
                     reads=["wst%d" % wi], writes=["wb%d" % wi])
                for half in range(1):
                    pi = cnt["pm"] % 4
                    cnt["pm"] += 1
                    for kc in range(32):
                        S.op("pe", lambda e, pi=pi, wi=wi, kc=kc, half=half: e.matmul(
                            out=pm[pi][:], lhsT=wb[wi][:, kc, :], rhs=xnT[:, kc, half * 512:(half + 1) * 512],
                            start=(kc == 0), stop=(kc == 31)),
                            reads=["wb%d" % wi] + (xkeys if kc in (0, 31) else []), writes=["pm%d" % pi])
                    S.op("act", lambda e, pi=pi, fb=fb, half=half: e.activation(
                        out=pq[:, fb, half * 512:(half + 1) * 512], in_=pm[pi][:], func=AF.Copy),
                        reads=["pm%d" % pi], writes=[("pq", fb)])
            pqkeys = [("pq", fb) for fb in range(16)]

            def route(tb):
                ri = cnt["tb"] % 2
                cnt["tb"] += 1
                r0 = tok0 + tb * 128
                for g in range(4):
                    pi = cnt["pm"] % 4
                    cnt["pm"] += 1
                    for k in range(4):
                        hs = g * 4 + k
                        S.op("pe", lambda e, pi=pi, k=k, hs=hs: e.matmul(
                            out=pm[pi][:, k * 128:(k + 1) * 128], lhsT=pq[:, hs, tb * 128:(tb + 1) * 128], rhs=KT[:, hs, :],
                            start=True, stop=True), reads=pqkeys + ["KT"], writes=["pm%d" % pi])
                    S.op("act", lambda e, pi=pi, g=g: e.activation(
                        out=sc[:, g * 4:(g + 1) * 4, :], in_=pm[pi][:].rearrange("p (k n) -> p k n", k=4), func=AF.Copy),
                        reads=["pm%d" % pi], writes=[("sc", g)])
                sck = [("sc", g) for g in range(4)]
                for hs in range(16):
                    S.op("dve", lambda e, hs=hs: e.max(out=v16[:, hs, 0:8], in_=sc[:, hs, :]),
                         reads=sck, writes=[("v16a", hs)])
                    S.op("dve", lambda e, hs=hs: e.match_replace(out=tmp[:, hs, :], in_to_replace=v16[:, hs, 0:8],
                                                                 in_values=sc[:, hs, :], imm_value=-1e30),
                         reads=[("v16a", hs)] + sck, writes=[("tmp", hs)])
                    S.op("dve", lambda e, hs=hs: e.max(out=v16[:, hs, 8:16], in_=tmp[:, hs, :]),
                         reads=[("tmp", hs)], writes=[("v16b", hs)])
                for h in range(8):
                    S.op("dve", lambda e, h=h: e.tensor_tensor(
                        out=cand[0][:].rearrange("p (a b) -> p a b", a=16),
                        in0=v16[:, 2 * h, :].unsqueeze(2).to_broadcast([128, 16, 16]),
                        in1=v16[:, 2 * h + 1, :].unsqueeze(1).to_broadcast([128, 16, 16]), op=ALU.add),
                        reads=[("v16a", 2 * h), ("v16b", 2 * h), ("v16a", 2 * h + 1), ("v16b", 2 * h + 1)], writes=["cand0"])
                    S.op("dve", lambda e, h=h: e.max(out=c24[:, h, 0:8], in_=cand[0][:]), reads=["cand0"], writes=["c24a"])
                    S.op("dve", lambda e, h=h: e.match_replace(out=cand[1][:], in_to_replace=c24[:, h, 0:8], in_values=cand[0][:],
                                                               imm_value=-1e30), reads=["cand0", "c24a"], writes=["cand1"])
                    S.op("dve", lambda e, h=h: e.max(out=c24[:, h, 8:16], in_=cand[1][:]), reads=["cand1"], writes=["c24b"])
                    S.op("dve", lambda e, h=h: e.match_replace(out=cand[2][:], in_to_replace=c24[:, h, 8:16], in_values=cand[1][:],
                                                               imm_value=-1e30), reads=["cand1", "c24b"], writes=["cand2"])
                    S.op("dve", lambda e, h=h: e.max(out=c24[:, h, 16:24], in_=cand[2][:]), reads=["cand2"], writes=[("c24", h)])
                ck = [("c24", h) for h in range(8)] + ["c24a", "c24b"]
                vk = [("v16a", hs) for hs in range(16)]
                S.op("dve", lambda e: e.tensor_tensor(out=rt[:, :, 0], in0=c24[:, :, 15], in1=c24[:, :, 16], op=ALU.add),
                     reads=ck, writes=["rt0"])
                S.op("dve", lambda e: e.tensor_scalar_mul(out=rt[:, :, 0], in0=rt[:, :, 0], scalar1=0.5), reads=["rt0"], writes=["rt0"])
                S.op("dve", lambda e: e.tensor_scalar_mul(out=rt[:, :, 1], in0=c24[:, :, 0], scalar1=-1.0), reads=ck, writes=["rt1"])
                v16h = v16[:].rearrange("p (h s) k -> p h s k", s=2)
                S.op("dve", lambda e: e.tensor_tensor(out=rt[:, :, 2], in0=rt[:, :, 0], in1=v16h[:, :, 1, 0], op=ALU.subtract),
                     reads=["rt0"] + vk, writes=["rt2"])
                S.op("dve", lambda e: e.tensor_scalar_mul(out=rt[:, :, 3], in0=v16h[:, :, 0, 0], scalar1=-1.0), reads=vk, writes=["rt3"])
                S.op("dve", lambda e: e.tensor_scalar_mul(out=rt[:, :, 4], in0=v16h[:, :, 1, 0], scalar1=-1.0), reads=vk, writes=["rt4"])
                for h in range(8):
                    S.op("act", lambda e, h=h: e.activation(out=e16[:], in_=c24[:, h, 0:16], func=AF.Exp, bias=rt[:, h, 1:2],
                                                            accum_out=rt[:, h, 5:6]),
                         reads=ck + ["rt1"], writes=["e16", ("Z", h)])
                    S.op("act", lambda e, h=h: e.activation(out=P2[ri][:, h, :], in_=sc[:, 2 * h + 1, :], func=AF.Exp,
                                                            bias=rt[:, h, 4:5]), reads=sck + ["rt4"], writes=["P2_%d" % ri])
                    S.op("act", lambda e, h=h: e.activation(out=TH[ri][:, h, :], in_=sc[:, 2 * h, :], func=AF.Exp,
                                                            bias=rt[:, h, 2:3], scale=-1.0), reads=sck + ["rt2"], writes=["TH_%d" % ri])
                    S.op("act", lambda e, h=h: e.activation(out=WG[ri][:, h, :], in_=sc[:, 2 * h, :], func=AF.Exp,
                                                            bias=rt[:, h, 3:4]), reads=sck + ["rt3"], writes=[("WG", ri, h)])
                S.op("dve", lambda e: e.reciprocal(out=rt[:, :, 6], in_=rt[:, :, 5]), reads=[("Z", h) for h in range(8)], writes=["rZ"])
                for h in range(8):
                    S.op("dve", lambda e, h=h: e.tensor_scalar(out=tmp[:, h, :], in0=sc[:, 2 * h, :], scalar1=v16[:, 2 * h, 15:16],
                                                               scalar2=1e30, op0=ALU.is_lt, op1=ALU.mult),
                         reads=sck + [("v16b", 2 * h)], writes=[("tmp", h)])
                    S.op("dve", lambda e, h=h: e.tensor_tensor(out=TH[ri][:, h, :], in0=TH[ri][:, h, :], in1=tmp[:, h, :], op=ALU.add),
                         reads=[("tmp", h), "TH_%d" % ri], writes=["TH_%d" % ri])
                    S.op("dve", lambda e, h=h: e.scalar_tensor_tensor(out=P2[ri][:, h, :], in0=sc[:, 2 * h + 1, :],
                                                                      scalar=v16[:, 2 * h + 1, 15:16], in1=P2[ri][:, h, :],
                                                                      op0=ALU.is_ge, op1=ALU.mult),
                         reads=sck + [("v16b", 2 * h + 1), "P2_%d" % ri], writes=["P2_%d" % ri])
                for h in range(8):
                    S.op("dve", lambda e, h=h: e.tensor_scalar_mul(out=WG[ri][:, h, :], in0=WG[ri][:, h, :], scalar1=rt[:, h, 6:7]),
                         reads=[("WG", ri, h), "rZ"], writes=[("WG", ri, h)])
                S.dma("act", lambda e: e.dma_start(out=C.P2[r0:r0 + 128, :], in_=P2[ri][:].rearrange("p h n -> p (h n)")),
                      reads=["P2_%d" % ri])
                S.dma("act", lambda e: e.dma_start(out=C.TH[r0:r0 + 128, :], in_=TH[ri][:].rearrange("p h n -> p (h n)")),
                      reads=["TH_%d" % ri])
                S.dma("act", lambda e: e.dma_start(out=C.WG[r0:r0 + 128, :], in_=WG[ri][:].rearrange("p h n -> p (h n)")),
                      reads=[("WG", ri, h) for h in range(8)])

            for tb in range(4):
                route(tb)

        for tt in tiles:
            tile(tt)
        S.barrier()
        S.flush()


def stage5a(nc, S, C, egs=range(32), vblks=range(128)):
    with ExitStack() as st:
        us = [_sb(nc, st, "us%d" % i, [128, D], F32) for i in range(2)]
        ub = [_sb(nc, st, "ub%d" % i, [128, D], BF16) for i in range(2)]
        ut = [_sb(nc, st, "ut%d" % i, [128, 32, 512], BF16) for i in range(2)]
        idb = _sb(nc, st, "idb", [128, 128], BF16)
        pt = [_ps(nc, st, "pt%d" % i, [128, 1024], BF16) for i in range(4)]
        S.dma("sp", lambda e: e.dma_start(out=idb[:], in_=C.ident_bf), writes=["idb"])
        cnt = {"u": 0, "pt": 0, "ev": 0}
        for eg in egs:
            ui = eg % 2
            for sub in range(4):
                eb = eg * 4 + sub
                bi = cnt["u"] % 2
                cnt["u"] += 1
                S.dma("sp", lambda e, bi=bi, eb=eb: e.dma_start(out=us[bi][:], in_=C.eu[eb * 128:(eb + 1) * 128, :]),
                      writes=["us%d" % bi])
                S.op("pool", lambda e, bi=bi: e.tensor_copy(out=ub[bi][:], in_=us[bi][:]), reads=["us%d" % bi], writes=["ub%d" % bi])
                for g in range(4):
                    pi = cnt["pt"] % 4
                    cnt["pt"] += 1
                    for k in range(8):
                        kc = g * 8 + k
                        S.op("pe", lambda e, pi=pi, bi=bi, k=k, kc=kc: e.transpose(
                            out=pt[pi][:, k * 128:(k + 1) * 128], in_=ub[bi][:, kc * 128:(kc + 1) * 128], identity=idb[:]),
                            reads=["ub%d" % bi, "idb"], writes=["pt%d" % pi])
                    eng = "act" if cnt["ev"] % 2 == 0 else "dve"
                    cnt["ev"] += 1
                    if eng == "act":
                        S.op("act", lambda e, pi=pi, ui=ui, g=g, sub=sub: e.activation(
                            out=ut[ui][:, g * 8:(g + 1) * 8, sub * 128:(sub + 1) * 128],
                            in_=pt[pi][:].rearrange("p (k t) -> p k t", k=8), func=AF.Copy),
                            reads=["pt%d" % pi], writes=["ut%d" % ui])
                    else:
                        S.op("dve", lambda e, pi=pi, ui=ui, g=g, sub=sub: e.tensor_copy(
                            out=ut[ui][:, g * 8:(g + 1) * 8, sub * 128:(sub + 1) * 128],
                            in_=pt[pi][:].rearrange("p (k t) -> p k t", k=8)),
                            reads=["pt%d" % pi], writes=["ut%d" % ui])
            S.dma("act", lambda e, ui=ui, eg=eg: e.dma_start(
                out=C.UT[:, eg * 512:(eg + 1) * 512].rearrange("(kc p) n -> p kc n", p=128), in_=ut[ui][:]),
                reads=["ut%d" % ui])
        for vbk in vblks:
            bi = cnt["u"] % 2
            cnt["u"] += 1
            S.dma("sp", lambda e, bi=bi, vbk=vbk: e.dma_start(out=us[bi][:], in_=C.ev[vbk * 128:(vbk + 1) * 128, :]),
                  writes=["us%d" % bi])
            S.op("pool", lambda e, bi=bi: e.tensor_copy(out=ub[bi][:], in_=us[bi][:]), reads=["us%d" % bi], writes=["ub%d" % bi])
            S.dma("act", lambda e, bi=bi, vbk=vbk: e.dma_start(out=C.Vb[vbk * 128:(vbk + 1) * 128, :], in_=ub[bi][:]),
                  reads=["ub%d" % bi])
        S.barrier()
        S.flush()


def stage5b(nc, S, C, tiles=range(12), egs=range(32), dps=range(4), necs=16):
    with ExitStack() as st:
        xt = _sb(nc, st, "xt", [128, 32, 256], BF16)
        P2 = _sb(nc, st, "P2", [128, 2, 1024], F32)
        TH = _sb(nc, st, "TH", [128, 2, 1024], F32)
        WG = _sb(nc, st, "WG", [128, 2, 1024], F32)
        ut = [_sb(nc, st, "ut%d" % i, [128, 32, 512], BF16) for i in range(2)]
        gl = [_sb(nc, st, "gl%d" % i, [128, 512], F32) for i in range(2)]
        G = [_sb(nc, st, "G%d" % i, [128, 512], F32) for i in range(2)]
        mk = [_sb(nc, st, "mk%d" % i, [128, 8, 128], F32) for i in range(4)]
        Ab = [_sb(nc, st, "Ab%d" % i, [128, 512], BF16) for i in range(2)]
        at = [_sb(nc, st, "at%d" % i, [128, 4, 128], BF16) for i in range(2)]
        atc = [_sb(nc, st, "atc%d" % i, [128, 8, 256], BF16) for i in range(2)]
        vbc = [_sb(nc, st, "vbc%d" % i, [128, 8, 1024], BF16) for i in range(2)]
        ht = [_sb(nc, st, "ht%d" % i, [128, 512], F32) for i in range(2)]
        idb = _sb(nc, st, "idb", [128, 128], BF16)
        pm = [_ps(nc, st, "pm%d" % i, [128, 512], F32) for i in range(2)]
        pa = _ps(nc, st, "pa", [128, 1024], BF16)
        acc = [_ps(nc, st, "acc%d" % i, [128, 512], F32) for i in range(4)]
        S.dma("sp", lambda e: e.dma_start(out=idb[:], in_=C.ident_bf), writes=["idb"])
        cnt = {"ut": 0, "pm": 0, "gl": 0, "G": 0, "Ab": 0, "at": 0, "c": 0, "ht": 0}
        POOL_BB = (3,)

        def hblock(tt, eg, tkb):
            tok0 = tt * 256
            pi = cnt["pm"] % 2
            cnt["pm"] += 1
            ui = cnt["ut"] % 2
            for kc in range(32):
                S.op("pe", lambda e, kc=kc: e.matmul(out=pm[pi][:], lhsT=xt[:, kc, tkb * 128:(tkb + 1) * 128], rhs=ut[ui][:, kc, :],
                                                     start=(kc == 0), stop=(kc == 31)),
                     reads=["xt", "ut%d" % ui], writes=["pm%d" % pi])
            gi = cnt["gl"] % 2
            cnt["gl"] += 1
            S.op("act", lambda e: e.activation(out=gl[gi][:], in_=pm[pi][:], func=AF.Gelu_apprx_tanh),
                 reads=["pm%d" % pi], writes=["gl%d" % gi])
            p2v = P2[:, tkb, :].rearrange("p (h n) -> p h n", h=8)
            thv = TH[:, tkb, :].rearrange("p (h n) -> p h n", h=8)
            wgv = WG[:, tkb, :].rearrange("p (h n) -> p h n", h=8)
            Gi = cnt["G"] % 2
            cnt["G"] += 1
            Gt = G[Gi]
            for step in range(4):
                for bb in range(4):
                    b = eg * 4 + bb
                    en = "pool" if (bb in POOL_BB and step >= 1) else "dve"
                    m = mk[bb]
                    mkey = "mk%d" % bb
                    if step == 0:
                        S.op(en, lambda e, b=b, m=m: e.tensor_tensor(out=m[:], in0=p2v,
                                                                      in1=thv[:, :, b:b + 1].to_broadcast([128, 8, 128]), op=ALU.is_ge),
                             reads=["P2", "TH"], writes=[mkey])
                    elif step == 1:
                        S.op(en, lambda e, m=m: e.tensor_tensor(out=m[:], in0=m[:], in1=p2v, op=ALU.mult),
                             reads=[mkey, "P2"], writes=[mkey])
                    elif step == 2:
                        S.op(en, lambda e, b=b, m=m: e.tensor_tensor(out=m[:], in0=m[:],
                                                                      in1=wgv[:, :, b:b + 1].to_broadcast([128, 8, 128]), op=ALU.mult),
                             reads=[mkey, "WG"], writes=[mkey])
                    elif en == "dve":
                        S.op(en, lambda e, bb=bb, m=m: e.tensor_reduce(out=Gt[:, bb * 128:(bb + 1) * 128],
                                                                        in_=m[:].rearrange("p h n -> p n h"), axis=AX.X, op=ALU.add),
                             reads=[mkey], writes=[("G", Gi, bb)])
                    else:
                        S.op(en, lambda e, m=m: e.tensor_tensor(out=m[:, 0:4, :], in0=m[:, 0:4, :], in1=m[:, 4:8, :], op=ALU.add),
                             reads=[mkey], writes=[mkey])
                        S.op(en, lambda e, m=m: e.tensor_tensor(out=m[:, 0:2, :], in0=m[:, 0:2, :], in1=m[:, 2:4, :], op=ALU.add),
                             reads=[mkey], writes=[mkey])
                        S.op(en, lambda e, bb=bb, m=m: e.tensor_tensor(out=Gt[:, bb * 128:(bb + 1) * 128], in0=m[:, 0, :], in1=m[:, 1, :],
                                                                        op=ALU.add),
                             reads=[mkey], writes=[("G", Gi, bb)])
            ai = cnt["Ab"] % 2
            cnt["Ab"] += 1
            S.op("dve", lambda e: e.tensor_tensor(out=Ab[ai][:], in0=gl[gi][:], in1=Gt[:], op=ALU.mult),
                 reads=["gl%d" % gi] + [("G", Gi, bb) for bb in range(4)], writes=["Ab%d" % ai])
            for bb in range(4):
                S.op("pe", lambda e, bb=bb: e.transpose(out=pa[:, bb * 128:(bb + 1) * 128], in_=Ab[ai][:, bb * 128:(bb + 1) * 128],
                                                        identity=idb[:]), reads=["Ab%d" % ai, "idb"], writes=["pa"])
            ti = cnt["at"] % 2
            cnt["at"] += 1
            S.op("act", lambda e: e.activation(out=at[ti][:], in_=pa[:, 0:512].rearrange("p (b t) -> p b t", b=4), func=AF.Copy),
                 reads=["pa"], writes=["at%d" % ti])
            S.dma("act", lambda e: e.dma_start(
                out=C.AT[eg * 512:(eg + 1) * 512, tok0 + tkb * 128:tok0 + (tkb + 1) * 128].rearrange("(b p) t -> p b t", p=128),
                in_=at[ti][:]), reads=["at%d" % ti], writes=[("AT", tt, eg // 2)])

        def av_chunk(tt, dp, ec):
            tok0 = tt * 256
            ci = cnt["c"] % 2
            cnt["c"] += 1
            S.dma("sp", lambda e: e.dma_start(
                out=atc[ci][:], in_=C.AT[ec * 1024:(ec + 1) * 1024, tok0:tok0 + 256].rearrange("(k p) t -> p k t", p=128)),
                reads=[("AT", tt, ec)], writes=["atc%d" % ci])
            S.dma("sp", lambda e: e.dma_start(
                out=vbc[ci][:], in_=C.Vb[ec * 1024:(ec + 1) * 1024, dp * 1024:(dp + 1) * 1024].rearrange("(k p) n -> p k n", p=128)),
                writes=["vbc%d" % ci])
            for k in range(8):
                for tkb in range(2):
                    for dg in range(2):
                        ac = tkb * 2 + dg
                        S.op("pe", lambda e, k=k, tkb=tkb, dg=dg, ac=ac: e.matmul(
                            out=acc[ac][:], lhsT=atc[ci][:, k, tkb * 128:(tkb + 1) * 128],
                            rhs=vbc[ci][:, k, dg * 512:(dg + 1) * 512],
                            start=(ec == 0 and k == 0), stop=(ec == necs - 1 and k == 7)),
                            reads=["atc%d" % ci, "vbc%d" % ci], writes=["acc%d" % ac])
            if ec == necs - 1:
                for tkb in range(2):
                    for dg in range(2):
                        ac = tkb * 2 + dg
                        hi = cnt["ht"] % 2
                        cnt["ht"] += 1
                        r0 = tok0 + tkb * 128
                        c0 = dp * 1024 + dg * 512
                        S.dma("sp", lambda e, hi=hi, r0=r0, c0=c0: e.dma_start(out=ht[hi][:], in_=C.h[r0:r0 + 128, c0:c0 + 512]),
                              writes=["ht%d" % hi])
                        S.op("dve", lambda e, hi=hi, ac=ac: e.tensor_tensor(out=ht[hi][:], in0=acc[ac][:], in1=ht[hi][:], op=ALU.add),
                             reads=["acc%d" % ac, "ht%d" % hi], writes=["ht%d" % hi])
                        S.dma("act", lambda e, hi=hi, r0=r0, c0=c0: e.dma_start(out=C.hp[r0:r0 + 128, c0:c0 + 512], in_=ht[hi][:]),
                              reads=["ht%d" % hi])

        pending = []
        for tt in tiles:
            tok0 = tt * 256
            S.dma("sp", lambda e, tok0=tok0: e.dma_start(out=xt[:], in_=C.xn2T[:, tok0:tok0 + 256].rearrange("(kc p) t -> p kc t", p=128)),
                  writes=["xt"])
            for (dst, src, nm) in ((P2, C.P2, "P2"), (TH, C.TH, "TH"), (WG, C.WG, "WG")):
                S.dma("sp", lambda e, dst=dst, src=src, tok0=tok0: e.dma_start(
                    out=dst[:], in_=src[tok0:tok0 + 256, :].rearrange("(t p) n -> p t n", p=128)), writes=[nm])
            for eg in egs:
                ui = cnt["ut"] % 2
                S.dma("sp", lambda e, ui=ui, eg=eg: e.dma_start(
                    out=ut[ui][:], in_=C.UT[:, eg * 512:(eg + 1) * 512].rearrange("(kc p) n -> p kc n", p=128)),
                    writes=["ut%d" % ui])
                for tkb in range(2):
                    hblock(tt, eg, tkb)
                    if pending:
                        av_chunk(*pending.pop(0))
                cnt["ut"] += 1
            while pending:
                av_chunk(*pending.pop(0))
            pending = [(tt, dp, ec) for dp in dps for ec in range(necs)]
        while pending:
            av_chunk(*pending.pop(0))
        S.barrier()
        S.flush()


def stage6(nc, S, C, blks=range(24)):
    with ExitStack() as st:
        xs = [_sb(nc, st, "xs%d" % i, [128, D], F32) for i in range(2)]
        yo = [_sb(nc, st, "yo%d" % i, [128, D], F32) for i in range(2)]
        gb = _sb(nc, st, "gb", [128, D], F32)
        ss = _sb(nc, st, "ss", [128, 4], F32)
        epsb = _sb(nc, st, "epsb", [128, 1], F32)
        S.dma("sp", lambda e: e.dma_start(out=gb[:], in_=C.g_final.partition_broadcast(128)), writes=["gb"])
        S.op("dve", lambda e: e.memset(epsb[:], EPS), writes=["epsb"])
        for n, tb in enumerate(blks):
            i = n % 2
            S.dma("sp", lambda e, i=i, tb=tb: e.dma_start(out=xs[i][:], in_=C.hp[tb * 128:(tb + 1) * 128, :]), writes=["xs%d" % i])
            S.op("act", lambda e, i=i: e.activation(out=yo[i][:], in_=xs[i][:], func=AF.Square, accum_out=ss[:, 0:1]),
                 reads=["xs%d" % i], writes=["yo%d" % i, "ss0"])
            S.op("act", lambda e: e.activation(out=ss[:, 1:2], in_=ss[:, 0:1], func=AF.Sqrt, scale=1.0 / D, bias=epsb[:, 0:1]),
                 reads=["ss0", "epsb"], writes=["ss1"])
            S.op("dve", lambda e: e.reciprocal(out=ss[:, 2:3], in_=ss[:, 1:2]), reads=["ss1"], writes=["ss2"])
            S.op("dve", lambda e, i=i: e.scalar_tensor_tensor(out=yo[i][:], in0=xs[i][:], scalar=ss[:, 2:3], in1=gb[:],
                                                              op0=ALU.mult, op1=ALU.mult),
                 reads=["xs%d" % i, "ss2", "gb"], writes=["yo%d" % i])
            S.dma("act", lambda e, i=i, tb=tb: e.dma_start(out=C.y[tb * 128:(tb + 1) * 128, :], in_=yo[i][:]), reads=["yo%d" % i])
        S.barrier()
        S.flush()

def build(stages=("s1",), debug_outs=(), s1_tiles=(0, 1, 2, 3), s2_kw={}, s3_kw={}, s4_kw={}, s5a_kw={}, s5_kw={}, s6_kw={}):
    nc = bass.Bass("TRN2", target_bir_lowering=False)
    C = Ctx()

    def din(name, shape, dt=F32):
        return nc.dram_tensor(name, shape, dt, kind="ExternalInput").ap()

    def dscr(name, shape, dt):
        kind = "ExternalOutput" if name in debug_outs else "Internal"
        return nc.dram_tensor(name, shape, dt, kind=kind).ap()

    C.x = din("x", [TK, D])
    C.w_in = din("w_in", [D, INW])
    C.w_pa = din("w_proj_a", [AW, D])
    C.w_pb = din("w_proj_b", [BW, D])
    C.w_out = din("w_out", [D, D])
    C.g_mix = din("g_mix_norm", [1, D])
    C.lam = din("lam4", [1, 512])
    C.g_subln = din("g_subln", [1, 256])
    C.g_ffn = din("g_ffn_norm", [1, D])
    C.w_query = din("w_query", [D, 2048])
    C.sk1 = din("sub_keys_1", [8, 128, 128])
    C.sk2 = din("sub_keys_2", [8, 128, 128])
    C.eu = din("expert_u", [NE, D])
    C.ev = din("expert_v", [NE, D])
    C.g_final = din("g_final", [1, D])
    C.ident_bf = din("ident_bf", [128, 128], BF16)
    C.ident_f = din("ident_f", [128, 128], F32)
    C.abt = din("abt", [128, ABW])
    C.lct = din("lct", [128, ABW])
    C.y = nc.dram_tensor("y", [TQ, D], F32, kind="ExternalOutput").ap()

    C.qaT = dscr("qaT", [AW, TQ], BF16)
    C.kaT = dscr("kaT", [AW, TK], BF16)
    C.va = dscr("va", [TK, AW], BF16)
    C.qbT = dscr("qbT", [BW, TQ], BF16)
    C.kbT = dscr("kbT", [BW, TK], BF16)
    C.vb = dscr("vb", [TK, BW], BF16)
    C.sga = dscr("sga", [D, TQ], F32)
    C.sgb = dscr("sgb", [D, TQ], F32)
    C.oaT = dscr("oaT", [AW, TQ], BF16)
    C.obT = dscr("obT", [BW, TQ], BF16)
    C.h = dscr("h", [TQ, D], F32)
    C.xn2T = dscr("xn2T", [D, TQ], BF16)
    C.P2 = dscr("P2", [TQ, 1024], F32)
    C.TH = dscr("TH", [TQ, 1024], F32)
    C.WG = dscr("WG", [TQ, 1024], F32)
    C.UT = dscr("UT", [D, NE], BF16)
    C.Vb = dscr("Vb", [NE, D], BF16)
    C.AT = dscr("AT", [NE, TQ], BF16)
    C.hp = dscr("hp", [TQ, D], F32)

    with ExitStack() as st:
        S = Sched(nc, st)
        if "s1" in stages:
            stage1(nc, S, C, tiles=s1_tiles)
        if "s2" in stages:
            stage2(nc, S, C, **s2_kw)
        if "s3" in stages:
            stage3(nc, S, C, **s3_kw)
        if "s4" in stages:
            stage4(nc, S, C, **s4_kw)
        if "s5a" in stages:
            stage5a(nc, S, C, **s5a_kw)
        if "s5b" in stages:
            stage5b(nc, S, C, **s5_kw)
        if "s6" in stages:
            stage6(nc, S, C, **s6_kw)
        C.n_inst = S.n_inst
    return nc, C


def host_consts():
    p = np.arange(128)[:, None].astype(np.int64)
    c = np.arange(ABW)[None, :].astype(np.int64)
    delta = p - c + 1920
    ad = np.abs(delta)
    cnt = ((ad <= 64).astype(np.int64) + ((delta % 4 == 0) & (ad <= 256)).astype(np.int64)
           + ((delta % 16 == 0) & (ad <= 1024)).astype(np.int64))
    lct = np.where(cnt > 0, np.log(np.maximum(cnt, 1).astype(np.float64)), NEGBIG).astype(np.float32)
    return {
        "ident_bf": np.eye(128, dtype=np.float32).astype(ml_dtypes.bfloat16),
        "ident_f": np.eye(128, dtype=np.float32),
        "abt": ad.astype(np.float32),
        "lct": lct,
    }


def make_in_maps(inputs, n_cores=8):
    xp = np.asarray(inputs["x_prompt"])
    xsm = np.asarray(inputs["x_sample"])
    shared = {
        "w_in": np.ascontiguousarray(np.asarray(inputs["w_in"])[0]),
        "w_proj_a": np.ascontiguousarray(np.asarray(inputs["w_proj_a"])[0]),
        "w_proj_b": np.ascontiguousarray(np.asarray(inputs["w_proj_b"])[0]),
        "w_out": np.ascontiguousarray(np.asarray(inputs["w_out"])[0]),
        "g_mix_norm": np.ascontiguousarray(np.asarray(inputs["g_mix_norm"])[0:1]),
        "lam4": np.ascontiguousarray(np.concatenate([np.asarray(inputs[k])[0:1] for k in
                                                     ("lam_q1", "lam_k1", "lam_q2", "lam_k2")], axis=1)),
        "g_subln": np.ascontiguousarray(np.asarray(inputs["g_subln"])[0:1]),
        "g_ffn_norm": np.ascontiguousarray(np.asarray(inputs["g_ffn_norm"])[0:1]),
        "w_query": np.ascontiguousarray(np.asarray(inputs["w_query"])[0]),
        "sub_keys_1": np.ascontiguousarray(np.asarray(inputs["sub_keys_1"])[0]),
        "sub_keys_2": np.ascontiguousarray(np.asarray(inputs["sub_keys_2"])[0]),
        "expert_u": np.ascontiguousarray(np.asarray(inputs["expert_u"])[0]),
        "expert_v": np.ascontiguousarray(np.asarray(inputs["expert_v"])[0]),
        "g_final": np.ascontiguousarray(np.asarray(inputs["g_final"]).reshape(1, D)),
    }
    shared.update(host_consts())
    maps = []
    for c in range(n_cores):
        xb = xsm[c // 2]
        if c % 2 == 1:
            xb = xb[::-1]
        xall = np.ascontiguousarray(np.concatenate([xp[c], xb], axis=0).astype(np.float32))
        m = dict(shared)
        m["x"] = xall
        maps.append(m)
    return maps


def kernel(**inputs):
    nc, C = build(stages=ALL_STAGES)
    maps = make_in_maps(inputs)
    res = run_bass_kernel_spmd(nc, maps, core_ids=list(range(8)))
    y_prompt = np.zeros((8, SEQ, D), np.float32)
    y_sample = np.zeros((4, SEQ, D), np.float32)
    for c in range(8):
        y = np.asarray(res.results[c]["y"])
        y_prompt[c] = y[0:2048]
        if c % 2 == 0:
            y_sample[c // 2, 0:1024] = y[2048:3072]
        else:
            y_sample[c // 2, 1024:2048] = y[2048:3072][::-1]
    return (y_prompt, y_sample)
```

```python
import math
from contextlib import ExitStack

import numpy as np
import ml_dtypes

import concourse.bass as bass
import concourse.mybir as mybir
from concourse.alu_op_type import AluOpType as ALU
from concourse.bass_utils import run_bass_kernel_spmd

F32 = mybir.dt.float32
BF16 = mybir.dt.bfloat16
AF = mybir.ActivationFunctionType
AX = mybir.AxisListType

D = 4096
SEQ = 2048
TQ = 3072
TK = 4096
AW = 2048
BW = 2048
INW = 20480
NE = 16384
EPS = 1e-6
SCALE = 128 ** -0.5
LAM_INIT = 0.8 - 0.6 * math.exp(-0.0)
NEGBIG = -30000.0
ABW = 3968

ALL_STAGES = ("s1", "s2", "s3", "s4", "s5a", "s5b", "s6")
ENGS = ("pe", "dve", "act", "pool", "sp")
NDMA = 8


class Sched:
    def __init__(self, nc, stack):
        self.nc = nc
        self.sem = {e: stack.enter_context(nc.semaphore("s_" + e)) for e in ENGS}
        self.dsem = {q: [stack.enter_context(nc.semaphore("d_%s%d" % (q, i))) for i in range(NDMA)]
                     for q in ("sp", "act", "pool")}
        self.cnt = {e: 0 for e in ENGS}
        self.dcnt = {q: 0 for q in self.dsem}
        self.lists = {e: [] for e in ENGS}
        self.waited = {e: {} for e in ENGS}
        self.lastw = {}
        self.readers = {}
        self.n_inst = 0

    def _semobj(self, key):
        if isinstance(key, tuple):
            return self.dsem[key[0]][key[1]]
        return self.sem[key]

    def _need(self, eng, ev, waits):
        if ev is None:
            return
        key, val = ev
        if self.waited[eng].get(key, 0) >= val:
            return
        self.waited[eng][key] = val
        waits.append((key, val))

    def _deps(self, eng, reads, writes, waits):
        for r in reads:
            self._need(eng, self.lastw.get(r), waits)
        for w in writes:
            self._need(eng, self.lastw.get(w), waits)
            for ev in self.readers.get(w, ()):
                self._need(eng, ev, waits)

    def _commit(self, ev, reads, writes):
        for r in reads:
            self.readers.setdefault(r, []).append(ev)
        for w in writes:
            self.lastw[w] = ev
            self.readers[w] = []

    def op(self, eng, fn, reads=(), writes=()):
        waits = []
        self._deps(eng, reads, writes, waits)
        if eng == "pe":
            waits = [w for w in waits if w[0] != "pe"]
        self.cnt[eng] += 1
        ev = (eng, self.cnt[eng])
        self.lists[eng].append((fn, waits, (eng, 1)))
        self._commit(ev, reads, writes)
        self.n_inst += 1 + len(waits)
        return ev

    def dma(self, q, fn, reads=(), writes=()):
        waits = []
        j = self.dcnt[q]
        slot = j % NDMA
        prev = 16 * (j // NDMA)
        key = (q, slot)
        if prev > 0:
            self._need(q, (key, prev), waits)
        self._deps(q, reads, writes, waits)
        self.dcnt[q] += 1
        ev = (key, prev + 16)
        self.lists[q].append((fn, waits, (key, 16)))
        self._commit(ev, reads, writes)
        self.n_inst += 1 + len(waits)
        return ev

    def barrier(self):
        evs = [(e, self.cnt[e]) for e in ENGS if self.cnt[e] > 0]
        for q in self.dsem:
            j = self.dcnt[q]
            for slot in range(NDMA):
                if j > slot:
                    n = (j - 1 - slot) // NDMA + 1
                    evs.append(((q, slot), 16 * n))
        for e in ENGS:
            waits = []
            for ev in evs:
                self._need(e, ev, waits)
            if waits:
                self.lists[e].append((None, waits, None))
        self.lastw = {}
        self.readers = {}

    def flush(self):
        nc = self.nc
        lists = self.lists
        self.lists = {e: [] for e in ENGS}
        sched = self

        def run(engobj, lst):
            for fn, waits, inc in lst:
                for key, val in waits:
                    engobj.wait_ge(sched._semobj(key), val)
                if fn is not None:
                    ins = fn(engobj)
                    ins.then_inc(sched._semobj(inc[0]), inc[1])

        with nc.Block() as block:
            @block.tensor
            def _(e):
                run(e, lists["pe"])

            @block.vector
            def _(e):
                run(e, lists["dve"])

            @block.scalar
            def _(e):
                run(e, lists["act"])

            @block.gpsimd
            def _(e):
                run(e, lists["pool"])

            @block.sync
            def _(e):
                run(e, lists["sp"])


class Ctx:
    pass


_uid = [0]


def _sb(nc, st, name, shape, dt):
    _uid[0] += 1
    return st.enter_context(nc.sbuf_tensor("sb%d_%s" % (_uid[0], name), shape, dt))


def _ps(nc, st, name, shape, dt):
    _uid[0] += 1
    return st.enter_context(nc.psum_tensor("ps%d_%s" % (_uid[0], name), shape, dt))


def norm_transpose(S, T, src_rows, col0, uid):
    xs, gb, xnb, ss, idb, pt, xnT = T["xs"], T["gb"], T["xnb"], T["ss"], T["idb"], T["pt"], T["xnT"]
    S.dma("sp", lambda e: e.dma_start(out=xs[:], in_=src_rows), writes=["xs"])
    S.op("act", lambda e: e.activation(out=xnb[:], in_=xs[:], func=AF.Square, accum_out=ss[:, 0:1]),
         reads=["xs"], writes=["xnb", "ss0"])
    S.op("act", lambda e: e.activation(out=ss[:, 1:2], in_=ss[:, 0:1], func=AF.Sqrt, scale=1.0 / D, bias=T["epsb"][:, 0:1]),
         reads=["ss0", "epsb"], writes=["ss1"])
    S.op("dve", lambda e: e.reciprocal(out=ss[:, 2:3], in_=ss[:, 1:2]), reads=["ss1"], writes=["ss2"])
    S.op("dve", lambda e: e.scalar_tensor_tensor(out=xnb[:], in0=xs[:], scalar=ss[:, 2:3], in1=gb[:],
                                                 op0=ALU.mult, op1=ALU.mult),
         reads=["xs", "ss2", "gb"], writes=["xnb"])
    for g in range(4):
        p = pt[g % 2]
        pk = "pt%d" % (g % 2)
        for k in range(8):
            kc = g * 8 + k
            S.op("pe", lambda e, p=p, k=k, kc=kc: e.transpose(out=p[:, k * 128:(k + 1) * 128],
                                                               in_=xnb[:, kc * 128:(kc + 1) * 128],
                                                               identity=idb[:]),
                 reads=["xnb", "idb"], writes=[pk])
        eng = "act" if g % 2 == 0 else "dve"
        if eng == "act":
            S.op("act", lambda e, p=p, g=g: e.activation(
                out=xnT[:, g * 8:(g + 1) * 8, col0:col0 + 128],
                in_=p[:].rearrange("p (k t) -> p k t", k=8), func=AF.Copy),
                reads=[pk], writes=[("xnT", uid, g)])
        else:
            S.op("dve", lambda e, p=p, g=g: e.tensor_copy(
                out=xnT[:, g * 8:(g + 1) * 8, col0:col0 + 128],
                in_=p[:].rearrange("p (k t) -> p k t", k=8)),
                reads=[pk], writes=[("xnT", uid, g)])


def xnT_keys(uids):
    return [("xnT", u, g) for u in uids for g in range(4)]


def stage1(nc, S, C, tiles=(0, 1, 2, 3)):
    with ExitStack() as st:
        T = {}
        T["xnT"] = _sb(nc, st, "xnT", [128, 32, 1024], BF16)
        T["xs"] = _sb(nc, st, "xs", [128, D], F32)
        T["gb"] = _sb(nc, st, "gb", [128, D], F32)
        T["xnb"] = _sb(nc, st, "xnb", [128, D], BF16)
        T["ss"] = _sb(nc, st, "ss", [128, 4], F32)
        T["idb"] = _sb(nc, st, "idb", [128, 128], BF16)
        T["epsb"] = _sb(nc, st, "epsb", [128, 1], F32)
        S.op("dve", lambda e: e.memset(T["epsb"][:], EPS), writes=["epsb"])
        wst = [_sb(nc, st, "wst%d" % i, [128, 32, 128], F32) for i in range(2)]
        wb = [_sb(nc, st, "wb%d" % i, [128, 32, 128], BF16) for i in range(2)]
        ob = [_sb(nc, st, "ob%d" % i, [128, 512], BF16) for i in range(2)]
        of = [_sb(nc, st, "of%d" % i, [128, 512], F32) for i in range(2)]
        vt = [_sb(nc, st, "vt%d" % i, [128, 4, 128], BF16) for i in range(2)]
        pm = [_ps(nc, st, "pm%d" % i, [128, 512], F32) for i in range(4)]
        T["pt"] = [_ps(nc, st, "pt%d" % i, [128, 1024], BF16) for i in range(2)]
        pv = [_ps(nc, st, "pv%d" % i, [128, 1024], BF16) for i in range(2)]
        xnT, idb = T["xnT"], T["idb"]

        S.dma("sp", lambda e: e.dma_start(out=T["gb"][:], in_=C.g_mix.partition_broadcast(128)), writes=["gb"])
        S.dma("sp", lambda e: e.dma_start(out=idb[:], in_=C.ident_bf), writes=["idb"])

        cnt = {"w": 0, "pm": 0, "ob": 0, "of": 0, "vt": 0}
        w_in = C.w_in
        for tt in tiles:
            tok0 = tt * 1024
            for tb in range(8):
                norm_transpose(S, T, C.x[tok0 + tb * 128: tok0 + (tb + 1) * 128, :], tb * 128, tb)
            xkeys = xnT_keys(range(8))
            plan = []
            kv_only = (tt == 3)
            for fb in range(160):
                if fb < 16:
                    if not kv_only:
                        plan.append((fb, "qk", C.qaT, fb))
                elif fb < 32:
                    plan.append((fb, "qk", C.kaT, fb - 16))
                elif fb < 48:
                    plan.append((fb, "v", C.va, fb - 32))
                elif fb < 64:
                    if not kv_only:
                        plan.append((fb, "qk", C.qbT, fb - 48))
                elif fb < 80:
                    plan.append((fb, "qk", C.kbT, fb - 64))
                elif fb < 96:
                    plan.append((fb, "v", C.vb, fb - 80))
                elif fb < 128:
                    if not kv_only:
                        plan.append((fb, "gate", C.sga, fb - 96))
                else:
                    if not kv_only:
                        plan.append((fb, "gate", C.sgb, fb - 128))
            for (fb, kind, dst, db) in plan:
                wi = cnt["w"] % 2
                cnt["w"] += 1
                S.dma("sp", lambda e, wi=wi, fb=fb: e.dma_start(
                    out=wst[wi][:], in_=w_in[:, fb * 128:(fb + 1) * 128].rearrange("(kc p) f -> p kc f", p=128)),
                    writes=["wst%d" % wi])
                S.op("dve", lambda e, wi=wi: e.tensor_copy(out=wb[wi][:], in_=wst[wi][:]),
                     reads=["wst%d" % wi], writes=["wb%d" % wi])
                for half in range(2):
                    pi = cnt["pm"] % 4
                    cnt["pm"] += 1
                    for kc in range(32):
                        S.op("pe", lambda e, pi=pi, wi=wi, kc=kc, half=half: e.matmul(
                            out=pm[pi][:], lhsT=wb[wi][:, kc, :], rhs=xnT[:, kc, half * 512:(half + 1) * 512],
                            start=(kc == 0), stop=(kc == 31)),
                            reads=["wb%d" % wi] + (xkeys if kc in (0, 31) else []), writes=["pm%d" % pi])
                    c0 = tok0 + half * 512
                    if kind == "qk":
                        oi = cnt["ob"] % 2
                        cnt["ob"] += 1
                        S.op("act", lambda e, oi=oi, pi=pi: e.activation(out=ob[oi][:], in_=pm[pi][:], func=AF.Copy),
                             reads=["pm%d" % pi], writes=["ob%d" % oi])
                        S.dma("act", lambda e, oi=oi, dst=dst, db=db, c0=c0: e.dma_start(
                            out=dst[db * 128:(db + 1) * 128, c0:c0 + 512], in_=ob[oi][:]),
                            reads=["ob%d" % oi])
                    elif kind == "gate":
                        oi = cnt["of"] % 2
                        cnt["of"] += 1
                        S.op("act", lambda e, oi=oi, pi=pi: e.activation(out=of[oi][:], in_=pm[pi][:], func=AF.Sigmoid),
                             reads=["pm%d" % pi], writes=["of%d" % oi])
                        S.dma("act", lambda e, oi=oi, dst=dst, db=db, c0=c0: e.dma_start(
                            out=dst[db * 128:(db + 1) * 128, c0:c0 + 512], in_=of[oi][:]),
                            reads=["of%d" % oi])
                    else:
                        oi = cnt["ob"] % 2
                        cnt["ob"] += 1
                        S.op("act", lambda e, oi=oi, pi=pi: e.activation(out=ob[oi][:], in_=pm[pi][:], func=AF.Copy),
                             reads=["pm%d" % pi], writes=["ob%d" % oi])
                        vi = cnt["vt"] % 2
                        cnt["vt"] += 1
                        for t in range(4):
                            S.op("pe", lambda e, oi=oi, vi=vi, t=t: e.transpose(
                                out=pv[vi][:, t * 128:(t + 1) * 128], in_=ob[oi][:, t * 128:(t + 1) * 128],
                                identity=idb[:]), reads=["ob%d" % oi, "idb"], writes=["pv%d" % vi])
                        S.op("dve", lambda e, vi=vi: e.tensor_copy(
                            out=vt[vi][:], in_=pv[vi][:, 0:512].rearrange("p (t c) -> p t c", t=4)),
                            reads=["pv%d" % vi], writes=["vt%d" % vi])
                        S.dma("act", lambda e, vi=vi, dst=dst, db=db, c0=c0: e.dma_start(
                            out=dst[c0:c0 + 512, db * 128:(db + 1) * 128].rearrange("(t p) c -> p t c", p=128),
                            in_=vt[vi][:]), reads=["vt%d" % vi])
        S.barrier()
        S.flush()


def stage2(nc, S, C, seqs=((0, 2048, 0), (2048, 1024, 2048)), a_heads=range(16), b_heads=range(8)):
    with ExitStack() as st:
        idb = _sb(nc, st, "idb2", [128, 128], BF16)
        abt = _sb(nc, st, "abt", [128, ABW], F32)
        lct = _sb(nc, st, "lct", [128, ABW], F32)
        tb = [_sb(nc, st, "tb%d" % i, [128, ABW], F32) for i in range(2)]
        qT = [_sb(nc, st, "qT%d" % i, [128, 2, 2048], BF16) for i in range(2)]
        kT = [_sb(nc, st, "kT%d" % i, [128, 2, 2048], BF16) for i in range(2)]
        vS = [_sb(nc, st, "vS%d" % i, [128, 16, 256], BF16) for i in range(2)]
        St = [[_sb(nc, st, "St%d_%d" % (i, p), [128, 2048], F32) for p in range(2)] for i in range(2)]
        Pf = [[_sb(nc, st, "Pf%d_%d" % (i, p), [128, 2048], F32) for p in range(2)] for i in range(2)]
        Pbs = [_sb(nc, st, "Pb%d" % p, [128, 2048], BF16) for p in range(2)]
        aT = [_sb(nc, st, "aT%d" % i, [128, 16, 128], BF16) for i in range(2)]
        sms = [_sb(nc, st, "sm%d" % p, [128, 16], F32) for p in range(2)]
        oS = _sb(nc, st, "oS", [128, 256], F32)
        junk = _sb(nc, st, "junk2", [128, 256], F32)
        ob16 = _sb(nc, st, "ob16", [128, 256], BF16)
        oT = [_sb(nc, st, "oT%d" % i, [128, 2, 128], BF16) for i in range(2)]
        gsub = _sb(nc, st, "gsub", [128, 256], F32)
        lamt = _sb(nc, st, "lamt", [128, 512], F32)
        lamv = _sb(nc, st, "lamv", [128, 8], F32)
        epsb = _sb(nc, st, "epsb2", [128, 1], F32)
        ps = [_ps(nc, st, "ps%d" % i, [128, 512], F32) for i in range(4)]
        ptr = [_ps(nc, st, "ptr%d" % i, [128, 1024], BF16) for i in range(2)]
        po = _ps(nc, st, "po", [128, 512], F32)
        pot = _ps(nc, st, "pot", [128, 1024], BF16)

        S.dma("sp", lambda e: e.dma_start(out=idb[:], in_=C.ident_bf), writes=["idb"])
        S.dma("sp", lambda e: e.dma_start(out=abt[:], in_=C.abt), writes=["abt"])
        S.dma("sp", lambda e: e.dma_start(out=lct[:], in_=C.lct), writes=["lct"])
        S.dma("sp", lambda e: e.dma_start(out=gsub[:], in_=C.g_subln.partition_broadcast(128)), writes=["gsub"])
        S.dma("sp", lambda e: e.dma_start(out=lamt[:], in_=C.lam.partition_broadcast(128)), writes=["lamt"])
        S.op("dve", lambda e: e.memset(epsb[:], EPS), writes=["epsb"])
        S.op("dve", lambda e: e.tensor_scalar_mul(out=gsub[:], in0=gsub[:], scalar1=1.0 - LAM_INIT),
             reads=["gsub"], writes=["gsub"])
        for j in range(2):
            S.op("dve", lambda e, j=j: e.tensor_tensor(out=junk[:, 0:128], in0=lamt[:, j * 256:j * 256 + 128],
                                                       in1=lamt[:, j * 256 + 128:j * 256 + 256], op=ALU.mult),
                 reads=["lamt"], writes=["junk"])
            S.op("dve", lambda e, j=j: e.tensor_reduce(out=lamv[:, j:j + 1], in_=junk[:, 0:128], axis=AX.X, op=ALU.add),
                 reads=["junk"], writes=["lamv%d" % j])
            S.op("act", lambda e, j=j: e.activation(out=lamv[:, 2 + j:3 + j], in_=lamv[:, j:j + 1], func=AF.Exp),
                 reads=["lamv%d" % j], writes=["lame%d" % j])
        S.op("dve", lambda e: e.tensor_tensor(out=lamv[:, 4:5], in0=lamv[:, 2:3], in1=lamv[:, 3:4], op=ALU.subtract),
             reads=["lame0", "lame1"], writes=["lamd"])
        S.op("dve", lambda e: e.tensor_scalar(out=lamv[:, 5:6], in0=lamv[:, 4:5], scalar1=LAM_INIT, scalar2=-1.0,
                                              op0=ALU.add, op1=ALU.mult), reads=["lamd"], writes=["nlam"])

        cnt = {"h": 0, "ps": 0, "aT": 0, "oT": 0}

        def head(kind, h, qbase, nq, kbase):
            b = cnt["h"] % 2
            cnt["h"] += 1
            nj = 1 if kind == "a" else 2
            ve = 128 if kind == "a" else 256
            if kind == "a":
                slope = 2.0 ** (-8.0 * (h + 1) / 16)
                qsrc, ksrc, vsrc, odst = C.qaT, C.kaT, C.va, C.oaT
            else:
                slope = 2.0 ** (-8.0 * (h + 1) / 8)
                qsrc, ksrc, vsrc, odst = C.qbT, C.kbT, C.vb, C.obT
            f0 = h * 128 * nj
            for j in range(nj):
                S.dma("sp", lambda e, j=j: e.dma_start(out=qT[b][:, j, 0:nq],
                                                        in_=qsrc[f0 + j * 128:f0 + (j + 1) * 128, qbase:qbase + nq]),
                      writes=["qT%d" % b])
                S.dma("sp", lambda e, j=j: e.dma_start(out=kT[b][:, j, :],
                                                        in_=ksrc[f0 + j * 128:f0 + (j + 1) * 128, kbase:kbase + 2048]),
                      writes=["kT%d" % b])
            S.dma("sp", lambda e: e.dma_start(
                out=vS[b][:, :, 0:ve],
                in_=vsrc[kbase:kbase + 2048, h * ve:(h + 1) * ve].rearrange("(kb p) c -> p kb c", p=128)),
                writes=["vS%d" % b])
            if kind == "a":
                S.op("dve", lambda e: e.scalar_tensor_tensor(out=tb[b][:], in0=abt[:], scalar=-slope, in1=lct[:],
                                                             op0=ALU.mult, op1=ALU.add),
                     reads=["abt", "lct"], writes=["tb%d" % b])
            else:
                S.op("dve", lambda e: e.tensor_scalar_mul(out=tb[b][:], in0=abt[:], scalar1=-slope),
                     reads=["abt"], writes=["tb%d" % b])
            def front(i):
                off = 1920 - 128 * i
                p = i % 2
                sm = sms[p]
                Pb = Pbs[p]
                for j in range(nj):
                    for c in range(4):
                        r = cnt["ps"] % 4
                        cnt["ps"] += 1
                        S.op("pe", lambda e, r=r, j=j, c=c: e.matmul(
                            out=ps[r][:], lhsT=qT[b][:, j, i * 128:(i + 1) * 128],
                            rhs=kT[b][:, j, c * 512:(c + 1) * 512], start=True, stop=True),
                            reads=["qT%d" % b, "kT%d" % b], writes=["ps%d" % r])
                        S.op("dve", lambda e, r=r, j=j, c=c: e.scalar_tensor_tensor(
                            out=St[j][p][:, c * 512:(c + 1) * 512], in0=ps[r][:], scalar=SCALE,
                            in1=tb[b][:, off + c * 512:off + (c + 1) * 512], op0=ALU.mult, op1=ALU.add),
                            reads=["ps%d" % r, "tb%d" % b], writes=[("St", j, p)])
                    S.op("dve", lambda e, j=j: e.tensor_reduce(out=sm[:, j:j + 1], in_=St[j][p][:], axis=AX.X, op=ALU.max),
                         reads=[("St", j, p)], writes=[("mx", j, p)])
                    S.op("dve", lambda e, j=j: e.tensor_scalar_mul(out=sm[:, 2 + j:3 + j], in0=sm[:, j:j + 1], scalar1=-1.0),
                         reads=[("mx", j, p)], writes=[("nm", j, p)])
                    if kind == "a":
                        S.op("act", lambda e: e.activation(out=Pb[:], in_=St[0][p][:], func=AF.Exp, bias=sm[:, 2:3],
                                                           accum_out=sm[:, 4:5]),
                             reads=[("St", 0, p), ("nm", 0, p)], writes=[("Pb", p), ("rs", 0, p)])
                    else:
                        S.op("act", lambda e, j=j: e.activation(out=Pf[j][p][:], in_=St[j][p][:], func=AF.Exp,
                                                                bias=sm[:, 2 + j:3 + j], accum_out=sm[:, 4 + j:5 + j]),
                             reads=[("St", j, p), ("nm", j, p)], writes=[("Pf", j, p), ("rs", j, p)])
                if kind == "b":
                    S.op("dve", lambda e: e.reciprocal(out=sm[:, 6:8], in_=sm[:, 4:6]), reads=[("rs", 0, p), ("rs", 1, p)], writes=[("rinv", p)])
                    S.op("dve", lambda e: e.scalar_tensor_tensor(out=sm[:, 8:9], in0=sm[:, 4:5], scalar=sm[:, 7:8],
                                                                 in1=lamv[:, 5:6], op0=ALU.mult, op1=ALU.mult),
                         reads=[("rs", 0, p), ("rinv", p), "nlam"], writes=[("ratio", p)])
                    S.op("dve", lambda e: e.scalar_tensor_tensor(out=Pb[:], in0=Pf[1][p][:], scalar=sm[:, 8:9], in1=Pf[0][p][:],
                                                                 op0=ALU.mult, op1=ALU.add),
                         reads=[("Pf", 0, p), ("Pf", 1, p), ("ratio", p)], writes=[("Pb", p)])
                else:
                    S.op("dve", lambda e: e.reciprocal(out=sm[:, 6:7], in_=sm[:, 4:5]), reads=[("rs", 0, p)], writes=[("rinv", p)])

            def back(i):
                p = i % 2
                sm = sms[p]
                Pb = Pbs[p]
                ai = cnt["aT"] % 2
                cnt["aT"] += 1
                for g in range(2):
                    for k in range(8):
                        kb = g * 8 + k
                        S.op("pe", lambda e, g=g, k=k, kb=kb: e.transpose(
                            out=ptr[g][:, k * 128:(k + 1) * 128], in_=Pb[:, kb * 128:(kb + 1) * 128], identity=idb[:]),
                            reads=[("Pb", p), "idb"], writes=["ptr%d" % g])
                    S.op("act", lambda e, g=g: e.activation(out=aT[ai][:, g * 8:(g + 1) * 8, :],
                                                            in_=ptr[g][:].rearrange("p (k t) -> p k t", k=8), func=AF.Copy),
                         reads=["ptr%d" % g], writes=[("aT", ai, g)])
                for kb in range(16):
                    S.op("pe", lambda e, kb=kb: e.matmul(out=po[:, 0:ve], lhsT=aT[ai][:, kb, :], rhs=vS[b][:, kb, 0:ve],
                                                         start=(kb == 0), stop=(kb == 15)),
                         reads=[("aT", ai, kb // 8), "vS%d" % b], writes=["po"])
                oi = cnt["oT"] % 2
                cnt["oT"] += 1
                qc = qbase + i * 128
                if kind == "a":
                    S.op("act", lambda e: e.activation(out=ob16[:, 0:128], in_=po[:, 0:128], func=AF.Copy, scale=sm[:, 6:7]),
                         reads=["po", ("rinv", p)], writes=["ob16"])
                    S.op("pe", lambda e: e.transpose(out=pot[:, 0:128], in_=ob16[:, 0:128], identity=idb[:]),
                         reads=["ob16", "idb"], writes=["pot"])
                    S.op("dve", lambda e: e.tensor_copy(out=oT[oi][:, 0, :], in_=pot[:, 0:128]),
                         reads=["pot"], writes=["oT%d" % oi])
                    S.dma("act", lambda e: e.dma_start(out=odst[h * 128:(h + 1) * 128, qc:qc + 128], in_=oT[oi][:, 0, :]),
                          reads=["oT%d" % oi])
                else:
                    S.op("act", lambda e: e.activation(out=oS[:], in_=po[:, 0:256], func=AF.Copy, scale=sm[:, 6:7]),
                         reads=["po", ("rinv", p)], writes=["oS"])
                    S.op("act", lambda e: e.activation(out=junk[:], in_=oS[:], func=AF.Square, accum_out=sm[:, 9:10]),
                         reads=["oS"], writes=["junk", ("ssq", p)])
                    S.op("act", lambda e: e.activation(out=sm[:, 10:11], in_=sm[:, 9:10], func=AF.Sqrt, scale=1.0 / 256,
                                                       bias=epsb[:, 0:1]), reads=[("ssq", p), "epsb"], writes=[("srt", p)])
                    S.op("dve", lambda e: e.reciprocal(out=sm[:, 11:12], in_=sm[:, 10:11]), reads=[("srt", p)], writes=[("rstd", p)])
                    S.op("dve", lambda e: e.scalar_tensor_tensor(out=ob16[:], in0=oS[:], scalar=sm[:, 11:12], in1=gsub[:],
                                                                 op0=ALU.mult, op1=ALU.mult),
                         reads=["oS", ("rstd", p), "gsub"], writes=["ob16"])
                    for t in range(2):
                        S.op("pe", lambda e, t=t: e.transpose(out=pot[:, t * 128:(t + 1) * 128],
                                                              in_=ob16[:, t * 128:(t + 1) * 128], identity=idb[:]),
                             reads=["ob16", "idb"], writes=["pot"])
                    S.op("dve", lambda e: e.tensor_copy(out=oT[oi][:], in_=pot[:, 0:256].rearrange("p (t c) -> p t c", t=2)),
                         reads=["pot"], writes=["oT%d" % oi])
                    S.dma("act", lambda e: e.dma_start(
                        out=odst[h * 256:(h + 1) * 256, qc:qc + 128].rearrange("(t p) c -> p t c", p=128), in_=oT[oi][:]),
                        reads=["oT%d" % oi])


            nqb = nq // 128
            front(0)
            for i in range(nqb):
                if i + 1 < nqb:
                    front(i + 1)
                back(i)

        for (qbase, nq, kbase) in seqs:
            for h in a_heads:
                head("a", h, qbase, nq, kbase)
            for h in b_heads:
                head("b", h, qbase, nq, kbase)
        S.barrier()
        S.flush()


def stage3(nc, S, C, tiles=range(3)):
    with ExitStack() as st:
        oat = _sb(nc, st, "oat", [128, 16, 1024], BF16)
        obt = _sb(nc, st, "obt", [128, 16, 1024], BF16)
        mT = _sb(nc, st, "mT", [128, 32, 1024], BF16)
        wst = [_sb(nc, st, "wst%d" % i, [128, 32, 128], F32) for i in range(2)]
        wb = [_sb(nc, st, "wb%d" % i, [128, 32, 128], BF16) for i in range(2)]
        sg = [_sb(nc, st, "sg%d" % i, [128, 2, 512], F32) for i in range(2)]
        m1 = _sb(nc, st, "m1", [128, 512], F32)
        m2 = _sb(nc, st, "m2", [128, 512], F32)
        hf = [_sb(nc, st, "hf%d" % i, [128, 512], F32) for i in range(2)]
        xt = [_sb(nc, st, "xt%d" % i, [128, 4, 128], F32) for i in range(2)]
        ho = [_sb(nc, st, "ho%d" % i, [128, 4, 128], F32) for i in range(2)]
        idf = _sb(nc, st, "idf", [128, 128], F32)
        pm = [_ps(nc, st, "pm%d" % i, [128, 512], F32) for i in range(4)]
        pT = [_ps(nc, st, "pT%d" % i, [128, 512], F32) for i in range(2)]
        S.dma("sp", lambda e: e.dma_start(out=idf[:], in_=C.ident_f), writes=["idf"])
        cnt = {"w": 0, "pm": 0, "sg": 0, "hf": 0, "xt": 0, "pT": 0}
        for tt in tiles:
            t0 = tt * 1024
            S.dma("sp", lambda e, t0=t0: e.dma_start(out=oat[:], in_=C.oaT[:, t0:t0 + 1024].rearrange("(kc p) t -> p kc t", p=128)),
                  writes=["oat"])
            S.dma("sp", lambda e, t0=t0: e.dma_start(out=obt[:], in_=C.obT[:, t0:t0 + 1024].rearrange("(kc p) t -> p kc t", p=128)),
                  writes=["obt"])
            for fb in range(32):
                wi = cnt["w"] % 2
                cnt["w"] += 1
                S.dma("sp", lambda e, wi=wi, fb=fb: e.dma_start(
                    out=wst[wi][:, 0:16, :], in_=C.w_pa[:, fb * 128:(fb + 1) * 128].rearrange("(kc p) f -> p kc f", p=128)),
                    writes=["wst%d" % wi])
                S.dma("sp", lambda e, wi=wi, fb=fb: e.dma_start(
                    out=wst[wi][:, 16:32, :], in_=C.w_pb[:, fb * 128:(fb + 1) * 128].rearrange("(kc p) f -> p kc f", p=128)),
                    writes=["wst%d" % wi])
                S.op("dve", lambda e, wi=wi: e.tensor_copy(out=wb[wi][:], in_=wst[wi][:]),
                     reads=["wst%d" % wi], writes=["wb%d" % wi])
                for half in range(2):
                    c0 = t0 + half * 512
                    hs = slice(half * 512, (half + 1) * 512)
                    si = cnt["sg"] % 2
                    cnt["sg"] += 1
                    S.dma("sp", lambda e, si=si, fb=fb, c0=c0: e.dma_start(out=sg[si][:, 0, :], in_=C.sga[fb * 128:(fb + 1) * 128, c0:c0 + 512]),
                          writes=["sg%d" % si])
                    S.dma("sp", lambda e, si=si, fb=fb, c0=c0: e.dma_start(out=sg[si][:, 1, :], in_=C.sgb[fb * 128:(fb + 1) * 128, c0:c0 + 512]),
                          writes=["sg%d" % si])
                    pa = cnt["pm"] % 4
                    pb = (cnt["pm"] + 1) % 4
                    cnt["pm"] += 2
                    for kc in range(16):
                        S.op("pe", lambda e, pa=pa, wi=wi, kc=kc, hs=hs: e.matmul(out=pm[pa][:], lhsT=wb[wi][:, kc, :], rhs=oat[:, kc, hs],
                                                                                   start=(kc == 0), stop=(kc == 15)),
                             reads=["wb%d" % wi, "oat"], writes=["pm%d" % pa])
                    for kc in range(16):
                        S.op("pe", lambda e, pb=pb, wi=wi, kc=kc, hs=hs: e.matmul(out=pm[pb][:], lhsT=wb[wi][:, 16 + kc, :], rhs=obt[:, kc, hs],
                                                                                   start=(kc == 0), stop=(kc == 15)),
                             reads=["wb%d" % wi, "obt"], writes=["pm%d" % pb])
                    S.op("dve", lambda e, pa=pa, si=si: e.tensor_tensor(out=m1[:], in0=pm[pa][:], in1=sg[si][:, 0, :], op=ALU.mult),
                         reads=["pm%d" % pa, "sg%d" % si], writes=["m1"])
                    S.op("dve", lambda e, pb=pb, si=si: e.tensor_tensor(out=m2[:], in0=pm[pb][:], in1=sg[si][:, 1, :], op=ALU.mult),
                         reads=["pm%d" % pb, "sg%d" % si], writes=["m2"])
                    S.op("dve", lambda e, fb=fb, hs=hs: e.tensor_tensor(out=mT[:, fb, hs], in0=m1[:], in1=m2[:], op=ALU.add),
                         reads=["m1", "m2"], writes=[("mT", fb)])
            mkeys = [("mT", fb) for fb in range(32)]
            for fb in range(32):
                wi = cnt["w"] % 2
                cnt["w"] += 1
                S.dma("sp", lambda e, wi=wi, fb=fb: e.dma_start(
                    out=wst[wi][:], in_=C.w_out[:, fb * 128:(fb + 1) * 128].rearrange("(kc p) f -> p kc f", p=128)),
                    writes=["wst%d" % wi])
                S.op("dve", lambda e, wi=wi: e.tensor_copy(out=wb[wi][:], in_=wst[wi][:]),
                     reads=["wst%d" % wi], writes=["wb%d" % wi])
                for half in range(2):
                    c0 = t0 + half * 512
                    hs = slice(half * 512, (half + 1) * 512)
                    xi = cnt["xt"] % 2
                    cnt["xt"] += 1
                    S.dma("sp", lambda e, xi=xi, fb=fb, c0=c0: e.dma_start(
                        out=xt[xi][:], in_=C.x[c0:c0 + 512, fb * 128:(fb + 1) * 128].rearrange("(t p) c -> p t c", p=128)),
                        writes=["xt%d" % xi])
                    pi = cnt["pm"] % 4
                    cnt["pm"] += 1
                    for kc in range(32):
                        S.op("pe", lambda e, pi=pi, wi=wi, kc=kc, hs=hs: e.matmul(out=pm[pi][:], lhsT=wb[wi][:, kc, :], rhs=mT[:, kc, hs],
                                                                                   start=(kc == 0), stop=(kc == 31)),
                             reads=["wb%d" % wi] + (mkeys if kc in (0, 31) else []), writes=["pm%d" % pi])
                    hi = cnt["hf"] % 2
                    cnt["hf"] += 1
                    S.op("act", lambda e, hi=hi, pi=pi: e.activation(out=hf[hi][:], in_=pm[pi][:], func=AF.Copy),
                         reads=["pm%d" % pi], writes=["hf%d" % hi])
                    ti = cnt["pT"] % 2
                    cnt["pT"] += 1
                    for t in range(4):
                        S.op("pe", lambda e, hi=hi, ti=ti, t=t: e.transpose(out=pT[ti][:, t * 128:(t + 1) * 128],
                                                                            in_=hf[hi][:, t * 128:(t + 1) * 128], identity=idf[:]),
                             reads=["hf%d" % hi, "idf"], writes=["pT%d" % ti])
                    S.op("dve", lambda e, xi=xi, ti=ti: e.tensor_tensor(out=ho[xi][:], in0=pT[ti][:].rearrange("p (t c) -> p t c", t=4),
                                                                        in1=xt[xi][:], op=ALU.add),
                         reads=["pT%d" % ti, "xt%d" % xi], writes=["ho%d" % xi])
                    S.dma("act", lambda e, xi=xi, fb=fb, c0=c0: e.dma_start(
                        out=C.h[c0:c0 + 512, fb * 128:(fb + 1) * 128].rearrange("(t p) c -> p t c", p=128), in_=ho[xi][:]),
                        reads=["ho%d" % xi])
        S.barrier()
        S.flush()


def stage4(nc, S, C, tiles=range(6)):
    with ExitStack() as st:
        T = {}
        T["xnT"] = _sb(nc, st, "xnT", [128, 32, 512], BF16)
        T["xs"] = _sb(nc, st, "xs", [128, D], F32)
        T["gb"] = _sb(nc, st, "gb", [128, D], F32)
        T["xnb"] = _sb(nc, st, "xnb", [128, D], BF16)
        T["ss"] = _sb(nc, st, "ss", [128, 4], F32)
        T["idb"] = _sb(nc, st, "idb", [128, 128], BF16)
        T["epsb"] = _sb(nc, st, "epsb", [128, 1], F32)
        idf = _sb(nc, st, "idf", [128, 128], F32)
        wst = [_sb(nc, st, "wst%d" % i, [128, 32, 128], F32) for i in range(2)]
        wb = [_sb(nc, st, "wb%d" % i, [128, 32, 128], BF16) for i in range(2)]
        pq = _sb(nc, st, "pq", [128, 16, 512], BF16)
        skl = _sb(nc, st, "skl", [128, 16, 128], F32)
        KT = _sb(nc, st, "KT", [128, 16, 128], BF16)
        sc = _sb(nc, st, "sc", [128, 16, 128], F32)
        tmp = _sb(nc, st, "tmp", [128, 16, 128], F32)
        v16 = _sb(nc, st, "v16", [128, 16, 16], F32)
        cand = [_sb(nc, st, "cand%d" % i, [128, 256], F32) for i in range(3)]
        c24 = _sb(nc, st, "c24", [128, 8, 24], F32)
        e16 = _sb(nc, st, "e16", [128, 16], F32)
        rt = _sb(nc, st, "rt", [128, 8, 8], F32)
        P2 = [_sb(nc, st, "P2_%d" % i, [128, 8, 128], F32) for i in range(2)]
        TH = [_sb(nc, st, "TH_%d" % i, [128, 8, 128], F32) for i in range(2)]
        WG = [_sb(nc, st, "WG_%d" % i, [128, 8, 128], F32) for i in range(2)]
        pm = [_ps(nc, st, "pm%d" % i, [128, 512], F32) for i in range(4)]
        T["pt"] = [_ps(nc, st, "pt%d" % i, [128, 1024], BF16) for i in range(2)]
        pk = _ps(nc, st, "pk", [128, 512], F32)
        xnT, idb = T["xnT"], T["idb"]
        S.dma("sp", lambda e: e.dma_start(out=T["gb"][:], in_=C.g_ffn.partition_broadcast(128)), writes=["gb"])
        S.dma("sp", lambda e: e.dma_start(out=idb[:], in_=C.ident_bf), writes=["idb"])
        S.dma("sp", lambda e: e.dma_start(out=idf[:], in_=C.ident_f), writes=["idf"])
        S.op("dve", lambda e: e.memset(T["epsb"][:], EPS), writes=["epsb"])
        S.dma("sp", lambda e: e.dma_start(out=skl[:].rearrange("p (h s) c -> p h s c", s=2)[:, :, 0, :],
                                          in_=C.sk1.rearrange("h n c -> n h c")), writes=["skl"])
        S.dma("sp", lambda e: e.dma_start(out=skl[:].rearrange("p (h s) c -> p h s c", s=2)[:, :, 1, :],
                                          in_=C.sk2.rearrange("h n c -> n h c")), writes=["skl"])
        for g in range(4):
            for k in range(4):
                hs = g * 4 + k
                S.op("pe", lambda e, k=k, hs=hs: e.transpose(out=pk[:, k * 128:(k + 1) * 128], in_=skl[:, hs, :], identity=idf[:]),
                     reads=["skl", "idf"], writes=["pk"])
            S.op("dve", lambda e, g=g: e.tensor_copy(out=KT[:, g * 4:(g + 1) * 4, :], in_=pk[:].rearrange("p (k n) -> p k n", k=4)),
                 reads=["pk"], writes=["KT"])
        cnt = {"w": 0, "pm": 0, "tb": 0}

        def tile(tt):
            tok0 = tt * 512
            for tb in range(4):
                norm_transpose(S, T, C.h[tok0 + tb * 128: tok0 + (tb + 1) * 128, :], tb * 128, tb)
            xkeys = xnT_keys(range(4))
            S.dma("act", lambda e: e.dma_start(
                out=C.xn2T[:, tok0:tok0 + 512].rearrange("(kc p) t -> p kc t", p=128), in_=xnT[:]), reads=xkeys)
            for fb in range(16):
                wi = cnt["w"] % 2
                cnt["w"] += 1
                S.dma("sp", lambda e, wi=wi, fb=fb: e.dma_start(
                    out=wst[wi][:], in_=C.w_query[:, fb * 128:(fb + 1) * 128].rearrange("(kc p) f -> p kc f", p=128)),
                    writes=["wst%d" % wi])
                S.op("dve", lambda e, wi=wi: e.tensor_copy(out=wb[wi][:], in_=wst[wi][:]),
                     reads=["wst%d" % wi], writes=["wb%d" % wi])
                for half in range(1):
                    pi = cnt["pm"] % 4
                    cnt["pm"] += 1
                    for kc in range(32):
                        S.op("pe", lambda e, pi=pi, wi=wi, kc=kc, half=half: e.matmul(
                            out=pm[pi][:], lhsT=wb[wi][:, kc, :], rhs=xnT[:, kc, half * 512:(half + 1) * 512],
                            start=(kc == 0), stop=(kc == 31)),
                            reads=["wb%d" % wi] + (xkeys if kc in (0, 31) else []), writes=["pm%d" % pi])
                    S.op("act", lambda e, pi=pi, fb=fb, half=half: e.activation(
                        out=pq[:, fb, half * 512:(half + 1) * 512], in_=pm[pi][:], func=AF.Copy),
                        reads=["pm%d" % pi], writes=[("pq", fb)])
            pqkeys = [("pq", fb) for fb in range(16)]

            def route(tb):
                ri = cnt["tb"] % 2
                cnt["tb"] += 1
                r0 = tok0 + tb * 128
                for g in range(4):
                    pi = cnt["pm"] % 4
                    cnt["pm"] += 1
                    for k in range(4):
                        hs = g * 4 + k
                        S.op("pe", lambda e, pi=pi, k=k, hs=hs: e.matmul(
                            out=pm[pi][:, k * 128:(k + 1) * 128], lhsT=pq[:, hs, tb * 128:(tb + 1) * 128], rhs=KT[:, hs, :],
                            start=True, stop=True), reads=pqkeys + ["KT"], writes=["pm%d" % pi])
                    S.op("act", lambda e, pi=pi, g=g: e.activation(
                        out=sc[:, g * 4:(g + 1) * 4, :], in_=pm[pi][:].rearrange("p (k n) -> p k n", k=4), func=AF.Copy),
                        reads=["pm%d" % pi], writes=[("sc", g)])
                sck = [("sc", g) for g in range(4)]
                for hs in range(16):
                    S.op("dve", lambda e, hs=hs: e.max(out=v16[:, hs, 0:8], in_=sc[:, hs, :]),
                         reads=sck, writes=[("v16a", hs)])
                    S.op("dve", lambda e, hs=hs: e.match_replace(out=tmp[:, hs, :], in_to_replace=v16[:, hs, 0:8],
                                                                 in_values=sc[:, hs, :], imm_value=-1e30),
                         reads=[("v16a", hs)] + sck, writes=[("tmp", hs)])
                    S.op("dve", lambda e, hs=hs: e.max(out=v16[:, hs, 8:16], in_=tmp[:, hs, :]),
                         reads=[("tmp", hs)], writes=[("v16b", hs)])
                for h in range(8):
                    S.op("dve", lambda e, h=h: e.tensor_tensor(
                        out=cand[0][:].rearrange("p (a b) -> p a b", a=16),
                        in0=v16[:, 2 * h, :].unsqueeze(2).to_broadcast([128, 16, 16]),
                        in1=v16[:, 2 * h + 1, :].unsqueeze(1).to_broadcast([128, 16, 16]), op=ALU.add),
                        reads=[("v16a", 2 * h), ("v16b", 2 * h), ("v16a", 2 * h + 1), ("v16b", 2 * h + 1)], writes=["cand0"])
                    S.op("dve", lambda e, h=h: e.max(out=c24[:, h, 0:8], in_=cand[0][:]), reads=["cand0"], writes=["c24a"])
                    S.op("dve", lambda e, h=h: e.match_replace(out=cand[1][:], in_to_replace=c24[:, h, 0:8], in_values=cand[0][:],
                                                               imm_value=-1e30), reads=["cand0", "c24a"], writes=["cand1"])
                    S.op("dve", lambda e, h=h: e.max(out=c24[:, h, 8:16], in_=cand[1][:]), reads=["cand1"], writes=["c24b"])
                    S.op("dve", lambda e, h=h: e.match_replace(out=cand[2][:], in_to_replace=c24[:, h, 8:16], in_values=cand[1][:],
                                                               imm_value=-1e30), reads=["cand1", "c24b"], writes=["cand2"])
                    S.op("dve", lambda e, h=h: e.max(out=c24[:, h, 16:24], in_=cand[2][:]), reads=["cand2"], writes=[("c24", h)])
                ck = [("c24", h) for h in range(8)] + ["c24a", "c24b"]
                vk = [("v16a", hs) for hs in range(16)]
                S.op("dve", lambda e: e.tensor_tensor(out=rt[:, :, 0], in0=c24[:, :, 15], in1=c24[:, :, 16], op=ALU.add),
                     reads=ck, writes=["rt0"])
                S.op("dve", lambda e: e.tensor_scalar_mul(out=rt[:, :, 0], in0=rt[:, :, 0], scalar1=0.5), reads=["rt0"], writes=["rt0"])
                S.op("dve", lambda e: e.tensor_scalar_mul(out=rt[:, :, 1], in0=c24[:, :, 0], scalar1=-1.0), reads=ck, writes=["rt1"])
                v16h = v16[:].rearrange("p (h s) k -> p h s k", s=2)
                S.op("dve", lambda e: e.tensor_tensor(out=rt[:, :, 2], in0=rt[:, :, 0], in1=v16h[:, :, 1, 0], op=ALU.subtract),
                     reads=["rt0"] + vk, writes=["rt2"])
                S.op("dve", lambda e: e.tensor_scalar_mul(out=rt[:, :, 3], in0=v16h[:, :, 0, 0], scalar1=-1.0), reads=vk, writes=["rt3"])
                S.op("dve", lambda e: e.tensor_scalar_mul(out=rt[:, :, 4], in0=v16h[:, :, 1, 0], scalar1=-1.0), reads=vk, writes=["rt4"])
                for h in range(8):
                    S.op("act", lambda e, h=h: e.activation(out=e16[:], in_=c24[:, h, 0:16], func=AF.Exp, bias=rt[:, h, 1:2],
                                                            accum_out=rt[:, h, 5:6]),
                         reads=ck + ["rt1"], writes=["e16", ("Z", h)])
                    S.op("act", lambda e, h=h: e.activation(out=P2[ri][:, h, :], in_=sc[:, 2 * h + 1, :], func=AF.Exp,
                                                            bias=rt[:, h, 4:5]), reads=sck + ["rt4"], writes=["P2_%d" % ri])
                    S.op("act", lambda e, h=h: e.activation(out=TH[ri][:, h, :], in_=sc[:, 2 * h, :], func=AF.Exp,
                                                            bias=rt[:, h, 2:3], scale=-1.0), reads=sck + ["rt2"], writes=["TH_%d" % ri])
                    S.op("act", lambda e, h=h: e.activation(out=WG[ri][:, h, :], in_=sc[:, 2 * h, :], func=AF.Exp,
                                                            bias=rt[:, h, 3:4]), reads=sck + ["rt3"], writes=[("WG", ri, h)])
                S.op("dve", lambda e: e.reciprocal(out=rt[:, :, 6], in_=rt[:, :, 5]), reads=[("Z", h) for h in range(8)], writes=["rZ"])
                for h in range(8):
                    S.op("dve", lambda e, h=h: e.tensor_scalar(out=tmp[:, h, :], in0=sc[:, 2 * h, :], scalar1=v16[:, 2 * h, 15:16],
                                                               scalar2=1e30, op0=ALU.is_lt, op1=ALU.mult),
                         reads=sck + [("v16b", 2 * h)], writes=[("tmp", h)])
                    S.op("dve", lambda e, h=h: e.tensor_tensor(out=TH[ri][:, h, :], in0=TH[ri][:, h, :], in1=tmp[:, h, :], op=ALU.add),
                         reads=[("tmp", h), "TH_%d" % ri], writes=["TH_%d" % ri])
                    S.op("dve", lambda e, h=h: e.scalar_tensor_tensor(out=P2[ri][:, h, :], in0=sc[:, 2 * h + 1, :],
                                                                      scalar=v16[:, 2 * h + 1, 15:16], in1=P2[ri][:, h, :],
                                                                      op0=ALU.is_ge, op1=ALU.mult),
                         reads=sck + [("v16b", 2 * h + 1), "P2_%d" % ri], writes=["P2_%d" % ri])
                for h in range(8):
                    S.op("dve", lambda e, h=h: e.tensor_scalar_mul(out=WG[ri][:, h, :], in0=WG[ri][:, h, :], scalar1=rt[:, h, 6:7]),
                         reads=[("WG", ri, h), "rZ"], writes=[("WG", ri, h)])
                S.dma("act", lambda e: e.dma_start(out=C.P2[r0:r0 + 128, :], in_=P2[ri][:].rearrange("p h n -> p (h n)")),
                      reads=["P2_%d" % ri])
                S.dma("act", lambda e: e.dma_start(out=C.TH[r0:r0 + 128, :], in_=TH[ri][:].rearrange("p h n -> p (h n)")),
                      reads=["TH_%d" % ri])
                S.dma("act", lambda e: e.dma_start(out=C.WG[r0:r0 + 128, :], in_=WG[ri][:].rearrange("p h n -> p (h n)")),
                      reads=[("WG", ri, h) for h in range(8)])

            for tb in range(4):
                route(tb)

        for tt in tiles:
            tile(tt)
        S.barrier()
        S.flush()


def stage5a(nc, S, C, egs=range(32), vblks=range(128)):
    with ExitStack() as st:
        us = [_sb(nc, st, "us%d" % i, [128, D], F32) for i in range(2)]
        ub = [_sb(nc, st, "ub%d" % i, [128, D], BF16) for i in range(2)]
        ut = [_sb(nc, st, "ut%d" % i, [128, 32, 512], BF16) for i in range(2)]
        idb = _sb(nc, st, "idb", [128, 128], BF16)
        pt = [_ps(nc, st, "pt%d" % i, [128, 1024], BF16) for i in range(4)]
        S.dma("sp", lambda e: e.dma_start(out=idb[:], in_=C.ident_bf), writes=["idb"])
        cnt = {"u": 0, "pt": 0, "ev": 0}
        for eg in egs:
            ui = eg % 2
            for sub in range(4):
                eb = eg * 4 + sub
                bi = cnt["u"] % 2
                cnt["u"] += 1
                S.dma("sp", lambda e, bi=bi, eb=eb: e.dma_start(out=us[bi][:], in_=C.eu[eb * 128:(eb + 1) * 128, :]),
                      writes=["us%d" % bi])
                S.op("pool", lambda e, bi=bi: e.tensor_copy(out=ub[bi][:], in_=us[bi][:]), reads=["us%d" % bi], writes=["ub%d" % bi])
                for g in range(4):
                    pi = cnt["pt"] % 4
                    cnt["pt"] += 1
                    for k in range(8):
                        kc = g * 8 + k
                        S.op("pe", lambda e, pi=pi, bi=bi, k=k, kc=kc: e.transpose(
                            out=pt[pi][:, k * 128:(k + 1) * 128], in_=ub[bi][:, kc * 128:(kc + 1) * 128], identity=idb[:]),
                            reads=["ub%d" % bi, "idb"], writes=["pt%d" % pi])
                    eng = "act" if cnt["ev"] % 2 == 0 else "dve"
                    cnt["ev"] += 1
                    if eng == "act":
                        S.op("act", lambda e, pi=pi, ui=ui, g=g, sub=sub: e.activation(
                            out=ut[ui][:, g * 8:(g + 1) * 8, sub * 128:(sub + 1) * 128],
                            in_=pt[pi][:].rearrange("p (k t) -> p k t", k=8), func=AF.Copy),
                            reads=["pt%d" % pi], writes=["ut%d" % ui])
                    else:
                        S.op("dve", lambda e, pi=pi, ui=ui, g=g, sub=sub: e.tensor_copy(
                            out=ut[ui][:, g * 8:(g + 1) * 8, sub * 128:(sub + 1) * 128],
                            in_=pt[pi][:].rearrange("p (k t) -> p k t", k=8)),
                            reads=["pt%d" % pi], writes=["ut%d" % ui])
            S.dma("act", lambda e, ui=ui, eg=eg: e.dma_start(
                out=C.UT[:, eg * 512:(eg + 1) * 512].rearrange("(kc p) n -> p kc n", p=128), in_=ut[ui][:]),
                reads=["ut%d" % ui])
        for vbk in vblks:
            bi = cnt["u"] % 2
            cnt["u"] += 1
            S.dma("sp", lambda e, bi=bi, vbk=vbk: e.dma_start(out=us[bi][:], in_=C.ev[vbk * 128:(vbk + 1) * 128, :]),
                  writes=["us%d" % bi])
            S.op("pool", lambda e, bi=bi: e.tensor_copy(out=ub[bi][:], in_=us[bi][:]), reads=["us%d" % bi], writes=["ub%d" % bi])
            S.dma("act", lambda e, bi=bi, vbk=vbk: e.dma_start(out=C.Vb[vbk * 128:(vbk + 1) * 128, :], in_=ub[bi][:]),
                  reads=["ub%d" % bi])
        S.barrier()
        S.flush()


def stage5b(nc, S, C, tiles=range(12), egs=range(32), dps=range(4), necs=16):
    with ExitStack() as st:
        xt = _sb(nc, st, "xt", [128, 32, 256], BF16)
        P2 = _sb(nc, st, "P2", [128, 2, 1024], F32)
        TH = _sb(nc, st, "TH", [128, 2, 1024], F32)
        WG = _sb(nc, st, "WG", [128, 2, 1024], F32)
        ut = [_sb(nc, st, "ut%d" % i, [128, 32, 512], BF16) for i in range(2)]
        gl = [_sb(nc, st, "gl%d" % i, [128, 512], F32) for i in range(2)]
        G = [_sb(nc, st, "G%d" % i, [128, 512], F32) for i in range(2)]
        mk = [_sb(nc, st, "mk%d" % i, [128, 8, 128], F32) for i in range(4)]
        Ab = [_sb(nc, st, "Ab%d" % i, [128, 512], BF16) for i in range(2)]
        at = [_sb(nc, st, "at%d" % i, [128, 4, 128], BF16) for i in range(2)]
        atc = [_sb(nc, st, "atc%d" % i, [128, 8, 256], BF16) for i in range(2)]
        vbc = [_sb(nc, st, "vbc%d" % i, [128, 8, 1024], BF16) for i in range(2)]
        ht = [_sb(nc, st, "ht%d" % i, [128, 512], F32) for i in range(2)]
        idb = _sb(nc, st, "idb", [128, 128], BF16)
        pm = [_ps(nc, st, "pm%d" % i, [128, 512], F32) for i in range(2)]
        pa = _ps(nc, st, "pa", [128, 1024], BF16)
        acc = [_ps(nc, st, "acc%d" % i, [128, 512], F32) for i in range(4)]
        S.dma("sp", lambda e: e.dma_start(out=idb[:], in_=C.ident_bf), writes=["idb"])
        cnt = {"ut": 0, "pm": 0, "gl": 0, "G": 0, "Ab": 0, "at": 0, "c": 0, "ht": 0}
        POOL_BB = (2, 3)

        def hblock(tt, eg, tkb):
            tok0 = tt * 256
            pi = cnt["pm"] % 2
            cnt["pm"] += 1
            ui = cnt["ut"] % 2
            for kc in range(32):
                S.op("pe", lambda e, kc=kc: e.matmul(out=pm[pi][:], lhsT=xt[:, kc, tkb * 128:(tkb + 1) * 128], rhs=ut[ui][:, kc, :],
                                                     start=(kc == 0), stop=(kc == 31)),
                     reads=["xt", "ut%d" % ui], writes=["pm%d" % pi])
            gi = cnt["gl"] % 2
            cnt["gl"] += 1
            S.op("act", lambda e: e.activation(out=gl[gi][:], in_=pm[pi][:], func=AF.Gelu_apprx_tanh),
                 reads=["pm%d" % pi], writes=["gl%d" % gi])
            p2v = P2[:, tkb, :].rearrange("p (h n) -> p h n", h=8)
            thv = TH[:, tkb, :].rearrange("p (h n) -> p h n", h=8)
            wgv = WG[:, tkb, :].rearrange("p (h n) -> p h n", h=8)
            Gi = cnt["G"] % 2
            cnt["G"] += 1
            Gt = G[Gi]
            for step in range(4):
                for bb in range(4):
                    b = eg * 4 + bb
                    en = "pool" if (bb in POOL_BB and step >= 1) else "dve"
                    m = mk[bb]
                    mkey = "mk%d" % bb
                    if step == 0:
                        S.op(en, lambda e, b=b, m=m: e.tensor_tensor(out=m[:], in0=p2v,
                                                                      in1=thv[:, :, b:b + 1].to_broadcast([128, 8, 128]), op=ALU.is_ge),
                             reads=["P2", "TH"], writes=[mkey])
                    elif step == 1:
                        S.op(en, lambda e, m=m: e.tensor_tensor(out=m[:], in0=m[:], in1=p2v, op=ALU.mult),
                             reads=[mkey, "P2"], writes=[mkey])
                    elif step == 2:
                        S.op(en, lambda e, b=b, m=m: e.tensor_tensor(out=m[:], in0=m[:],
                                                                      in1=wgv[:, :, b:b + 1].to_broadcast([128, 8, 128]), op=ALU.mult),
                             reads=[mkey, "WG"], writes=[mkey])
                    elif en == "dve":
                        S.op(en, lambda e, bb=bb, m=m: e.tensor_reduce(out=Gt[:, bb * 128:(bb + 1) * 128],
                                                                        in_=m[:].rearrange("p h n -> p n h"), axis=AX.X, op=ALU.add),
                             reads=[mkey], writes=[("G", Gi, bb)])
                    else:
                        S.op(en, lambda e, m=m: e.tensor_tensor(out=m[:, 0:4, :], in0=m[:, 0:4, :], in1=m[:, 4:8, :], op=ALU.add),
                             reads=[mkey], writes=[mkey])
                        S.op(en, lambda e, m=m: e.tensor_tensor(out=m[:, 0:2, :], in0=m[:, 0:2, :], in1=m[:, 2:4, :], op=ALU.add),
                             reads=[mkey], writes=[mkey])
                        S.op(en, lambda e, bb=bb, m=m: e.tensor_tensor(out=Gt[:, bb * 128:(bb + 1) * 128], in0=m[:, 0, :], in1=m[:, 1, :],
                                                                        op=ALU.add),
                             reads=[mkey], writes=[("G", Gi, bb)])
            ai = cnt["Ab"] % 2
            cnt["Ab"] += 1
            S.op("dve", lambda e: e.tensor_tensor(out=Ab[ai][:], in0=gl[gi][:], in1=Gt[:], op=ALU.mult),
                 reads=["gl%d" % gi] + [("G", Gi, bb) for bb in range(4)], writes=["Ab%d" % ai])
            for bb in range(4):
                S.op("pe", lambda e, bb=bb: e.transpose(out=pa[:, bb * 128:(bb + 1) * 128], in_=Ab[ai][:, bb * 128:(bb + 1) * 128],
                                                        identity=idb[:]), reads=["Ab%d" % ai, "idb"], writes=["pa"])
            ti = cnt["at"] % 2
            cnt["at"] += 1
            S.op("act", lambda e: e.activation(out=at[ti][:], in_=pa[:, 0:512].rearrange("p (b t) -> p b t", b=4), func=AF.Copy),
                 reads=["pa"], writes=["at%d" % ti])
            S.dma("act", lambda e: e.dma_start(
                out=C.AT[eg * 512:(eg + 1) * 512, tok0 + tkb * 128:tok0 + (tkb + 1) * 128].rearrange("(b p) t -> p b t", p=128),
                in_=at[ti][:]), reads=["at%d" % ti], writes=[("AT", tt, eg // 2)])

        def av_chunk(tt, dp, ec):
            tok0 = tt * 256
            ci = cnt["c"] % 2
            cnt["c"] += 1
            S.dma("sp", lambda e: e.dma_start(
                out=atc[ci][:], in_=C.AT[ec * 1024:(ec + 1) * 1024, tok0:tok0 + 256].rearrange("(k p) t -> p k t", p=128)),
                reads=[("AT", tt, ec)], writes=["atc%d" % ci])
            S.dma("sp", lambda e: e.dma_start(
                out=vbc[ci][:], in_=C.Vb[ec * 1024:(ec + 1) * 1024, dp * 1024:(dp + 1) * 1024].rearrange("(k p) n -> p k n", p=128)),
                writes=["vbc%d" % ci])
            for k in range(8):
                for tkb in range(2):
                    for dg in range(2):
                        ac = tkb * 2 + dg
                        S.op("pe", lambda e, k=k, tkb=tkb, dg=dg, ac=ac: e.matmul(
                            out=acc[ac][:], lhsT=atc[ci][:, k, tkb * 128:(tkb + 1) * 128],
                            rhs=vbc[ci][:, k, dg * 512:(dg + 1) * 512],
                            start=(ec == 0 and k == 0), stop=(ec == necs - 1 and k == 7)),
                            reads=["atc%d" % ci, "vbc%d" % ci], writes=["acc%d" % ac])
            if ec == necs - 1:
                for tkb in range(2):
                    for dg in range(2):
                        ac = tkb * 2 + dg
                        hi = cnt["ht"] % 2
                        cnt["ht"] += 1
                        r0 = tok0 + tkb * 128
                        c0 = dp * 1024 + dg * 512
                        S.dma("sp", lambda e, hi=hi, r0=r0, c0=c0: e.dma_start(out=ht[hi][:], in_=C.h[r0:r0 + 128, c0:c0 + 512]),
                              writes=["ht%d" % hi])
                        S.op("dve", lambda e, hi=hi, ac=ac: e.tensor_tensor(out=ht[hi][:], in0=acc[ac][:], in1=ht[hi][:], op=ALU.add),
                             reads=["acc%d" % ac, "ht%d" % hi], writes=["ht%d" % hi])
                        S.dma("act", lambda e, hi=hi, r0=r0, c0=c0: e.dma_start(out=C.hp[r0:r0 + 128, c0:c0 + 512], in_=ht[hi][:]),
                              reads=["ht%d" % hi])

        pending = []
        for tt in tiles:
            tok0 = tt * 256
            S.dma("sp", lambda e, tok0=tok0: e.dma_start(out=xt[:], in_=C.xn2T[:, tok0:tok0 + 256].rearrange("(kc p) t -> p kc t", p=128)),
                  writes=["xt"])
            for (dst, src, nm) in ((P2, C.P2, "P2"), (TH, C.TH, "TH"), (WG, C.WG, "WG")):
                S.dma("sp", lambda e, dst=dst, src=src, tok0=tok0: e.dma_start(
                    out=dst[:], in_=src[tok0:tok0 + 256, :].rearrange("(t p) n -> p t n", p=128)), writes=[nm])
            for eg in egs:
                ui = cnt["ut"] % 2
                S.dma("sp", lambda e, ui=ui, eg=eg: e.dma_start(
                    out=ut[ui][:], in_=C.UT[:, eg * 512:(eg + 1) * 512].rearrange("(kc p) n -> p kc n", p=128)),
                    writes=["ut%d" % ui])
                for tkb in range(2):
                    hblock(tt, eg, tkb)
                    if pending:
                        av_chunk(*pending.pop(0))
                cnt["ut"] += 1
            while pending:
                av_chunk(*pending.pop(0))
            pending = [(tt, dp, ec) for dp in dps for ec in range(necs)]
        while pending:
            av_chunk(*pending.pop(0))
        S.barrier()
        S.flush()


def stage6(nc, S, C, blks=range(24)):
    with ExitStack() as st:
        xs = [_sb(nc, st, "xs%d" % i, [128, D], F32) for i in range(2)]
        yo = [_sb(nc, st, "yo%d" % i, [128, D], F32) for i in range(2)]
        gb = _sb(nc, st, "gb", [128, D], F32)
        ss = _sb(nc, st, "ss", [128, 4], F32)
        epsb = _sb(nc, st, "epsb", [128, 1], F32)
        S.dma("sp", lambda e: e.dma_start(out=gb[:], in_=C.g_final.partition_broadcast(128)), writes=["gb"])
        S.op("dve", lambda e: e.memset(epsb[:], EPS), writes=["epsb"])
        for n, tb in enumerate(blks):
            i = n % 2
            S.dma("sp", lambda e, i=i, tb=tb: e.dma_start(out=xs[i][:], in_=C.hp[tb * 128:(tb + 1) * 128, :]), writes=["xs%d" % i])
            S.op("act", lambda e, i=i: e.activation(out=yo[i][:], in_=xs[i][:], func=AF.Square, accum_out=ss[:, 0:1]),
                 reads=["xs%d" % i], writes=["yo%d" % i, "ss0"])
            S.op("act", lambda e: e.activation(out=ss[:, 1:2], in_=ss[:, 0:1], func=AF.Sqrt, scale=1.0 / D, bias=epsb[:, 0:1]),
                 reads=["ss0", "epsb"], writes=["ss1"])
            S.op("dve", lambda e: e.reciprocal(out=ss[:, 2:3], in_=ss[:, 1:2]), reads=["ss1"], writes=["ss2"])
            S.op("dve", lambda e, i=i: e.scalar_tensor_tensor(out=yo[i][:], in0=xs[i][:], scalar=ss[:, 2:3], in1=gb[:],
                                                              op0=ALU.mult, op1=ALU.mult),
                 reads=["xs%d" % i, "ss2", "gb"], writes=["yo%d" % i])
            S.dma("act", lambda e, i=i, tb=tb: e.dma_start(out=C.y[tb * 128:(tb + 1) * 128, :], in_=yo[i][:]), reads=["yo%d" % i])
        S.barrier()
        S.flush()

def build(stages=("s1",), debug_outs=(), s1_tiles=(0, 1, 2, 3), s2_kw={}, s3_kw={}, s4_kw={}, s5a_kw={}, s5_kw={}, s6_kw={}):
    nc = bass.Bass("TRN2", target_bir_lowering=False)
    C = Ctx()

    def din(name, shape, dt=F32):
        return nc.dram_tensor(name, shape, dt, kind="ExternalInput").ap()

    def dscr(name, shape, dt):
        kind = "ExternalOutput" if name in debug_outs else "Internal"
        return nc.dram_tensor(name, shape, dt, kind=kind).ap()

    C.x = din("x", [TK, D])
    C.w_in = din("w_in", [D, INW])
    C.w_pa = din("w_proj_a", [AW, D])
    C.w_pb = din("w_proj_b", [BW, D])
    C.w_out = din("w_out", [D, D])
    C.g_mix = din("g_mix_norm", [1, D])
    C.lam = din("lam4", [1, 512])
    C.g_subln = din("g_subln", [1, 256])
    C.g_ffn = din("g_ffn_norm", [1, D])
    C.w_query = din("w_query", [D, 2048])
    C.sk1 = din("sub_keys_1", [8, 128, 128])
    C.sk2 = din("sub_keys_2", [8, 128, 128])
    C.eu = din("expert_u", [NE, D])
    C.ev = din("expert_v", [NE, D])
    C.g_final = din("g_final", [1, D])
    C.ident_bf = din("ident_bf", [128, 128], BF16)
    C.ident_f = din("ident_f", [128, 128], F32)
    C.abt = din("abt", [128, ABW])
    C.lct = din("lct", [128, ABW])
    C.y = nc.dram_tensor("y", [TQ, D], F32, kind="ExternalOutput").ap()

    C.qaT = dscr("qaT", [AW, TQ], BF16)
    C.kaT = dscr("kaT", [AW, TK], BF16)
    C.va = dscr("va", [TK, AW], BF16)
    C.qbT = dscr("qbT", [BW, TQ], BF16)
    C.kbT = dscr("kbT", [BW, TK], BF16)
    C.vb = dscr("vb", [TK, BW], BF16)
    C.sga = dscr("sga", [D, TQ], F32)
    C.sgb = dscr("sgb", [D, TQ], F32)
    C.oaT = dscr("oaT", [AW, TQ], BF16)
    C.obT = dscr("obT", [BW, TQ], BF16)
    C.h = dscr("h", [TQ, D], F32)
    C.xn2T = dscr("xn2T", [D, TQ], BF16)
    C.P2 = dscr("P2", [TQ, 1024], F32)
    C.TH = dscr("TH", [TQ, 1024], F32)
    C.WG = dscr("WG", [TQ, 1024], F32)
    C.UT = dscr("UT", [D, NE], BF16)
    C.Vb = dscr("Vb", [NE, D], BF16)
    C.AT = dscr("AT", [NE, TQ], BF16)
    C.hp = dscr("hp", [TQ, D], F32)

    with ExitStack() as st:
        S = Sched(nc, st)
        if "s1" in stages:
            stage1(nc, S, C, tiles=s1_tiles)
        if "s2" in stages:
            stage2(nc, S, C, **s2_kw)
        if "s3" in stages:
            stage3(nc, S, C, **s3_kw)
        if "s4" in stages:
            stage4(nc, S, C, **s4_kw)
        if "s5a" in stages:
            stage5a(nc, S, C, **s5a_kw)
        if "s5b" in stages:
            stage5b(nc, S, C, **s5_kw)
        if "s6" in stages:
            stage6(nc, S, C, **s6_kw)
        C.n_inst = S.n_inst
    return nc, C


def host_consts():
    p = np.arange(128)[:, None].astype(np.int64)
    c = np.arange(ABW)[None, :].astype(np.int64)
    delta = p - c + 1920
    ad = np.abs(delta)
    cnt = ((ad <= 64).astype(np.int64) + ((delta % 4 == 0) & (ad <= 256)).astype(np.int64)
           + ((delta % 16 == 0) & (ad <= 1024)).astype(np.int64))
    lct = np.where(cnt > 0, np.log(np.maximum(cnt, 1).astype(np.float64)), NEGBIG).astype(np.float32)
    return {
        "ident_bf": np.eye(128, dtype=np.float32).astype(ml_dtypes.bfloat16),
        "ident_f": np.eye(128, dtype=np.float32),
        "abt": ad.astype(np.float32),
        "lct": lct,
    }


def make_in_maps(inputs, n_cores=8):
    xp = np.asarray(inputs["x_prompt"])
    xsm = np.asarray(inputs["x_sample"])
    shared = {
        "w_in": np.ascontiguousarray(np.asarray(inputs["w_in"])[0]),
        "w_proj_a": np.ascontiguousarray(np.asarray(inputs["w_proj_a"])[0]),
        "w_proj_b": np.ascontiguousarray(np.asarray(inputs["w_proj_b"])[0]),
        "w_out": np.ascontiguousarray(np.asarray(inputs["w_out"])[0]),
        "g_mix_norm": np.ascontiguousarray(np.asarray(inputs["g_mix_norm"])[0:1]),
        "lam4": np.ascontiguousarray(np.concatenate([np.asarray(inputs[k])[0:1] for k in
                                                     ("lam_q1", "lam_k1", "lam_q2", "lam_k2")], axis=1)),
        "g_subln": np.ascontiguousarray(np.asarray(inputs["g_subln"])[0:1]),
        "g_ffn_norm": np.ascontiguousarray(np.asarray(inputs["g_ffn_norm"])[0:1]),
        "w_query": np.ascontiguousarray(np.asarray(inputs["w_query"])[0]),
        "sub_keys_1": np.ascontiguousarray(np.asarray(inputs["sub_keys_1"])[0]),
        "sub_keys_2": np.ascontiguousarray(np.asarray(inputs["sub_keys_2"])[0]),
        "expert_u": np.ascontiguousarray(np.asarray(inputs["expert_u"])[0]),
        "expert_v": np.ascontiguousarray(np.asarray(inputs["expert_v"])[0]),
        "g_final": np.ascontiguousarray(np.asarray(inputs["g_final"]).reshape(1, D)),
    }
    shared.update(host_consts())
    maps = []
    for c in range(n_cores):
        xb = xsm[c // 2]
        if c % 2 == 1:
            xb = xb[::-1]
        xall = np.ascontiguousarray(np.concatenate([xp[c], xb], axis=0).astype(np.float32))
        m = dict(shared)
        m["x"] = xall
        maps.append(m)
    return maps


def kernel(**inputs):
    nc, C = build(stages=ALL_STAGES)
    maps = make_in_maps(inputs)
    res = run_bass_kernel_spmd(nc, maps, core_ids=list(range(8)))
    y_prompt = np.zeros((8, SEQ, D), np.float32)
    y_sample = np.zeros((4, SEQ, D), np.float32)
    for c in range(8):
        y = np.asarray(res.results[c]["y"])
        y_prompt[c] = y[0:2048]
        if c % 2 == 0:
            y_sample[c // 2, 0:1024] = y[2048:3072]
        else:
            y_sample[c // 2, 1024:2048] = y[2048:3072][::-1]
    return (y_prompt, y_sample)
```
